# Optimizing a Trainium2 kernel written in Bass

```python
import jax, jax.numpy as jnp
from jax import lax
import numpy as np

D_MODEL = 1024
BATCH = 2
SEQ = 8192
DEPTH = 1

N_HEADS = 8
HEAD_DIM = 64
ATTN_WIDTH = N_HEADS * HEAD_DIM
Q_BLOCK = 128
CONV_WIDTH = D_MODEL // 2
CONV_KERNEL = 31
N_EXPERTS = 32
TOP_K = 4
D_EXPERT = D_MODEL
SWIGLU_LIMIT = 7.0
SWIGLU_ALPHA = 1.702
MOE_BLOCK = 128
RMS_EPS = 1e-5
LN_EPS = 1e-5
N_MOD = 6
COL_SIZES = (CONV_WIDTH, CONV_WIDTH,
             ATTN_WIDTH, ATTN_WIDTH, ATTN_WIDTH,
             N_HEADS,
             D_MODEL, D_MODEL)
IN_COLS = sum(COL_SIZES)

kernel_name = "hybrid_conv_fox_moe_block"


def rms_norm(x, g):
    xf = x.astype(jnp.float32)
    y = xf * lax.rsqrt(jnp.mean(xf * xf, axis=-1, keepdims=True) + RMS_EPS)
    return (y * g.astype(jnp.float32)).astype(x.dtype)


def layer_norm(x, g, b):
    xf = x.astype(jnp.float32)
    mu = jnp.mean(xf, axis=-1, keepdims=True)
    var = jnp.mean(jnp.square(xf - mu), axis=-1, keepdims=True)
    y = (xf - mu) * lax.rsqrt(var + LN_EPS)
    return (y * g.astype(jnp.float32) + b.astype(jnp.float32)).astype(x.dtype)


def conv_module(u_val, u_gate, conv_w, conv_b, ln_g, ln_b, w_proj):
    a = u_val * jax.nn.sigmoid(u_gate)
    a = lax.conv_general_dilated(
        a, conv_w[:, None, :].astype(a.dtype), window_strides=(1,),
        padding=[(CONV_KERNEL - 1, 0)],
        dimension_numbers=('NWC', 'WIO', 'NWC'),
        feature_group_count=CONV_WIDTH) + conv_b
    a = jax.nn.silu(layer_norm(a, ln_g, ln_b))
    return a @ w_proj


def forgetting_attention(q, k, v, f_logit):
    B, S = q.shape[0], q.shape[1]
    n_blocks = S // Q_BLOCK
    log_f = jax.nn.log_sigmoid(f_logit.astype(jnp.float32))
    F = jnp.cumsum(log_f, axis=1).transpose(0, 2, 1)
    kh = k.transpose(0, 2, 1, 3)
    vh = v.transpose(0, 2, 1, 3)
    q_blocks = q.transpose(0, 2, 1, 3).reshape(B, N_HEADS, n_blocks, Q_BLOCK, HEAD_DIM).transpose(2, 0, 1, 3, 4)
    F_blocks = F.reshape(B, N_HEADS, n_blocks, Q_BLOCK).transpose(2, 0, 1, 3)
    k_pos = jnp.arange(S)
    scale = HEAD_DIM ** -0.5

    def one_block(args):
        qi, Fi, bi = args
        q_pos = bi * Q_BLOCK + jnp.arange(Q_BLOCK)
        s = jnp.einsum('bhqd,bhkd->bhqk', qi, kh).astype(jnp.float32) * scale
        s = s + Fi[..., :, None] - F[:, :, None, :]
        s = jnp.where(k_pos[None, :] <= q_pos[:, None], s, -jnp.inf)
        p = jax.nn.softmax(s, axis=-1)
        return jnp.einsum('bhqk,bhkd->bhqd', p.astype(vh.dtype), vh)

    out = lax.map(one_block, (q_blocks, F_blocks, jnp.arange(n_blocks)))
    return out.transpose(1, 0, 3, 2, 4).reshape(B, S, ATTN_WIDTH)


def moe_ffn(h, w_router, b_router, w1, b1, w2, b2):
    B, S, D = h.shape
    T = B * S
    A = T * TOP_K
    hf = h.reshape(T, D)
    logits = (hf @ w_router + b_router).astype(jnp.float32)
    top_v, top_e = lax.top_k(logits, TOP_K)
    gates = jax.nn.softmax(top_v, axis=-1)

    flat_e = top_e.reshape(A).astype(jnp.int32)
    flat_tok = (jnp.arange(A, dtype=jnp.int32) // TOP_K)
    flat_gate = gates.reshape(A)
    order = jnp.argsort(flat_e)
    se, stok, sg = flat_e[order], flat_tok[order], flat_gate[order]
    counts = jnp.bincount(flat_e, length=N_EXPERTS)
    padded = (counts + MOE_BLOCK - 1) // MOE_BLOCK * MOE_BLOCK
    pend = jnp.cumsum(padded)
    pstart = pend - padded
    ustart = jnp.cumsum(counts) - counts
    dest = pstart[se] + jnp.arange(A, dtype=jnp.int32) - ustart[se]
    P = A + N_EXPERTS * MOE_BLOCK
    n_blocks = P // MOE_BLOCK
    tok_buf = jnp.zeros((P,), jnp.int32).at[dest].set(stok)
    gate_buf = jnp.zeros((P,), jnp.float32).at[dest].set(sg)
    block_e = jnp.clip(jnp.searchsorted(pend, jnp.arange(n_blocks) * MOE_BLOCK, side='right'), 0, N_EXPERTS - 1)
    xb = hf[tok_buf].reshape(n_blocks, MOE_BLOCK, D)

    def expert_block(args):
        xi, e = args
        hc = xi @ w1[e] + b1[e]
        g, u = hc[:, :D_EXPERT], hc[:, D_EXPERT:]
        g = jnp.minimum(g, SWIGLU_LIMIT)
        u = jnp.clip(u, -SWIGLU_LIMIT, SWIGLU_LIMIT)
        hm = (u + 1.0) * (g * jax.nn.sigmoid(SWIGLU_ALPHA * g))
        return hm @ w2[e] + b2[e]

    yb = lax.map(expert_block, (xb, block_e)).reshape(P, D)
    y = jax.ops.segment_sum(yb.astype(jnp.float32) * gate_buf[:, None], tok_buf, num_segments=T)
    return y.astype(h.dtype).reshape(B, S, D)


def setup_inputs(seed: int = 0) -> dict:
    key = jax.random.key(seed)
    ks = jax.random.split(key, 24)
    f32 = jnp.float32
    L, D = DEPTH, D_MODEL

    def nrm(k, shape, fan_in, mult=1.0):
        return jax.random.normal(k, shape, f32) * (mult * fan_in ** -0.5)

    return {
        "x": jax.random.normal(ks[0], (BATCH, SEQ, D), f32),
        "c": jax.random.normal(ks[1], (BATCH, D), f32),
        "w_ada": nrm(ks[2], (L, D, N_MOD * D), D, 0.5),
        "b_ada": 0.02 * jax.random.normal(ks[3], (L, N_MOD * D), f32),
        "norm1_g": 1.0 + 0.05 * jax.random.normal(ks[4], (L, D), f32),
        "w_in": nrm(ks[5], (L, D, IN_COLS), D),
        "b_forget": jax.random.uniform(ks[6], (L, N_HEADS), f32, 1.0, 4.0),
        "conv_w": nrm(ks[7], (L, CONV_KERNEL, CONV_WIDTH), CONV_KERNEL),
        "conv_b": 0.02 * jax.random.normal(ks[8], (L, CONV_WIDTH), f32),
        "conv_ln_g": 1.0 + 0.05 * jax.random.normal(ks[9], (L, CONV_WIDTH), f32),
        "conv_ln_b": 0.02 * jax.random.normal(ks[10], (L, CONV_WIDTH), f32),
        "w_conv_out": nrm(ks[11], (L, CONV_WIDTH, D), CONV_WIDTH),
        "w_attn_out": nrm(ks[12], (L, ATTN_WIDTH, D), ATTN_WIDTH),
        "w_out": nrm(ks[13], (L, D, D), D),
        "norm2_g": 1.0 + 0.05 * jax.random.normal(ks[14], (L, D), f32),
        "w_router": nrm(ks[15], (L, D, N_EXPERTS), D),
        "b_router": 0.01 * jax.random.normal(ks[16], (L, N_EXPERTS), f32),
        "w_exp_in": nrm(ks[17], (L, N_EXPERTS, D, 2 * D_EXPERT), D),
        "b_exp_in": 0.02 * jax.random.normal(ks[18], (L, N_EXPERTS, 2 * D_EXPERT), f32),
        "w_exp_out": nrm(ks[19], (L, N_EXPERTS, D_EXPERT, D), D_EXPERT),
        "b_exp_out": 0.02 * jax.random.normal(ks[20], (L, N_EXPERTS, D), f32),
        "final_g": 1.0 + 0.05 * jax.random.normal(ks[21], (D,), f32),
    }


def reference(x, c, w_ada, b_ada, norm1_g, w_in, b_forget, conv_w, conv_b, conv_ln_g, conv_ln_b,
              w_conv_out, w_attn_out, w_out, norm2_g, w_router, b_router, w_exp_in, b_exp_in,
              w_exp_out, b_exp_out, final_g):
    B, S, D = x.shape
    offs = np.cumsum(COL_SIZES)[:-1].tolist()
    for l in range(DEPTH):
        mod = (jax.nn.silu(c) @ w_ada[l] + b_ada[l]).reshape(B, N_MOD, D)
        sh1, sc1, g1, sh2, sc2, g2 = (mod[:, i, None, :] for i in range(N_MOD))

        h = rms_norm(x, norm1_g[l]) * (1.0 + sc1) + sh1
        z = h @ w_in[l]
        u_val, u_gate, q, k, v, f_logit, gate_c, gate_a = jnp.split(z, offs, axis=-1)
        conv_out = conv_module(u_val, u_gate, conv_w[l], conv_b[l], conv_ln_g[l], conv_ln_b[l], w_conv_out[l])
        attn = forgetting_attention(q.reshape(B, S, N_HEADS, HEAD_DIM),
                                    k.reshape(B, S, N_HEADS, HEAD_DIM),
                                    v.reshape(B, S, N_HEADS, HEAD_DIM),
                                    f_logit + b_forget[l])
        attn_out = attn @ w_attn_out[l]
        mixed = jax.nn.sigmoid(gate_c) * conv_out + jax.nn.sigmoid(gate_a) * attn_out
        x = x + g1 * (mixed @ w_out[l])

        h2 = rms_norm(x, norm2_g[l]) * (1.0 + sc2) + sh2
        x = x + g2 * moe_ffn(h2, w_router[l], b_router[l], w_exp_in[l], b_exp_in[l], w_exp_out[l], b_exp_out[l])
    return rms_norm(x, final_g)
```

```python
import numpy as np
from contextlib import ExitStack
import concourse.bass as bass
import concourse.mybir as mybir
from concourse.bass_utils import run_bass_kernel_spmd

F32 = mybir.dt.float32
BF16 = mybir.dt.bfloat16
I32 = mybir.dt.int32
U32 = mybir.dt.uint32
AF = mybir.ActivationFunctionType
ALU = mybir.AluOpType

ENGS = ("sync", "scalar", "vector", "gpsimd", "tensor")
NCORES = 8
D = 1024
KD = 8
S = 8192
NKB = 64
OWN = 2048
NCH = 8
NQB = 16
HALO = 32
CW = 256 + HALO
NE = 32
KMAX = 7
C_UV, C_UG, C_Q, C_K, C_V, C_F, C_GC, C_GA = 0, 512, 1024, 1536, 2048, 2560, 2568, 3592
_DT_SIZE = {F32: 4, BF16: 2, I32: 4, U32: 4}
SEM_ROT = 2000
NPOOL = 24
NSW = 12


class Tok:
    __slots__ = ("sem", "val", "gen")

    def __init__(self, sem=None, val=None, gen=0):
        self.sem = sem
        self.val = val
        self.gen = gen


class Rec:
    __slots__ = ("p0", "p1", "lo", "hi", "w", "tok", "eng", "alive", "name")


def region(ap):
    es = _DT_SIZE[ap.dtype]
    pairs = ap.ap
    off = ap.offset
    sp = str(ap.space)
    if sp in ("SB", "PSUM"):
        pstride, npart = pairs[0]
        if pstride <= 0:
            pstride = 1 << 40
        p0 = off // pstride
        col = off % pstride
        ext = 1 + sum((c - 1) * abs(s) for s, c in pairs[1:])
        return (ap.name, p0, p0 + npart, col * es, (col + ext) * es, 4096)
    ext = 1 + sum((c - 1) * abs(s) for s, c in pairs)
    return (ap.name, 0, 1, off * es, (off + ext) * es, 1 << 20)


class Prog:
    def __init__(self, nc, es):
        self.nc = nc
        self.es = es
        self.streams = {e: [] for e in ENGS}
        self.esem = {}
        self.ecount = {}
        self.nsem = 0
        self.pending = {e: [] for e in ENGS}
        self.bins = {}
        self.pool = []
        self.pool_last = []
        self.pool_i = 0
        self.dma_uid = 0
        self.noguard = 0
        self.guard = None
        self.fake = {}
        self.cnt_ap = None
        for e in ENGS:
            self._new_esem(e)
        for i in range(NPOOL):
            self.pool.append(self._mksem(f"dq{i}"))
            self.pool_last.append(None)
        self.swpool = [self._mksem(f"sq{i}") for i in range(NSW)]
        self.sw_last = [None] * NSW
        self.sw_i = 0

    def _mksem(self, name):
        self.nsem += 1
        return self.es.enter_context(self.nc.semaphore(name))

    def _new_esem(self, e):
        self.esem[e] = self._mksem(f"s_{e}_{self.nsem}")
        self.ecount[e] = 0

    def _bins(self, reg):
        name, p0, p1, lo, hi, bs = reg
        return [(name, b) for b in range(lo // bs, (hi - 1) // bs + 1)]

    def _query(self, reg):
        name, p0, p1, lo, hi, bs = reg
        seen = set()
        out = []
        for key in self._bins(reg):
            lst = self.bins.get(key)
            if not lst:
                continue
            dead = 0
            for r in lst:
                if not r.alive:
                    dead += 1
                    continue
                if id(r) in seen:
                    continue
                if r.lo < hi and lo < r.hi and r.p0 < p1 and p0 < r.p1:
                    seen.add(id(r))
                    out.append(r)
            if dead > 16:
                self.bins[key] = [r for r in lst if r.alive]
        return out

    def _add(self, reg, w, tok, eng):
        name, p0, p1, lo, hi, bs = reg
        r = Rec()
        r.p0, r.p1, r.lo, r.hi, r.w, r.tok, r.eng, r.alive, r.name = p0, p1, lo, hi, w, tok, eng, True, name
        for key in self._bins(reg):
            self.bins.setdefault(key, []).append(r)

    def _deps(self, eng, reads, writes, tok, engkey):
        deps = []
        rregs = [a if isinstance(a, tuple) else region(a) for a in reads]
        wregs = [a if isinstance(a, tuple) else region(a) for a in writes]
        for reg in rregs:
            for r in self._query(reg):
                if r.w and not (eng == "tensor" and r.eng == "tensor"):
                    deps.append(r.tok)
            if reg[0] == "parena":
                name, p0, p1, lo, hi, bs = reg
                breg = (name, 0, 128, (lo // 2048) * 2048, ((hi + 2047) // 2048) * 2048, bs)
                for r in self._query(breg):
                    if (not r.w) and r.eng != engkey and r.eng != "tensor":
                        deps.append(r.tok)
        for reg in wregs:
            for r in self._query(reg):
                if not (eng == "tensor" and r.eng == "tensor"):
                    deps.append(r.tok)
        for reg in rregs:
            name, p0, p1, lo, hi, bs = reg
            rep = False
            for r in self._query(reg):
                if (not r.w) and r.eng == engkey and r.p0 == p0 and r.p1 == p1 and r.lo == lo and r.hi == hi:
                    r.tok = tok
                    rep = True
                    break
            if not rep:
                self._add(reg, False, tok, engkey)
        for reg in wregs:
            name, p0, p1, lo, hi, bs = reg
            for r in self._query(reg):
                if r.p0 >= p0 and r.p1 <= p1 and r.lo >= lo and r.hi <= hi:
                    r.alive = False
            self._add(reg, True, tok, engkey)
        return deps

    def op(self, eng, fn, r=(), w=(), signal=True):
        if signal:
            if self.ecount[eng] >= SEM_ROT:
                self._new_esem(eng)
            self.ecount[eng] += 1
            tok = Tok(self.esem[eng], self.ecount[eng])
            for p in self.pending[eng]:
                p.sem, p.val = tok.sem, tok.val
            self.pending[eng] = []
        else:
            tok = Tok()
            self.pending[eng].append(tok)
        deps = self._deps(eng, r, w, tok, eng)
        self.streams[eng].append((fn, deps, tok if signal else None, 1, self.guard))
        return tok

    def regload(self, key, deps):
        for e in ENGS:
            self.streams[e].append((("regload", key), list(deps), None, 0, None))

    def dma(self, eng, fn, r=(), w=()):
        if eng == "gpsimd":
            i = self.sw_i
            self.sw_i = (self.sw_i + 1) % NSW
            prev = self.sw_last[i]
            val = (prev.val if prev is not None else 0) + 16
            tok = Tok(self.swpool[i], val)
            self.sw_last[i] = tok
            self.dma_uid += 1
            deps = self._deps(eng, r, w, tok, "dma%d" % self.dma_uid)
            if prev is not None:
                deps.append(prev)
            self.streams[eng].append((fn, deps, tok, 16, self.guard))
            return tok
        i = self.pool_i
        self.pool_i = (self.pool_i + 1) % NPOOL
        prev = self.pool_last[i]
        val = (prev.val if prev is not None else 0) + 16
        tok = Tok(self.pool[i], val)
        self.pool_last[i] = tok
        self.dma_uid += 1
        deps = self._deps(eng, r, w, tok, "dma%d" % self.dma_uid)
        if prev is not None:
            deps.append(prev)
        self.streams[eng].append((fn, deps, tok, 16, self.guard))
        return tok

    def emit(self):
        nc = self.nc
        final = [t for t in self.pool_last if t is not None] + [t for t in self.sw_last if t is not None]
        with nc.Block() as block:
            for e in ENGS:
                stream = self.streams[e]
                fw = final if e == "sync" else []

                def body(engine, stream=stream, fw=fw, ename=e):
                    waited = {}
                    lastfake = [None]
                    reg = engine.alloc_register("gcnt")

                    def dowait(d):
                        assert d.sem is not None, "unresolved token"
                        k = (id(d.sem), d.gen)
                        if waited.get(k, 0) >= d.val:
                            return
                        waited[k] = d.val
                        engine.wait_ge(d.sem, d.val)

                    def emit_one(ent):
                        fn, deps, tok, inc, g = ent
                        for d in deps:
                            dowait(d)
                        if isinstance(fn, tuple):
                            if self.noguard != 2:
                                engine.reg_load(reg, self.cnt_ap(fn[1]))
                            return
                        ins = fn(engine)
                        if tok is not None:
                            ins.then_inc(tok.sem, inc)

                    i = 0
                    n = len(stream)
                    while i < n:
                        g = stream[i][4]
                        if g is None or self.noguard:
                            emit_one(stream[i])
                            i += 1
                            continue
                        j = i
                        while j < n and stream[j][4] == g:
                            j += 1
                        grp = stream[i:j]
                        saved = dict(waited)
                        with engine.If_lt(reg, g[1] + 1):
                            for fn, deps, tok, inc, _ in grp:
                                for d in deps:
                                    dowait(d)
                                if tok is None:
                                    continue
                                if inc == 16:
                                    engine.sem_inc(tok.sem, 16)
                                else:
                                    if ename == "scalar" and lastfake[0] is not None:
                                        engine.wait_ge(lastfake[0].sem, lastfake[0].val)
                                    self.fake[ename](engine).then_inc(tok.sem, 1)
                                    lastfake[0] = tok
                        waited.clear()
                        waited.update(saved)
                        with engine.Else():
                            for ent in grp:
                                emit_one(ent)
                        waited.clear()
                        waited.update(saved)
                        i = j
                    for d in fw:
                        dowait(d)

                getattr(block, e)(body)


class Arena:
    def __init__(self, ap, nwords):
        self.ap = ap
        self.n = nwords
        self.off = 0

    def mark(self):
        return self.off

    def reset(self, off=0):
        self.off = off

    def alloc(self, shape, dt):
        free = int(np.prod(shape[1:]))
        words = (free * _DT_SIZE[dt] + 3) // 4
        words += words % 2
        assert self.off + words <= self.n, ("arena overflow", self.off, words, self.n)
        v = self.ap[0:shape[0], self.off:self.off + words]
        self.off += words
        if dt != F32:
            v = v.bitcast(dt)
        v = v[:, 0:free]
        if len(shape) > 2:
            names = [f"a{i}" for i in range(len(shape) - 1)]
            pat = "p (" + " ".join(names) + ") -> p " + " ".join(names)
            v = v.rearrange(pat, **{n: s for n, s in zip(names[:-1], shape[1:-1])})
        return v


def chunks_of(j):
    ch = []
    for g in range(4):
        ch += [8 * g + j, 8 * g + 7 - j]
    return ch


def kb_end(lc):
    g, s = lc // 2, lc % 2
    return 16 * g + 8 * s + 8


def win_start(lc):
    g, s = lc // 2, lc % 2
    return 16 * g + 8 * s


def isap(x):
    return hasattr(x, "ap") and hasattr(x, "dtype") and hasattr(x, "offset")


def build(debug=False, kmax=KMAX, stop_after=99, variant=''):
    cap = kmax * 128
    opts = {}
    for vv in variant.split("_"):
        if len(vv) > 1 and vv[1:].isdigit():
            opts[vv[0]] = int(vv[1:])
    nc = bass.Bass("TRN2", target_bir_lowering=False)
    dram_in = lambda n, shp, dt=F32: nc.dram_tensor(n, list(shp), dt, kind="ExternalInput").ap()
    xfull = dram_in("xfull", [S, D])
    xown = dram_in("xown", [NCH, CW, D])
    halov = dram_in("halov", [128, NCH * HALO])
    cvec = dram_in("cvec", [128, KD])
    w_ada = dram_in("w_ada", [D, 6 * D])
    b_ada_col = dram_in("b_ada_col", [128, 48])
    n1g_col = dram_in("n1g_col", [128, KD])
    n2g_rep = dram_in("n2g_rep", [128, D])
    fin_rep = dram_in("fin_rep", [128, D])
    w_in = dram_in("w_in", [D, 4616])
    bf_col = dram_in("bf_col", [8, 1])
    cwT = dram_in("cwT", [128, 4 * 31])
    cvecs = dram_in("cvecs", [128, 12])
    w_co = dram_in("w_co", [512, D])
    w_ao = dram_in("w_ao", [512, D])
    w_out = dram_in("w_out", [D, D])
    w_router = dram_in("w_router", [D, NE])
    br_rep = dram_in("br_rep", [128, NE])
    qposB = dram_in("qposB", [128, OWN])
    refsel = dram_in("refsel", [128, NKB * NQB])
    kposc = dram_in("kposc", [128, NKB])
    ecap = dram_in("ecap", [128, NE])
    if stop_after >= 5:
        w_e1 = dram_in("w_e1", [NE, D, 2 * D])
        b1c = dram_in("b1c", [128, NE * 16])
        w_e2 = dram_in("w_e2", [NE, D, D])
        b_e2 = dram_in("b_e2", [NE, D])
    out = nc.dram_tensor("out", [OWN, D], F32, kind="ExternalOutput").ap()
    dbg = {}

    def dbg_out(name, shp, dt=F32):
        dbg[name] = nc.dram_tensor("dbg_" + name, list(shp), dt, kind="ExternalOutput").ap()
        return dbg[name]

    scr = lambda n, shp, dt: nc.dram_tensor(n, list(shp), dt, kind="Internal").ap()
    cg_d = scr("cg_d", [NCH, 128, KD * 256], BF16)
    sga_d = scr("sga_d", [NCH, 128, KD * 256], BF16)
    kvB_d = scr("kvB_d", [128, 2 * S + NKB * 4 * 65], BF16)
    x1_d = scr("x1_d", [NQB, 128, D], F32)
    xs_d = scr("xs_d", [NE * cap + 128, D], BF16)
    ys_d = scr("ys_d", [NE * cap + 128, D], F32)
    f_d = scr("f_d", [8, S], F32)

    with ExitStack() as es:
        SBW = 51 * 1024
        sb_all = es.enter_context(nc.sbuf_tensor("arena", [128, SBW], F32))
        ps_all = es.enter_context(nc.psum_tensor("parena", [128, 4096], F32))
        A = Arena(sb_all, SBW)
        P = Prog(nc, es)

        def bank(i, n=1):
            return ps_all[:, i * 512:(i + n) * 512]

        def act(out, in_, func, scale=None, bias=None, accum=None):
            r = [in_] + [x for x in (scale, bias) if isap(x)]
            w = [out] + ([accum] if accum is not None else [])
            kw = {}
            if scale is not None:
                kw["scale"] = scale
            if bias is not None:
                kw["bias"] = bias
            if accum is not None:
                kw["accum_out"] = accum
            return P.op("scalar", lambda e: e.activation(out=out, in_=in_, func=func, **kw), r, w)

        def tsc(eng, out, in0, s1, s2=None, op0=ALU.mult, op1=None):
            r = [in0] + [x for x in (s1, s2) if isap(x)]
            kw = {} if op1 is None else {"op1": op1}
            return P.op(eng, lambda e: e.tensor_scalar(out=out, in0=in0, scalar1=s1, scalar2=s2, op0=op0, **kw), r, [out])

        def tt(eng, out, in0, in1, op):
            return P.op(eng, lambda e: e.tensor_tensor(out=out, in0=in0, in1=in1, op=op), [in0, in1], [out])

        def stt(out, in0, scalar, in1, op0, op1):
            r = [in0, in1] + ([scalar] if isap(scalar) else [])
            return P.op("vector", lambda e: e.scalar_tensor_tensor(out=out, in0=in0, scalar=scalar, in1=in1, op0=op0, op1=op1), r, [out])

        def ttr(out, in0, in1, accum):
            return P.op("vector", lambda e: e.scalar_tensor_tensor(out=out, in0=in0, scalar=1.0, in1=in1, op0=ALU.mult, op1=ALU.mult, accum_out=accum),
                        [in0, in1], [out, accum])

        def cp(eng, out, in_):
            if eng == "scalar":
                return P.op("scalar", lambda e: e.activation(out=out, in_=in_, func=AF.Copy), [in_], [out])
            return P.op(eng, lambda e: e.tensor_copy(out=out, in_=in_), [in_], [out])

        def mset(eng, ap, v):
            return P.op(eng, lambda e: e.memset(ap, v), [], [ap])

        def recip(out, in_):
            return P.op("vector", lambda e: e.reciprocal(out=out, in_=in_), [in_], [out])

        def bankreg(ap):
            name, p0, p1, lo, hi, bs = region(ap)
            return (name, 0, 128, (lo // 2048) * 2048, ((hi + 2047) // 2048) * 2048, bs)

        def mm(out, lhsT, rhs, start, stop, signal=None):
            if signal is None:
                signal = stop
            return P.op("tensor", lambda e: e.matmul(out, lhsT=lhsT, rhs=rhs, start=start, stop=stop), [lhsT, rhs], [bankreg(out)], signal=signal)

        def tr(out, in_, ident, signal=True):
            return P.op("tensor", lambda e: e.transpose(out=out, in_=in_, identity=ident), [in_, ident], [bankreg(out)], signal=signal)

        def dma(eng, out, in_):
            return P.dma(eng, lambda e: e.dma_start(out=out, in_=in_), [in_], [out])

        def rms_rstd(ssq_ap, rstd_ap):
            tsc("vector", rstd_ap, ssq_ap, 1.0 / D, 1e-5, ALU.mult, ALU.add)
            act(rstd_ap, rstd_ap, AF.Sqrt)
            recip(rstd_ap, rstd_ap)

        identi = A.alloc([128, 128], I32)
        ident_f = A.alloc([128, 128], F32)
        ident_b = A.alloc([128, 128], BF16)
        ones_f = A.alloc([128, 128], F32)
        ones_b = A.alloc([128, 128], BF16)
        ustrict = A.alloc([128, 128], BF16)
        modcol = A.alloc([128, 48], F32)
        a1col = A.alloc([128, KD], F32)
        modrow = A.alloc([128, 4, D], F32)
        a2row = A.alloc([128, D], F32)
        gates = A.alloc([128, NQB, 4], F32)
        dests = A.alloc([128, NQB, 4], I32)
        cnt_i = A.alloc([128, NE], I32)
        fsb = {en: A.alloc([128, 2], F32) for en in ("scalar", "vector", "gpsimd")}
        A_BASE = A.mark()
        qT = A.alloc([128, 4, OWN], BF16)

        P.op("gpsimd", lambda e: e.iota(identi, pattern=[[1, 128]], base=0, channel_multiplier=-1), [], [identi])
        tsc("vector", ident_f, identi, 0, None, ALU.is_equal)
        tsc("vector", ident_b, identi, 0, None, ALU.is_equal)
        tsc("vector", ustrict, identi, 0, None, ALU.is_gt)
        mset("vector", ones_f, 1.0)
        mset("vector", ones_b, 1.0)
        for en in ("scalar", "vector", "gpsimd"):
            mset(en if en != "scalar" else "vector", fsb[en], 0.0)
        fps = bank(7)[:, 510:511]
        P.fake = {
            "tensor": lambda e: e.matmul(fps, lhsT=ones_b, rhs=ones_b[:, 0:1], start=True, stop=True),
            "scalar": lambda e: e.activation(out=fsb["scalar"][:, 0:1], in_=fsb["scalar"][:, 1:2], func=AF.Copy),
            "vector": lambda e: e.engine_nop(),
            "gpsimd": lambda e: e.engine_nop(),
        }
        P.cnt_ap = lambda ei: cnt_i[0:1, ei:ei + 1]
        P.noguard = opts.get("d", 0)
        if opts:
            mset("gpsimd", qT, 0.0)
            mset("gpsimd", gates, 0.0)
            mset("gpsimd", dests, 0)

        m0 = A.mark()
        cv = A.alloc([128, KD], F32)
        scv = A.alloc([128, KD], F32)
        ydiag = A.alloc([128, D], F32)
        bcol = A.alloc([128, 48], F32)
        n1g = A.alloc([128, KD], F32)
        n2g = A.alloc([128, D], F32)
        wab = [A.alloc([128, KD, 512], F32) for _ in range(2)]
        dma("sync", cv, cvec)
        dma("sync", bcol, b_ada_col)
        dma("sync", n1g, n1g_col)
        dma("sync", n2g, n2g_rep)
        act(scv, cv, AF.Silu)
        pmod = bank(0)[:, 0:48]
        wa_v = w_ada.rearrange("(k p) n -> p k n", p=128)
        for gi in range(12):
            buf = wab[gi % 2]
            dma("sync" if gi % 2 == 0 else "scalar", buf, wa_v[:, :, gi * 512:(gi + 1) * 512])
            for oc in range(4):
                col = gi * 4 + oc
                for k in range(KD):
                    mm(pmod[:, col:col + 1], buf[:, k, oc * 128:(oc + 1) * 128], scv[:, k:k + 1], k == 0, k == KD - 1)
        tt("vector", modcol, pmod, bcol, ALU.add)
        stt(a1col, modcol[:, 8:16], 1.0, n1g, ALU.add, ALU.mult)
        sh1col = modcol[:, 0:8]
        for r_, mi in enumerate((2, 3, 4, 5)):
            for k in range(KD):
                tsc("vector", ydiag[:, k * 128:(k + 1) * 128], ident_f, modcol[:, mi * 8 + k:mi * 8 + k + 1], None, ALU.mult)
            for hf in range(2):
                pr = bank(1 + hf)
                mm(pr, ones_f, ydiag[:, hf * 512:(hf + 1) * 512], True, True)
                cp("vector", modrow[:, r_, hf * 512:(hf + 1) * 512], pr)
        stt(a2row, modrow[:, 2, :], 1.0, n2g, ALU.add, ALU.mult)
        if debug:
            dma("sync", dbg_out("modcol", [128, 48]), modcol)
            dma("sync", dbg_out("modrow", [128, 4 * D]), modrow.rearrange("p a b -> p (a b)"))
        A.reset(m0)
        if stop_after <= 0:
            P.emit()
            return nc, dbg

        m1 = A.mark()
        w_own = A.alloc([128, KD, 3584], BF16)
        O_GC, O_GA = 1536, 2560
        wco = A.alloc([128, 4, D], BF16)
        cw_sb = A.alloc([128, 4 * 31], F32)
        cvs = A.alloc([128, 12], F32)
        diag = A.alloc([128, 4 * 31, 128], BF16)
        hv = A.alloc([128, NCH * HALO], F32)
        xo = [A.alloc([128, 3, D], F32) for _ in range(2)]
        xsb = A.alloc([128, 3, D], BF16)
        junk = A.alloc([128, D], BF16)
        ssq = A.alloc([128, 4], F32)
        rstd = A.alloc([128, 4], F32)
        hT = A.alloc([128, KD, CW], BF16)
        sg = A.alloc([128, CW], F32)
        a_bf = A.alloc([128, 4, CW], BF16)
        y_f = A.alloc([128, 4, 256], F32)
        y_b = A.alloc([128, 4, 256], BF16)
        ysq = A.alloc([128, 4, 256], BF16)
        mean_s = A.alloc([128, 256], F32)
        var_s = A.alloc([128, 256], F32)
        yn = A.alloc([128, 256], F32)
        actv = A.alloc([128, 4, 256], BF16)
        sgate = A.alloc([128, 256], F32)
        cg_s = A.alloc([128, KD, 256], BF16)
        sga_s = A.alloc([128, KD, 256], BF16)
        win_v = w_in.rearrange("(k p) n -> p k n", p=128)
        dma("gpsimd", w_own[:, :, 0:1536], win_v[:, :, 0:1536])
        dma("gpsimd", w_own[:, :, 1536:2560], win_v[:, :, C_GC:C_GC + 1024])
        dma("gpsimd", w_own[:, :, 2560:3584], win_v[:, :, C_GA:C_GA + 1024])
        dma("gpsimd", wco, w_co.rearrange("(k p) n -> p k n", p=128))
        dma("sync", cw_sb, cwT)
        dma("sync", cvs, cvecs)
        dma("sync", hv, halov)
        mset("vector", ssq, 1.0)
        for i in range(4 * 31):
            tsc("gpsimd", diag[:, i, :], ident_f, cw_sb[:, i:i + 1], None, ALU.mult)
        convb, lng, lnb = cvs[:, 0:4], cvs[:, 4:8], cvs[:, 8:12]
        pT = [bank(b).bitcast(BF16).rearrange("p (k t) -> p k t", k=KD) for b in range(3)]
        for lc in range(opts.get("c", NCH)):
            xb = xo[lc % 2]
            dma("sync", xb[:, 0:2, :], xown[lc, HALO:CW, :].rearrange("(b p) d -> p b d", p=128))
            dma("sync", xb[0:HALO, 2, :], xown[lc, 0:HALO, :])
            for b in range(3):
                np_ = 128 if b < 2 else HALO
                act(junk[0:np_, :], xb[0:np_, b, :], AF.Square, accum=ssq[0:np_, b:b + 1])
            rms_rstd(ssq, rstd)
            for b in range(3):
                np_ = 128 if b < 2 else HALO
                tsc("vector", xsb[0:np_, b, :], xb[0:np_, b, :], rstd[0:np_, b:b + 1], None, ALU.mult)
            for b in range(3):
                np_ = 128 if b < 2 else HALO
                for k in range(KD):
                    tr(pT[b][:, k, 0:np_], xsb[0:np_, b, k * 128:(k + 1) * 128], ident_b[0:np_, 0:np_], signal=(k == KD - 1))
            for k in range(KD):
                for b in range(3):
                    np_ = 128 if b < 2 else HALO
                    c0 = HALO + b * 128 if b < 2 else 0
                    act(hT[:, k, c0:c0 + np_], pT[b][:, k, 0:np_], AF.Identity, scale=a1col[:, k:k + 1], bias=sh1col[:, k:k + 1])
            for cc in range(4):
                puv, pug = bank(3)[:, 0:CW], bank(4)[:, 0:CW]
                for k in range(KD):
                    mm(puv, w_own[:, k, C_UV + cc * 128:C_UV + (cc + 1) * 128], hT[:, k, :], k == 0, k == KD - 1)
                for k in range(KD):
                    mm(pug, w_own[:, k, C_UG + cc * 128:C_UG + (cc + 1) * 128], hT[:, k, :], k == 0, k == KD - 1)
                act(sg, pug, AF.Sigmoid)
                tt("vector", a_bf[:, cc, :], puv, sg, ALU.mult)
                tt("gpsimd", a_bf[:, cc, 0:HALO], a_bf[:, cc, 0:HALO], hv[:, lc * HALO:(lc + 1) * HALO], ALU.mult)
                pconv = bank(5)[:, 0:256]
                for kk in range(31):
                    mm(pconv, diag[:, cc * 31 + kk, :], a_bf[:, cc, 2 + kk:2 + kk + 256], kk == 0, kk == 30)
                act(y_f[:, cc, :], pconv, AF.Identity, bias=convb[:, cc:cc + 1])
                cp("vector", y_b[:, cc, :], y_f[:, cc, :])
                tt("gpsimd", ysq[:, cc, :], y_f[:, cc, :], y_f[:, cc, :], ALU.mult)
            pmean, pmsq = bank(6)[:, 0:256], bank(7)[:, 0:256]
            for cc in range(4):
                mm(pmean, ones_b, y_b[:, cc, :], cc == 0, cc == 3)
            for cc in range(4):
                mm(pmsq, ones_b, ysq[:, cc, :], cc == 0, cc == 3)
            act(mean_s, pmean, AF.Copy, scale=1.0 / 512)
            tt("vector", var_s, mean_s, mean_s, ALU.mult)
            stt(var_s, pmsq, 1.0 / 512, var_s, ALU.mult, ALU.subtract)
            tsc("vector", var_s, var_s, 1e-5, None, ALU.add)
            act(var_s, var_s, AF.Sqrt)
            recip(var_s, var_s)
            for cc in range(4):
                tt("vector", yn, y_f[:, cc, :], mean_s, ALU.subtract)
                tt("vector", yn, yn, var_s, ALU.mult)
                act(actv[:, cc, :], yn, AF.Silu, scale=lng[:, cc:cc + 1], bias=lnb[:, cc:cc + 1])
            for dmc in range(KD):
                pco, pgc = bank(3)[:, 0:256], bank(4)[:, 0:256]
                for cc in range(4):
                    mm(pco, wco[:, cc, dmc * 128:(dmc + 1) * 128], actv[:, cc, :], cc == 0, cc == 3)
                for k in range(KD):
                    mm(pgc, w_own[:, k, O_GC + dmc * 128:O_GC + (dmc + 1) * 128], hT[:, k, HALO:CW], k == 0, k == KD - 1)
                act(sgate, pgc, AF.Sigmoid)
                tt("vector", cg_s[:, dmc, :], pco, sgate, ALU.mult)
            dma("scalar", cg_d[lc], cg_s.rearrange("p a b -> p (a b)"))
            for dmc in range(KD):
                pga = bank(5 + dmc % 2)[:, 0:256]
                for k in range(KD):
                    mm(pga, w_own[:, k, O_GA + dmc * 128:O_GA + (dmc + 1) * 128], hT[:, k, HALO:CW], k == 0, k == KD - 1)
                act(sga_s[:, dmc, :], pga, AF.Sigmoid)
            dma("scalar", sga_d[lc], sga_s.rearrange("p a b -> p (a b)"))
            for hp in range(4):
                pq = bank(3 + hp % 2)[:, 0:256]
                for k in range(KD):
                    mm(pq, w_own[:, k, C_Q + hp * 128:C_Q + (hp + 1) * 128], hT[:, k, HALO:CW], k == 0, k == KD - 1)
                cp("vector", qT[:, hp, lc * 256:(lc + 1) * 256], pq)
        if debug:
            dma("sync", dbg_out("qT", [128, 4 * OWN], BF16), qT.rearrange("p a b -> p (a b)"))
        A.reset(m1)
        if stop_after <= 1:
            P.emit()
            return nc, dbg

        attn = A.alloc([128, NQB, 512], BF16)
        KT = A.alloc([128, 2, S], BF16)
        V = A.alloc([128, NKB, 4, 65], BF16)
        Fneg = A.alloc([128, NKB, 8], F32)
        FrefB = A.alloc([128, NQB, 8], F32)
        m2 = A.mark()
        w_kv = A.alloc([128, KD, 1024], BF16)
        wf = A.alloc([128, KD, 128], BF16)
        refs = A.alloc([128, NKB, NQB], F32)
        fst = [A.alloc([8, 256], F32) for _ in range(2)]
        ssq2 = A.alloc([128, 2], F32)
        rstd2 = A.alloc([128, 2], F32)
        junk2 = A.alloc([128, D], BF16)
        bfc = A.alloc([8, 1], F32)
        GrefT = A.alloc([8, NQB], F32)
        Eexp = A.alloc([8, NQB, 8], F32)
        kst = [A.alloc([128, 2, 256], BF16) for _ in range(2)]
        vst = [A.alloc([128, 2, 4, 65], BF16) for _ in range(2)]
        m_alias = A.mark()
        xf = [A.alloc([128, 2, D], F32) for _ in range(2)]
        xs2 = [A.alloc([128, 2, D], BF16) for _ in range(2)]
        hT2 = [A.alloc([128, KD, 256], BF16) for _ in range(2)]
        if opts:
            mset("gpsimd", KT, 0.0)
            mset("gpsimd", V, 0.0)
            mset("gpsimd", attn, 0.0)
        dma("gpsimd", w_kv, win_v[:, :, C_K:C_K + 1024])
        mset("vector", wf, 0.0)
        dma("gpsimd", wf[:, :, 0:8], win_v[:, :, C_F:C_F + 8])
        dma("sync", refs.rearrange("p a b -> p (a b)"), refsel)
        dma("sync", bfc, bf_col)
        mset("gpsimd", V[:, :, :, 64:65], 1.0)
        for i in range(2):
            mset("gpsimd", vst[i][:, :, :, 64:65], 1.0)
        kvB_K = kvB_d[:, 0:2 * S].rearrange("p (a b) -> p a b", a=2)
        kvB_V = kvB_d[:, 2 * S:2 * S + NKB * 260].rearrange("p (a b) -> p a b", a=NKB)
        pTv = ps_all[:, 0:1024].bitcast(BF16).rearrange("p (b k t) -> p b k t", b=2, k=KD)
        NG = opts.get("g", 32)
        for gi in range(NG):
            i2 = gi % 2
            xb, xsb_, hb = xf[i2], xs2[i2], hT2[i2]
            dma("sync" if i2 == 0 else "scalar", xb, xfull[gi * 256:(gi + 1) * 256, :].rearrange("(b p) d -> p b d", p=128))
            for b in range(2):
                act(junk2, xb[:, b, :], AF.Square, accum=ssq2[:, b:b + 1])
            rms_rstd(ssq2, rstd2)
            for b in range(2):
                tsc("vector", xsb_[:, b, :], xb[:, b, :], rstd2[:, b:b + 1], None, ALU.mult)
            for b in range(2):
                for k in range(KD):
                    tr(pTv[:, b, k, :], xsb_[:, b, k * 128:(k + 1) * 128], ident_b, signal=(b == 1 and k == KD - 1))
            for k in range(KD):
                act(hb[:, k, :].rearrange("p (b t) -> p b t", b=2), pTv[:, :, k, :], AF.Identity, scale=a1col[:, k:k + 1], bias=sh1col[:, k:k + 1])
            NSK = opts.get("n", 0)
            for hp in range(4 if not (NSK & 2) else 0):
                pk = bank(2 + hp % 2)[:, 0:256]
                for k in range(KD):
                    mm(pk, w_kv[:, k, hp * 128:(hp + 1) * 128], hb[:, k, :], k == 0, k == KD - 1)
                if hp < 2:
                    cp("vector", KT[:, hp, gi * 256:(gi + 1) * 256], pk)
                else:
                    cp("vector", kst[i2][:, hp - 2, :], pk)
            if not (NSK & 2):
                dma("sync", kvB_K[:, :, gi * 256:(gi + 1) * 256], kst[i2])
            for b in range(2 if not (NSK & 4) else 0):
                pv = bank(4 + b)
                for k in range(KD):
                    mm(pv, hb[:, k, b * 128:(b + 1) * 128], w_kv[:, k, 512:1024], k == 0, k == KD - 1)
                kb = gi * 2 + b
                if not (NSK & 16):
                    cp("scalar", V[:, kb, :, 0:64], pv[:, 0:256].rearrange("p (h d) -> p h d", h=4))
                if not (NSK & 32):
                    cp("vector", vst[i2][:, b, :, 0:64], pv[:, 256:512].rearrange("p (h d) -> p h d", h=4))
            if not (NSK & 4) and not (NSK & 64):
                dma("sync", kvB_V[:, gi * 2:gi * 2 + 2, :], vst[i2].rearrange("p a b c -> p a (b c)"))
            if NSK & 8:
                continue
            pf = bank(6)[0:8, 0:256]
            pf_full = bank(6)[:, 0:256]
            for k in range(KD):
                mm(pf_full, wf[:, k, :], hb[:, k, :], k == 0, k == KD - 1)
            cp("vector", fst[i2], pf)
            dma("sync", f_d[:, gi * 256:(gi + 1) * 256], fst[i2])
        if opts.get("n", 0) & 1:
            if debug:
                dma("sync", dbg_out("KT", [128, 2 * S], BF16), KT.rearrange("p a b -> p (a b)"))
                dma("sync", dbg_out("V", [128, NKB * 260], BF16), V.rearrange("p a b c -> p (a b c)"))
            P.emit()
            return nc, dbg
        A.reset(m_alias)
        fT = A.alloc([8, S], F32)
        dma("sync", fT, f_d)
        tsc("vector", bfc, bfc, -1.0, None, ALU.mult)
        act(fT, fT, AF.Exp, scale=-1.0, bias=bfc[:, 0:1])
        act(fT, fT, AF.Ln, bias=1.0)
        P.op("vector", lambda e: e.tensor_tensor_scan(out=fT, data0=fT, data1=fT, initial=0.0, op0=ALU.add, op1=ALU.max), [fT], [fT])
        pF = bank(7)
        for kb in range(NKB):
            tr(pF[:, kb * 8:(kb + 1) * 8], fT[0:8, kb * 128:(kb + 1) * 128], ident_f[0:8, 0:8], signal=(kb == NKB - 1))
        cp("vector", Fneg.rearrange("p a b -> p (a b)"), pF)
        pG = bank(6)[0:8, 0:NQB]
        for kb in range(NKB):
            mm(pG, Fneg[:, kb, :], refs[:, kb, :], kb == 0, kb == NKB - 1)
        cp("vector", GrefT, pG)
        tt("vector", Eexp, GrefT.unsqueeze(2).to_broadcast([8, NQB, 8]), ident_f[0:8, 0:8].unsqueeze(1).to_broadcast([8, NQB, 8]), ALU.mult)
        pB = bank(5)[:, 0:128]
        mm(pB, ones_f[0:8, :], Eexp.rearrange("p a b -> p (a b)"), True, True)
        cp("vector", FrefB.rearrange("p a b -> p (a b)"), pB)
        if debug:
            dma("sync", dbg_out("KT", [128, 2 * S], BF16), KT.rearrange("p a b -> p (a b)"))
            dma("sync", dbg_out("V", [128, NKB * 260], BF16), V.rearrange("p a b c -> p (a b c)"))
            dma("sync", dbg_out("G", [128, NKB * 8]), Fneg.rearrange("p a b -> p (a b)"))
            dma("sync", dbg_out("Gref", [128, NQB * 8]), FrefB.rearrange("p a b -> p (a b)"))
        A.reset(m2)
        if stop_after <= 2:
            P.emit()
            return nc, dbg

        qpos = A.alloc([128, OWN], F32)
        kpos = A.alloc([128, NKB], F32)
        biasq = [A.alloc([128, NKB, 4], F32) for _ in range(2)]
        masks = [A.alloc([128, 8, 128], BF16) for _ in range(2)]
        Pt = A.alloc([128, 8, 128], BF16)
        rden = A.alloc([128, 4], F32)
        dma("sync", qpos, qposB)
        dma("sync", kpos, kposc)
        ztile = A.alloc([128, 2, D], BF16)
        ztile_f = A.alloc([128, 2, D], F32)
        mset("gpsimd", ztile, 0.0)
        mset("gpsimd", ztile_f, 0.0)
        nrows = NE * cap + 128
        for r0 in range(0, nrows, 256):
            nr = min(256, nrows - r0)
            dma("sync", xs_d[r0:r0 + nr, :].rearrange("(r p) d -> p r d", p=128), ztile[:, 0:nr // 128, :])
        Sps = ps_all[:, 0:2048].rearrange("p (s t) -> p s t", s=4)[:, :, 0:128]
        NQR = opts.get("q", NQB)
        for half in range(2):
            if half == 1:
                dma("sync", KT, kvB_K)
                dma("scalar", V.rearrange("p a b c -> p a (b c)"), kvB_V)
                for r0 in range(0, nrows, 256):
                    nr = min(256, nrows - r0)
                    dma("sync", ys_d[r0:r0 + nr, :].rearrange("(r p) d -> p r d", p=128), ztile_f[:, 0:nr // 128, :])
            tile_no = 0
            for qb in range(NQR):
                lc = qb // 2
                nkb, ws = kb_end(lc), win_start(lc)
                bq, mk = biasq[qb % 2], masks[qb % 2]
                tt("vector", bq, Fneg[:, :, half * 4:(half + 1) * 4], FrefB[:, qb, half * 4:(half + 1) * 4].unsqueeze(1).to_broadcast([128, NKB, 4]), ALU.subtract)
                tsc("vector", bq, bq, 0.0, None, ALU.min)
                for w in range(8):
                    tsc("gpsimd", mk[:, w, :], qpos[:, qb * 128:(qb + 1) * 128], kpos[:, ws + w:ws + w + 1], None, ALU.is_ge)
                tiles = [(hl, kb) for hl in range(4) for kb in range(nkb)]
                LA = 2

                def emit_S(i, tiles=tiles, qb=qb, half=half, tile_no=tile_no):
                    hl, kb = tiles[i]
                    n = tile_no + i
                    prt = (hl % 2) * 64
                    hpl = hl // 2
                    mm(Sps[:, n % 4, :], KT[prt:prt + 64, hpl, kb * 128:(kb + 1) * 128],
                       qT[prt:prt + 64, half * 2 + hpl, qb * 128:(qb + 1) * 128], True, True)

                for i in range(min(LA, len(tiles))):
                    emit_S(i)
                for i, (hl, kb) in enumerate(tiles):
                    n = tile_no + i
                    if i + LA < len(tiles):
                        emit_S(i + LA)
                    Ob = bank(4 + (hl % 2) + 2 * (qb % 2))
                    act(Pt[:, n % 8, :], Sps[:, n % 4, :], AF.Exp, scale=0.125, bias=bq[:, kb, hl:hl + 1])
                    if kb >= ws:
                        tt("gpsimd", Pt[:, n % 8, :], Pt[:, n % 8, :], mk[:, kb - ws, :], ALU.mult)
                    mm(Ob[:, 0:65], Pt[:, n % 8, :], V[:, kb, hl, :], kb == 0, kb == nkb - 1, signal=True)
                    if kb == nkb - 1:
                        h = half * 4 + hl
                        recip(rden[:, hl:hl + 1], Ob[:, 64:65])
                        tsc("vector", attn[:, qb, h * 64:(h + 1) * 64], Ob[:, 0:64], rden[:, hl:hl + 1], None, ALU.mult)
                tile_no += len(tiles)
        if debug:
            dma("sync", dbg_out("attn", [128, NQB * 512], BF16), attn.rearrange("p a b -> p (a b)"))
        A.reset(m2)
        if stop_after <= 3:
            P.emit()
            return nc, dbg

        wao = A.alloc([128, 4, D], BF16)
        wout = A.alloc([128, KD, D], BF16)
        wr = A.alloc([128, KD, NE], F32)
        brp = A.alloc([128, NE], F32)
        ecp = A.alloc([128, NE], F32)
        maskb = A.alloc([128, NQB, NE], BF16)
        sga_b = [A.alloc([128, KD, 128], BF16) for _ in range(2)]
        cg_b = [A.alloc([128, KD, 128], BF16) for _ in range(2)]
        xblk = [A.alloc([128, D], F32) for _ in range(2)]
        attnT = A.alloc([128, 4, 128], BF16)
        tmpf = A.alloc([128, D], F32)
        mixT = A.alloc([128, KD, 128], BF16)
        x1 = [A.alloc([128, D], F32) for _ in range(2)]
        h2f = A.alloc([128, D], F32)
        h2b = [A.alloc([128, D], BF16) for _ in range(2)]
        h2T = A.alloc([128, KD, 128], F32)
        junk3 = A.alloc([128, D], BF16)
        ssq3 = A.alloc([128, 1], F32)
        rstd3 = A.alloc([128, 1], F32)
        lg = A.alloc([128, NE], F32)
        top8 = A.alloc([128, 8], F32)
        msk = A.alloc([128, NE], F32)
        negm = A.alloc([128, 1], F32)
        ex = A.alloc([128, NE], F32)
        ssum = A.alloc([128, 1], F32)
        g32 = A.alloc([128, NE], F32)
        destf = A.alloc([128, NE], F32)
        oh = A.alloc([128, NE], F32)
        jk32 = A.alloc([128, NE], F32)
        dkf = A.alloc([128, 4], F32)
        dma("gpsimd", wao, w_ao.rearrange("(k p) n -> p k n", p=128))
        dma("gpsimd", wout, w_out.rearrange("(k p) n -> p k n", p=128))
        dma("sync", wr, w_router.rearrange("(k p) n -> p k n", p=128))
        dma("sync", brp, br_rep)
        dma("sync", ecp, ecap)
        for qb in range(NQR):
            lc, off = qb // 2, (qb % 2) * 128
            i2 = qb % 2
            dma("sync", sga_b[i2], sga_d[lc].rearrange("p (a b) -> p a b", a=KD)[:, :, off:off + 128])
            dma("sync", cg_b[i2], cg_d[lc].rearrange("p (a b) -> p a b", a=KD)[:, :, off:off + 128])
            dma("scalar", xblk[i2], xown[lc, HALO + off:HALO + off + 128, :])
            pAT = bank(0).bitcast(BF16)[:, 0:512].rearrange("p (c t) -> p c t", c=4)
            for c4 in range(4):
                tr(pAT[:, c4, :], attn[:, qb, c4 * 128:(c4 + 1) * 128], ident_b, signal=(c4 == 3))
            cp("vector", attnT, pAT)
            pAO = ps_all[:, 512:1536]
            for dmc in range(KD):
                for c4 in range(4):
                    mm(pAO[:, dmc * 128:(dmc + 1) * 128], wao[:, c4, dmc * 128:(dmc + 1) * 128], attnT[:, c4, :], c4 == 0, c4 == 3)
            tt("vector", tmpf, pAO, sga_b[i2].rearrange("p a b -> p (a b)"), ALU.mult)
            tt("gpsimd", mixT.rearrange("p a b -> p (a b)"), tmpf, cg_b[i2].rearrange("p a b -> p (a b)"), ALU.add)
            pD = ps_all[:, 1536:2560]
            for nh in range(2):
                for dk in range(KD):
                    mm(pD[:, nh * 512:(nh + 1) * 512], mixT[:, dk, :], wout[:, dk, nh * 512:(nh + 1) * 512], dk == 0, dk == KD - 1)
            x1b = x1[i2]
            tt("vector", tmpf, pD, modrow[:, 0, :], ALU.mult)
            tt("gpsimd", x1b, tmpf, xblk[i2], ALU.add)
            dma("scalar", x1_d[qb], x1b)
            act(junk3, x1b, AF.Square, accum=ssq3)
            rms_rstd(ssq3, rstd3)
            stt(h2f, x1b, rstd3[:, 0:1], a2row, ALU.mult, ALU.mult)
            tt("vector", h2f, h2f, modrow[:, 1, :], ALU.add)
            cp("scalar", h2b[i2], h2f)
            pHT = ps_all[:, 2560:3584].rearrange("p (k t) -> p k t", k=KD)
            for k in range(KD):
                tr(pHT[:, k, :], h2f[:, k * 128:(k + 1) * 128], ident_f, signal=(k == KD - 1))
            cp("vector", h2T.rearrange("p a b -> p (a b)"), ps_all[:, 2560:3584])
            plog = bank(7)[:, 0:NE]
            for k in range(KD):
                mm(plog, h2T[:, k, :], wr[:, k, :], k == 0, k == KD - 1)
            tt("vector", lg, plog, brp, ALU.add)
            P.op("vector", lambda e: e.max(out=top8, in_=lg), [lg], [top8])
            tsc("vector", msk, lg, top8[:, 3:4], None, ALU.is_ge)
            cp("vector", maskb[:, qb, :], msk)
            tsc("vector", negm, top8[:, 0:1], -1.0, None, ALU.mult)
            act(ex, lg, AF.Exp, bias=negm[:, 0:1])
            ttr(ex, ex, msk, ssum)
            recip(ssum, ssum)
            tsc("vector", g32, ex, ssum[:, 0:1], None, ALU.mult)
            ppos = bank(7)[:, 64:64 + NE]
            for b2 in range(qb + 1):
                mm(ppos, ones_b if b2 < qb else ustrict, maskb[:, b2, :], b2 == 0, b2 == qb)
            tt("vector", destf, ppos, ecp, ALU.add)
            for k in range(4):
                tsc("vector", oh, lg, top8[:, k:k + 1], None, ALU.is_equal)
                ttr(jk32, oh, g32, gates[:, qb, k:k + 1])
                ttr(jk32, oh, destf, dkf[:, k:k + 1])
            cp("vector", dests[:, qb, :], dkf)
            for k in range(4):
                P.dma("gpsimd", lambda e, qb=qb, k=k, i2=i2: e.indirect_dma_start(
                    out=xs_d[:, :], out_offset=bass.IndirectOffsetOnAxis(ap=dests[:, qb, k:k + 1], axis=0),
                    in_=h2b[i2], in_offset=None), [h2b[i2], dests[:, qb, k:k + 1]], [xs_d[:, :]])
        pcnt = bank(7)[:, 128:128 + NE]
        for b2 in range(NQR):
            mm(pcnt, ones_b, maskb[:, b2, :], b2 == 0, b2 == NQR - 1)
        t_cnt = cp("vector", cnt_i, pcnt)
        if debug:
            dma("sync", dbg_out("cnt", [128, NE], I32), cnt_i)
            dma("sync", dbg_out("lg", [128, NE]), lg)
            dma("sync", dbg_out("top8", [128, 8]), top8)
            dma("sync", dbg_out("h2f", [128, D]), h2f)
            dma("sync", dbg_out("gates", [128, NQB * 4]), gates.rearrange("p a b -> p (a b)"))
            dma("sync", dbg_out("dests", [128, NQB * 4], I32), dests.rearrange("p a b -> p (a b)"))
            d_x1 = dbg_out("x1", [NQB, 128, D])
            for qb in range(NQR):
                dma("sync", xblk[0], x1_d[qb])
                dma("sync", d_x1[qb], xblk[0])
        A.reset(A_BASE)
        if stop_after <= 4:
            P.emit()
            return nc, dbg

        W1 = [A.alloc([128, KD, 2 * D], BF16) for _ in range(2)]
        W2 = [A.alloc([128, KD, D], BF16) for _ in range(2)]
        b1s = A.alloc([128, NE * 16], F32)
        b2r = [A.alloc([128, D], F32) for _ in range(2)]
        xbk = [A.alloc([128, D], BF16) for _ in range(2)]
        xT = [A.alloc([128, KD, 128], BF16) for _ in range(2)]
        gm = [A.alloc([128, 512], F32) for _ in range(2)]
        sgm = [A.alloc([128, 512], F32) for _ in range(2)]
        uc = [A.alloc([128, 512], F32) for _ in range(2)]
        hmT = [A.alloc([128, 8, 128], BF16) for _ in range(2)]
        yb = [A.alloc([128, D], F32) for _ in range(2)]
        dma("sync", b1s, b1c)
        NEr = opts.get("e", NE)
        stg = [A.alloc([128, KD, 256], F32) for _ in range(4)]
        stg_i = [0]

        def expert_chunks(ei):
            wb1_, wb2_ = W1[ei % 2], W2[ei % 2]
            w1v = w_e1[ei].rearrange("(k p) n -> p k n", p=128)
            w2v = w_e2[ei].rearrange("(k p) n -> p k n", p=128)
            ch = []
            for c in range(12):
                if c < 8:
                    ch.append((w1v[:, :, c * 256:(c + 1) * 256], wb1_[:, :, c * 256:(c + 1) * 256]))
                else:
                    ch.append((w2v[:, :, (c - 8) * 256:(c - 7) * 256], wb2_[:, :, (c - 8) * 256:(c - 7) * 256]))
            return ch

        def w_dma(ei, c):
            src, dst = expert_chunks(ei)[c]
            dma("sync", stg[(ei * 8 + c) % 4], src)

        def w_cast(ei, c):
            src, dst = expert_chunks(ei)[c]
            cp("scalar", dst, stg[(ei * 8 + c) % 4])

        NCHK = 8

        def w2_dma(ei):
            dma("gpsimd", W2[ei % 2], w_e2[ei].rearrange("(k p) n -> p k n", p=128))

        dma("sync", b2r[0], b_e2[0:1, :].to_broadcast([128, D]))
        w2_dma(0)
        for c in range(4):
            w_dma(0, c)
        for c in range(NCHK):
            w_cast(0, c)
            if c + 4 < NCHK:
                w_dma(0, c + 4)
        for e_ in range(NEr):
            wb1, wb2 = W1[e_ % 2], W2[e_ % 2]
            nxt = e_ + 1 if e_ + 1 < NEr else None
            if nxt is not None:
                dma("sync", b2r[nxt % 2], b_e2[nxt:nxt + 1, :].to_broadcast([128, D]))
                w2_dma(nxt)
                for c in range(4):
                    w_dma(nxt, c)
            P.regload(e_, [t_cnt])
            dma("scalar", xbk[0], xs_d[e_ * cap:e_ * cap + 128, :])

            def st_T(kb_):
                P.guard = (e_, kb_ * 128)
                xk = xbk[kb_ % 2]
                xTk = xT[kb_ % 2]
                pXT = bank(0).bitcast(BF16).rearrange("p (k t) -> p k t", k=KD)
                for k in range(KD):
                    tr(pXT[:, k, :], xk[:, k * 128:(k + 1) * 128], ident_b, signal=(k == KD - 1))
                cp("vector", xTk.rearrange("p a b -> p (a b)"), bank(0).bitcast(BF16))
                P.guard = None

            def st_H(kb_, half2):
                P.guard = (e_, kb_ * 128)
                xTk = xT[kb_ % 2]
                gm_, uc_, sgm_ = gm[half2], uc[half2], sgm[half2]
                pg, pu = bank(1 + half2), bank(3 + half2)
                for j4 in range(4):
                    fc = half2 * 4 + j4
                    for k in range(KD):
                        mm(pg[:, j4 * 128:(j4 + 1) * 128], wb1[:, k, fc * 128:(fc + 1) * 128], xTk[:, k, :], k == 0, k == KD - 1)
                for j4 in range(4):
                    fc = 8 + half2 * 4 + j4
                    for k in range(KD):
                        mm(pu[:, j4 * 128:(j4 + 1) * 128], wb1[:, k, fc * 128:(fc + 1) * 128], xTk[:, k, :], k == 0, k == KD - 1)
                for j4 in range(4):
                    fcg = half2 * 4 + j4
                    tsc("vector", gm_[:, j4 * 128:(j4 + 1) * 128], pg[:, j4 * 128:(j4 + 1) * 128], b1s[:, e_ * 16 + fcg:e_ * 16 + fcg + 1], 7.0, ALU.add, ALU.min)
                    tsc("vector", uc_[:, j4 * 128:(j4 + 1) * 128], pu[:, j4 * 128:(j4 + 1) * 128], b1s[:, e_ * 16 + 8 + fcg:e_ * 16 + 8 + fcg + 1], 7.0, ALU.add, ALU.min)
                act(sgm_, gm_, AF.Silu, scale=1.702)
                tsc("vector", uc_, uc_, -7.0, 1.0, ALU.max, ALU.add)
                stt(hmT[kb_ % 2][:, half2 * 4:(half2 + 1) * 4, :].rearrange("p a b -> p (a b)"), uc_, 1.0 / 1.702, sgm_, ALU.mult, ALU.mult)
                P.guard = None

            def st_Y(kb_):
                P.guard = (e_, kb_ * 128)
                row0 = e_ * cap + kb_ * 128
                pY = ps_all[:, 2560:3584]
                for nh in range(2):
                    for fc in range(8):
                        mm(pY[:, nh * 512:(nh + 1) * 512], hmT[kb_ % 2][:, fc, :], wb2[:, fc, nh * 512:(nh + 1) * 512], fc == 0, fc == 7)
                ybk = yb[kb_ % 2]
                tt("vector", ybk, pY, b2r[e_ % 2], ALU.add)
                dma("scalar", ys_d[row0:row0 + 128, :], ybk)
                P.guard = None

            def st_prefetch(kb_):
                if kb_ < kmax:
                    P.guard = (e_, kb_ * 128)
                    dma("scalar", xbk[kb_ % 2], xs_d[e_ * cap + kb_ * 128:e_ * cap + kb_ * 128 + 128, :])
                    P.guard = None

            st_prefetch(1)
            st_T(0)
            st_H(0, 0)
            for kblk in range(kmax):
                st_H(kblk, 1)
                if kblk + 1 < kmax:
                    st_prefetch(kblk + 2)
                    st_T(kblk + 1)
                    st_H(kblk + 1, 0)
                st_Y(kblk)
                if nxt is not None:
                    for c in (2 * kblk, 2 * kblk + 1):
                        if c < NCHK:
                            w_cast(nxt, c)
                            if c + 4 < NCHK:
                                w_dma(nxt, c + 4)
        A.reset(A_BASE)
        if stop_after <= 5:
            P.emit()
            return nc, dbg

        yk = [A.alloc([128, D], F32) for _ in range(4)]
        acc = A.alloc([128, D], F32)
        x1r = [A.alloc([128, D], F32) for _ in range(2)]
        finr = A.alloc([128, D], F32)
        junk5 = A.alloc([128, D], BF16)
        ssq5 = A.alloc([128, 1], F32)
        rstd5 = A.alloc([128, 1], F32)
        ob = [A.alloc([128, D], F32) for _ in range(2)]
        dma("sync", finr, fin_rep)
        for qb in range(NQR):
            i2 = qb % 2
            dma("sync", x1r[i2], x1_d[qb])
            for k in range(4):
                P.dma("gpsimd", lambda e, qb=qb, k=k: e.indirect_dma_start(
                    out=yk[k], out_offset=None, in_=ys_d[:, :],
                    in_offset=bass.IndirectOffsetOnAxis(ap=dests[:, qb, k:k + 1], axis=0)), [ys_d[:, :], dests[:, qb, k:k + 1]], [yk[k]])
            tsc("vector", acc, yk[0], gates[:, qb, 0:1], None, ALU.mult)
            for k in range(1, 4):
                stt(acc, yk[k], gates[:, qb, k:k + 1], acc, ALU.mult, ALU.add)
            tt("gpsimd", acc, acc, modrow[:, 3, :], ALU.mult)
            tt("gpsimd", acc, acc, x1r[i2], ALU.add)
            act(junk5, acc, AF.Square, accum=ssq5)
            rms_rstd(ssq5, rstd5)
            stt(ob[i2], acc, rstd5[:, 0:1], finr, ALU.mult, ALU.mult)
            dma("sync", out[qb * 128:(qb + 1) * 128, :], ob[i2])
        P.emit()
    return nc, dbg


def host_inputs(inp, kmax=KMAX):
    cap = kmax * 128
    f = lambda a: np.ascontiguousarray(a, dtype=np.float32)
    x = inp["x"]
    col = lambda v: f(v.reshape(-1, 128).T)
    rep = lambda v: f(np.broadcast_to(v.reshape(1, -1), (128, v.size)))
    shared = {
        "w_ada": f(inp["w_ada"][0]),
        "b_ada_col": col(inp["b_ada"][0]),
        "n1g_col": col(inp["norm1_g"][0]),
        "n2g_rep": rep(inp["norm2_g"][0]),
        "fin_rep": rep(inp["final_g"]),
        "w_in": f(inp["w_in"][0]),
        "bf_col": f(inp["b_forget"][0].reshape(8, 1)),
        "cwT": f(inp["conv_w"][0].T.reshape(4, 128, 31).transpose(1, 0, 2).reshape(128, 124)),
        "cvecs": f(np.concatenate([col(inp["conv_b"][0]), col(inp["conv_ln_g"][0]), col(inp["conv_ln_b"][0])], axis=1)),
        "w_co": f(inp["w_conv_out"][0]),
        "w_ao": f(inp["w_attn_out"][0]),
        "w_out": f(inp["w_out"][0]),
        "w_router": f(inp["w_router"][0]),
        "br_rep": rep(inp["b_router"][0]),
        "w_e1": f(inp["w_exp_in"][0]),
        "b1c": f(inp["b_exp_in"][0].reshape(NE, 16, 128).transpose(2, 0, 1).reshape(128, NE * 16)),
        "w_e2": f(inp["w_exp_out"][0]),
        "b_e2": f(inp["b_exp_out"][0]),
        "kposc": f((np.arange(NKB)[None, :] * 128 + np.arange(128)[:, None])),
        "ecap": rep(np.arange(NE, dtype=np.float32) * cap),
    }
    maps = []
    poss = []
    for c in range(NCORES):
        b, j = c // 4, c % 4
        ch = chunks_of(j)
        xo = np.zeros((NCH, CW, D), np.float32)
        hv = np.zeros((128, NCH * HALO), np.float32)
        pos = np.zeros(OWN, np.int64)
        for lc, m in enumerate(ch):
            s0 = m * 256
            xo[lc, HALO:] = x[b, s0:s0 + 256]
            if m > 0:
                xo[lc, :HALO] = x[b, s0 - HALO:s0]
                hv[:, lc * HALO:(lc + 1) * HALO] = 1.0
            pos[lc * 256:(lc + 1) * 256] = np.arange(s0, s0 + 256)
        refs = np.zeros((128, NKB, NQB), np.float32)
        for qb in range(NQB):
            pl = pos[qb * 128 + 127]
            refs[pl % 128, pl // 128, qb] = 1.0
        d = dict(shared)
        d.update({
            "xfull": f(x[b]), "xown": xo, "halov": hv, "cvec": col(inp["c"][b]),
            "qposB": rep(pos.astype(np.float32)), "refsel": refs.reshape(128, NKB * NQB),
        })
        maps.append(d)
        poss.append((b, pos))
    return maps, poss


def kernel(**inp):
    inp = {k: np.asarray(v) for k, v in inp.items()}
    nc, _ = build()
    maps, poss = host_inputs(inp)
    res = run_bass_kernel_spmd(nc, maps, core_ids=list(range(NCORES)))
    out = np.zeros((2, S, D), np.float32)
    for c in range(NCORES):
        b, pos = poss[c]
        out[b, pos] = res.results[c]["out"]
    return out
```

```python
import numpy as np
from contextlib import ExitStack
import concourse.bass as bass
import concourse.mybir as mybir
from concourse.bass_utils import run_bass_kernel_spmd

F32 = mybir.dt.float32
BF16 = mybir.dt.bfloat16
I32 = mybir.dt.int32
U32 = mybir.dt.uint32
AF = mybir.ActivationFunctionType
ALU = mybir.AluOpType

ENGS = ("sync", "scalar", "vector", "gpsimd", "tensor")
NCORES = 8
D = 1024
KD = 8
S = 8192
NKB = 64
OWN = 2048
NCH = 8
NQB = 16
HALO = 32
CW = 256 + HALO
NE = 32
KMAX = 7
C_UV, C_UG, C_Q, C_K, C_V, C_F, C_GC, C_GA = 0, 512, 1024, 1536, 2048, 2560, 2568, 3592
_DT_SIZE = {F32: 4, BF16: 2, I32: 4, U32: 4}
SEM_ROT = 2000
NPOOL = 24
NSW = 12


class Tok:
    __slots__ = ("sem", "val", "gen")

    def __init__(self, sem=None, val=None, gen=0):
        self.sem = sem
        self.val = val
        self.gen = gen


class Rec:
    __slots__ = ("p0", "p1", "lo", "hi", "w", "tok", "eng", "alive", "name")


def region(ap):
    es = _DT_SIZE[ap.dtype]
    pairs = ap.ap
    off = ap.offset
    sp = str(ap.space)
    if sp in ("SB", "PSUM"):
        pstride, npart = pairs[0]
        if pstride <= 0:
            pstride = 1 << 40
        p0 = off // pstride
        col = off % pstride
        ext = 1 + sum((c - 1) * abs(s) for s, c in pairs[1:])
        return (ap.name, p0, p0 + npart, col * es, (col + ext) * es, 4096)
    ext = 1 + sum((c - 1) * abs(s) for s, c in pairs)
    return (ap.name, 0, 1, off * es, (off + ext) * es, 1 << 20)


class Prog:
    def __init__(self, nc, es):
        self.nc = nc
        self.es = es
        self.streams = {e: [] for e in ENGS}
        self.esem = {}
        self.ecount = {}
        self.nsem = 0
        self.pending = {e: [] for e in ENGS}
        self.bins = {}
        self.pool = []
        self.pool_last = []
        self.pool_i = 0
        self.dma_uid = 0
        self.noguard = 0
        self.guard = None
        self.fake = {}
        self.cnt_ap = None
        for e in ENGS:
            self._new_esem(e)
        self.qpool = {q: [self._mksem(f"dq_{q}{i}") for i in range(n)] for q, n in (("sync", 16), ("scalar", 12))}
        self.qlast = {q: [None] * len(v) for q, v in self.qpool.items()}
        self.qi = {q: 0 for q in self.qpool}
        self.swpool = [self._mksem(f"sq{i}") for i in range(NSW)]
        self.sw_last = [None] * NSW
        self.sw_i = 0

    def _mksem(self, name):
        self.nsem += 1
        return self.es.enter_context(self.nc.semaphore(name))

    def _new_esem(self, e):
        self.esem[e] = self._mksem(f"s_{e}_{self.nsem}")
        self.ecount[e] = 0

    def _bins(self, reg):
        name, p0, p1, lo, hi, bs = reg
        return [(name, b) for b in range(lo // bs, (hi - 1) // bs + 1)]

    def _query(self, reg):
        name, p0, p1, lo, hi, bs = reg
        seen = set()
        out = []
        for key in self._bins(reg):
            lst = self.bins.get(key)
            if not lst:
                continue
            dead = 0
            for r in lst:
                if not r.alive:
                    dead += 1
                    continue
                if id(r) in seen:
                    continue
                if r.lo < hi and lo < r.hi and r.p0 < p1 and p0 < r.p1:
                    seen.add(id(r))
                    out.append(r)
            if dead > 16:
                self.bins[key] = [r for r in lst if r.alive]
        return out

    def _add(self, reg, w, tok, eng):
        name, p0, p1, lo, hi, bs = reg
        r = Rec()
        r.p0, r.p1, r.lo, r.hi, r.w, r.tok, r.eng, r.alive, r.name = p0, p1, lo, hi, w, tok, eng, True, name
        for key in self._bins(reg):
            self.bins.setdefault(key, []).append(r)

    def _deps(self, eng, reads, writes, tok, engkey):
        deps = []
        rregs = [a if isinstance(a, tuple) else region(a) for a in reads]
        wregs = [a if isinstance(a, tuple) else region(a) for a in writes]
        for reg in rregs:
            for r in self._query(reg):
                if r.w and not (eng == "tensor" and r.eng == "tensor"):
                    deps.append(r.tok)
            if reg[0] == "parena":
                name, p0, p1, lo, hi, bs = reg
                breg = (name, 0, 128, (lo // 2048) * 2048, ((hi + 2047) // 2048) * 2048, bs)
                for r in self._query(breg):
                    if (not r.w) and r.eng != engkey and r.eng != "tensor":
                        deps.append(r.tok)
        for reg in wregs:
            for r in self._query(reg):
                if not (eng == "tensor" and r.eng == "tensor"):
                    deps.append(r.tok)
        for reg in rregs:
            name, p0, p1, lo, hi, bs = reg
            rep = False
            for r in self._query(reg):
                if (not r.w) and r.eng == engkey and r.p0 == p0 and r.p1 == p1 and r.lo == lo and r.hi == hi:
                    r.tok = tok
                    rep = True
                    break
            if not rep:
                self._add(reg, False, tok, engkey)
        for reg in wregs:
            name, p0, p1, lo, hi, bs = reg
            for r in self._query(reg):
                if r.p0 >= p0 and r.p1 <= p1 and r.lo >= lo and r.hi <= hi:
                    r.alive = False
            self._add(reg, True, tok, engkey)
        return deps

    def op(self, eng, fn, r=(), w=(), signal=True):
        if signal:
            if self.ecount[eng] >= SEM_ROT:
                self._new_esem(eng)
            self.ecount[eng] += 1
            tok = Tok(self.esem[eng], self.ecount[eng])
            for p in self.pending[eng]:
                p.sem, p.val = tok.sem, tok.val
            self.pending[eng] = []
        else:
            tok = Tok()
            self.pending[eng].append(tok)
        deps = self._deps(eng, r, w, tok, eng)
        self.streams[eng].append((fn, deps, tok if signal else None, 1, self.guard))
        return tok

    def regload(self, key, deps):
        for e in ENGS:
            self.streams[e].append((("regload", key), list(deps), None, 0, None))

    def dma(self, eng, fn, r=(), w=()):
        if eng == "gpsimd":
            i = self.sw_i
            self.sw_i = (self.sw_i + 1) % NSW
            prev = self.sw_last[i]
            val = (prev.val if prev is not None else 0) + 16
            tok = Tok(self.swpool[i], val)
            self.sw_last[i] = tok
            self.dma_uid += 1
            deps = self._deps(eng, r, w, tok, "dma%d" % self.dma_uid)
            if prev is not None:
                deps.append(prev)
            self.streams[eng].append((fn, deps, tok, 16, self.guard))
            return tok
        pool, last = self.qpool[eng], self.qlast[eng]
        i = self.qi[eng]
        self.qi[eng] = (i + 1) % len(pool)
        prev = last[i]
        val = (prev.val if prev is not None else 0) + 16
        tok = Tok(pool[i], val)
        last[i] = tok
        self.dma_uid += 1
        deps = self._deps(eng, r, w, tok, "dma%d" % self.dma_uid)
        if prev is not None:
            deps.append(prev)
        self.streams[eng].append((fn, deps, tok, 16, self.guard))
        return tok

    def emit(self):
        nc = self.nc
        final = [t for q in self.qlast for t in self.qlast[q] if t is not None] + [t for t in self.sw_last if t is not None]
        with nc.Block() as block:
            for e in ENGS:
                stream = self.streams[e]
                fw = final if e == "sync" else []

                def body(engine, stream=stream, fw=fw, ename=e):
                    waited = {}
                    lastfake = [None]
                    reg = engine.alloc_register("gcnt")

                    def dowait(d):
                        assert d.sem is not None, "unresolved token"
                        k = (id(d.sem), d.gen)
                        if waited.get(k, 0) >= d.val:
                            return
                        waited[k] = d.val
                        engine.wait_ge(d.sem, d.val)

                    def emit_one(ent):
                        fn, deps, tok, inc, g = ent
                        for d in deps:
                            dowait(d)
                        if isinstance(fn, tuple):
                            if self.noguard != 2:
                                engine.reg_load(reg, self.cnt_ap(fn[1]))
                            return
                        ins = fn(engine)
                        if tok is not None:
                            ins.then_inc(tok.sem, inc)

                    i = 0
                    n = len(stream)
                    while i < n:
                        g = stream[i][4]
                        if g is None or self.noguard:
                            emit_one(stream[i])
                            i += 1
                            continue
                        j = i
                        while j < n and stream[j][4] == g:
                            j += 1
                        grp = stream[i:j]
                        saved = dict(waited)
                        with engine.If_lt(reg, g[1] + 1):
                            for fn, deps, tok, inc, _ in grp:
                                for d in deps:
                                    dowait(d)
                                if tok is None:
                                    continue
                                if inc == 16:
                                    engine.sem_inc(tok.sem, 16)
                                else:
                                    if ename == "scalar" and lastfake[0] is not None:
                                        engine.wait_ge(lastfake[0].sem, lastfake[0].val)
                                    self.fake[ename](engine).then_inc(tok.sem, 1)
                                    lastfake[0] = tok
                        waited.clear()
                        waited.update(saved)
                        with engine.Else():
                            for ent in grp:
                                emit_one(ent)
                        waited.clear()
                        waited.update(saved)
                        i = j
                    for d in fw:
                        dowait(d)

                getattr(block, e)(body)


class Arena:
    def __init__(self, ap, nwords):
        self.ap = ap
        self.n = nwords
        self.off = 0

    def mark(self):
        return self.off

    def reset(self, off=0):
        self.off = off

    def alloc(self, shape, dt):
        free = int(np.prod(shape[1:]))
        words = (free * _DT_SIZE[dt] + 3) // 4
        words += words % 2
        assert self.off + words <= self.n, ("arena overflow", self.off, words, self.n)
        v = self.ap[0:shape[0], self.off:self.off + words]
        self.off += words
        if dt != F32:
            v = v.bitcast(dt)
        v = v[:, 0:free]
        if len(shape) > 2:
            names = [f"a{i}" for i in range(len(shape) - 1)]
            pat = "p (" + " ".join(names) + ") -> p " + " ".join(names)
            v = v.rearrange(pat, **{n: s for n, s in zip(names[:-1], shape[1:-1])})
        return v


def chunks_of(j):
    ch = []
    for g in range(4):
        ch += [8 * g + j, 8 * g + 7 - j]
    return ch


def kb_end(lc):
    g, s = lc // 2, lc % 2
    return 16 * g + 8 * s + 8


def win_start(lc):
    g, s = lc // 2, lc % 2
    return 16 * g + 8 * s


def isap(x):
    return hasattr(x, "ap") and hasattr(x, "dtype") and hasattr(x, "offset")


def build(debug=False, kmax=KMAX, stop_after=99, variant=''):
    cap = kmax * 128
    opts = {}
    for vv in variant.split("_"):
        if len(vv) > 1 and vv[1:].isdigit():
            opts[vv[0]] = int(vv[1:])
    nc = bass.Bass("TRN2", target_bir_lowering=False)
    dram_in = lambda n, shp, dt=F32: nc.dram_tensor(n, list(shp), dt, kind="ExternalInput").ap()
    xfull = dram_in("xfull", [S, D])
    xown = dram_in("xown", [NCH, CW, D])
    halov = dram_in("halov", [128, NCH * HALO])
    cvec = dram_in("cvec", [128, KD])
    w_ada = dram_in("w_ada", [D, 6 * D])
    b_ada_col = dram_in("b_ada_col", [128, 48])
    n1g_col = dram_in("n1g_col", [128, KD])
    n2g_rep = dram_in("n2g_rep", [128, D])
    fin_rep = dram_in("fin_rep", [128, D])
    w_in = dram_in("w_in", [D, 4616])
    bf_col = dram_in("bf_col", [8, 1])
    cwT = dram_in("cwT", [128, 4 * 31])
    cvecs = dram_in("cvecs", [128, 12])
    w_co = dram_in("w_co", [512, D])
    w_ao = dram_in("w_ao", [512, D])
    w_out = dram_in("w_out", [D, D])
    w_router = dram_in("w_router", [D, NE])
    br_rep = dram_in("br_rep", [128, NE])
    qposB = dram_in("qposB", [128, OWN])
    refsel = dram_in("refsel", [128, NKB * NQB])
    kposc = dram_in("kposc", [128, NKB])
    ecap = dram_in("ecap", [128, NE])
    if stop_after >= 5:
        w_e1 = dram_in("w_e1", [NE, D, 2 * D])
        b1c = dram_in("b1c", [128, NE * 16])
        w_e2 = dram_in("w_e2", [NE, D, D])
        b_e2 = dram_in("b_e2", [NE, D])
    out = nc.dram_tensor("out", [OWN, D], F32, kind="ExternalOutput").ap()
    dbg = {}

    def dbg_out(name, shp, dt=F32):
        dbg[name] = nc.dram_tensor("dbg_" + name, list(shp), dt, kind="ExternalOutput").ap()
        return dbg[name]

    scr = lambda n, shp, dt: nc.dram_tensor(n, list(shp), dt, kind="Internal").ap()
    cg_d = scr("cg_d", [NCH, 128, KD * 256], BF16)
    sga_d = scr("sga_d", [NCH, 128, KD * 256], BF16)
    kvB_d = scr("kvB_d", [128, 2 * S + NKB * 4 * 65], BF16)
    x1_d = scr("x1_d", [NQB, 128, D], F32)
    xs_d = scr("xs_d", [NE * cap + 128, D], BF16)
    ys_d = scr("ys_d", [NE * cap + 128, D], F32)
    f_d = scr("f_d", [8, S], F32)

    with ExitStack() as es:
        SBW = 51 * 1024
        sb_all = es.enter_context(nc.sbuf_tensor("arena", [128, SBW], F32))
        ps_all = es.enter_context(nc.psum_tensor("parena", [128, 4096], F32))
        A = Arena(sb_all, SBW)
        P = Prog(nc, es)

        def bank(i, n=1):
            return ps_all[:, i * 512:(i + n) * 512]

        def act(out, in_, func, scale=None, bias=None, accum=None):
            r = [in_] + [x for x in (scale, bias) if isap(x)]
            w = [out] + ([accum] if accum is not None else [])
            kw = {}
            if scale is not None:
                kw["scale"] = scale
            if bias is not None:
                kw["bias"] = bias
            if accum is not None:
                kw["accum_out"] = accum
            return P.op("scalar", lambda e: e.activation(out=out, in_=in_, func=func, **kw), r, w)

        def tsc(eng, out, in0, s1, s2=None, op0=ALU.mult, op1=None):
            r = [in0] + [x for x in (s1, s2) if isap(x)]
            kw = {} if op1 is None else {"op1": op1}
            return P.op(eng, lambda e: e.tensor_scalar(out=out, in0=in0, scalar1=s1, scalar2=s2, op0=op0, **kw), r, [out])

        def tt(eng, out, in0, in1, op):
            return P.op(eng, lambda e: e.tensor_tensor(out=out, in0=in0, in1=in1, op=op), [in0, in1], [out])

        def stt(out, in0, scalar, in1, op0, op1):
            r = [in0, in1] + ([scalar] if isap(scalar) else [])
            return P.op("vector", lambda e: e.scalar_tensor_tensor(out=out, in0=in0, scalar=scalar, in1=in1, op0=op0, op1=op1), r, [out])

        def ttr(out, in0, in1, accum):
            return P.op("vector", lambda e: e.scalar_tensor_tensor(out=out, in0=in0, scalar=1.0, in1=in1, op0=ALU.mult, op1=ALU.mult, accum_out=accum),
                        [in0, in1], [out, accum])

        def cp(eng, out, in_):
            if eng == "scalar":
                return P.op("scalar", lambda e: e.activation(out=out, in_=in_, func=AF.Copy), [in_], [out])
            return P.op(eng, lambda e: e.tensor_copy(out=out, in_=in_), [in_], [out])

        def mset(eng, ap, v):
            return P.op(eng, lambda e: e.memset(ap, v), [], [ap])

        def recip(out, in_):
            return P.op("vector", lambda e: e.reciprocal(out=out, in_=in_), [in_], [out])

        def bankreg(ap):
            name, p0, p1, lo, hi, bs = region(ap)
            return (name, 0, 128, (lo // 2048) * 2048, ((hi + 2047) // 2048) * 2048, bs)

        def mm(out, lhsT, rhs, start, stop, signal=None):
            if signal is None:
                signal = stop
            return P.op("tensor", lambda e: e.matmul(out, lhsT=lhsT, rhs=rhs, start=start, stop=stop), [lhsT, rhs], [bankreg(out)], signal=signal)

        def tr(out, in_, ident, signal=True):
            return P.op("tensor", lambda e: e.transpose(out=out, in_=in_, identity=ident), [in_, ident], [bankreg(out)], signal=signal)

        def dma(eng, out, in_):
            return P.dma(eng, lambda e: e.dma_start(out=out, in_=in_), [in_], [out])

        def rms_rstd(ssq_ap, rstd_ap):
            tsc("vector", rstd_ap, ssq_ap, 1.0 / D, 1e-5, ALU.mult, ALU.add)
            act(rstd_ap, rstd_ap, AF.Sqrt)
            recip(rstd_ap, rstd_ap)

        identi = A.alloc([128, 128], I32)
        ident_f = A.alloc([128, 128], F32)
        ident_b = A.alloc([128, 128], BF16)
        ones_f = A.alloc([128, 128], F32)
        ones_b = A.alloc([128, 128], BF16)
        ustrict = A.alloc([128, 128], BF16)
        modcol = A.alloc([128, 48], F32)
        a1col = A.alloc([128, KD], F32)
        modrow = A.alloc([128, 4, D], F32)
        a2row = A.alloc([128, D], F32)
        gates = A.alloc([128, NQB, 4], F32)
        dests = A.alloc([128, NQB, 4], I32)
        cnt_i = A.alloc([128, NE], I32)
        fsb = {en: A.alloc([128, 2], F32) for en in ("scalar", "vector", "gpsimd")}
        A_BASE = A.mark()
        qT = A.alloc([128, 4, OWN], BF16)

        P.op("gpsimd", lambda e: e.iota(identi, pattern=[[1, 128]], base=0, channel_multiplier=-1), [], [identi])
        tsc("vector", ident_f, identi, 0, None, ALU.is_equal)
        tsc("vector", ident_b, identi, 0, None, ALU.is_equal)
        tsc("vector", ustrict, identi, 0, None, ALU.is_gt)
        mset("vector", ones_f, 1.0)
        mset("vector", ones_b, 1.0)
        for en in ("scalar", "vector", "gpsimd"):
            mset(en if en != "scalar" else "vector", fsb[en], 0.0)
        fps = bank(7)[:, 510:511]
        P.fake = {
            "tensor": lambda e: e.matmul(fps, lhsT=ones_b, rhs=ones_b[:, 0:1], start=True, stop=True),
            "scalar": lambda e: e.drain(),
            "vector": lambda e: e.engine_nop(),
            "gpsimd": lambda e: e.engine_nop(),
        }
        P.cnt_ap = lambda ei: cnt_i[0:1, ei:ei + 1]
        P.noguard = opts.get("d", 0)
        if opts:
            mset("gpsimd", qT, 0.0)
            mset("gpsimd", gates, 0.0)
            mset("gpsimd", dests, 0)

        m0 = A.mark()
        cv = A.alloc([128, KD], F32)
        scv = A.alloc([128, KD], F32)
        ydiag = A.alloc([128, D], F32)
        bcol = A.alloc([128, 48], F32)
        n1g = A.alloc([128, KD], F32)
        n2g = A.alloc([128, D], F32)
        wab = [A.alloc([128, KD, 512], F32) for _ in range(2)]
        dma("sync", cv, cvec)
        dma("sync", bcol, b_ada_col)
        dma("sync", n1g, n1g_col)
        dma("sync", n2g, n2g_rep)
        act(scv, cv, AF.Silu)
        pmod = bank(0)[:, 0:48]
        wa_v = w_ada.rearrange("(k p) n -> p k n", p=128)
        for gi in range(12):
            buf = wab[gi % 2]
            dma("sync" if gi % 2 == 0 else "scalar", buf, wa_v[:, :, gi * 512:(gi + 1) * 512])
            for oc in range(4):
                col = gi * 4 + oc
                for k in range(KD):
                    mm(pmod[:, col:col + 1], buf[:, k, oc * 128:(oc + 1) * 128], scv[:, k:k + 1], k == 0, k == KD - 1)
        tt("vector", modcol, pmod, bcol, ALU.add)
        stt(a1col, modcol[:, 8:16], 1.0, n1g, ALU.add, ALU.mult)
        sh1col = modcol[:, 0:8]
        for r_, mi in enumerate((2, 3, 4, 5)):
            for k in range(KD):
                tsc("vector", ydiag[:, k * 128:(k + 1) * 128], ident_f, modcol[:, mi * 8 + k:mi * 8 + k + 1], None, ALU.mult)
            for hf in range(2):
                pr = bank(1 + hf)
                mm(pr, ones_f, ydiag[:, hf * 512:(hf + 1) * 512], True, True)
                cp("vector", modrow[:, r_, hf * 512:(hf + 1) * 512], pr)
        stt(a2row, modrow[:, 2, :], 1.0, n2g, ALU.add, ALU.mult)
        if debug:
            dma("sync", dbg_out("modcol", [128, 48]), modcol)
            dma("sync", dbg_out("modrow", [128, 4 * D]), modrow.rearrange("p a b -> p (a b)"))
        A.reset(m0)
        if stop_after <= 0:
            P.emit()
            return nc, dbg

        m1 = A.mark()
        w_own = A.alloc([128, KD, 3584], BF16)
        O_GC, O_GA = 1536, 2560
        wco = A.alloc([128, 4, D], BF16)
        cw_sb = A.alloc([128, 4 * 31], F32)
        cvs = A.alloc([128, 12], F32)
        diag = A.alloc([128, 4 * 31, 128], BF16)
        hv = A.alloc([128, NCH * HALO], F32)
        xo = [A.alloc([128, 3, D], F32) for _ in range(2)]
        xsb = A.alloc([128, 3, D], BF16)
        junk = A.alloc([128, D], BF16)
        ssq = A.alloc([128, 4], F32)
        rstd = A.alloc([128, 4], F32)
        hT = A.alloc([128, KD, CW], BF16)
        sg = A.alloc([128, CW], F32)
        a_bf = A.alloc([128, 4, CW], BF16)
        y_f = A.alloc([128, 4, 256], F32)
        y_b = A.alloc([128, 4, 256], BF16)
        ysq = A.alloc([128, 4, 256], BF16)
        mean_s = A.alloc([128, 256], F32)
        var_s = A.alloc([128, 256], F32)
        yn = A.alloc([128, 256], F32)
        actv = A.alloc([128, 4, 256], BF16)
        sgate = A.alloc([128, 256], F32)
        cg_s = A.alloc([128, KD, 256], BF16)
        sga_s = A.alloc([128, KD, 256], BF16)
        win_v = w_in.rearrange("(k p) n -> p k n", p=128)
        dma("gpsimd", w_own[:, :, 0:1536], win_v[:, :, 0:1536])
        dma("gpsimd", w_own[:, :, 1536:2560], win_v[:, :, C_GC:C_GC + 1024])
        dma("gpsimd", w_own[:, :, 2560:3584], win_v[:, :, C_GA:C_GA + 1024])
        dma("gpsimd", wco, w_co.rearrange("(k p) n -> p k n", p=128))
        dma("sync", cw_sb, cwT)
        dma("sync", cvs, cvecs)
        dma("sync", hv, halov)
        mset("vector", ssq, 1.0)
        for i in range(4 * 31):
            tsc("gpsimd", diag[:, i, :], ident_f, cw_sb[:, i:i + 1], None, ALU.mult)
        convb, lng, lnb = cvs[:, 0:4], cvs[:, 4:8], cvs[:, 8:12]
        pT = [bank(b).bitcast(BF16).rearrange("p (k t) -> p k t", k=KD) for b in range(3)]
        for lc in range(opts.get("c", NCH)):
            xb = xo[lc % 2]
            dma("sync", xb[:, 0:2, :], xown[lc, HALO:CW, :].rearrange("(b p) d -> p b d", p=128))
            dma("sync", xb[0:HALO, 2, :], xown[lc, 0:HALO, :])
            for b in range(3):
                np_ = 128 if b < 2 else HALO
                act(junk[0:np_, :], xb[0:np_, b, :], AF.Square, accum=ssq[0:np_, b:b + 1])
            rms_rstd(ssq, rstd)
            for b in range(3):
                np_ = 128 if b < 2 else HALO
                tsc("vector", xsb[0:np_, b, :], xb[0:np_, b, :], rstd[0:np_, b:b + 1], None, ALU.mult)
            for b in range(3):
                np_ = 128 if b < 2 else HALO
                for k in range(KD):
                    tr(pT[b][:, k, 0:np_], xsb[0:np_, b, k * 128:(k + 1) * 128], ident_b[0:np_, 0:np_], signal=(k == KD - 1))
            for k in range(KD):
                for b in range(3):
                    np_ = 128 if b < 2 else HALO
                    c0 = HALO + b * 128 if b < 2 else 0
                    act(hT[:, k, c0:c0 + np_], pT[b][:, k, 0:np_], AF.Identity, scale=a1col[:, k:k + 1], bias=sh1col[:, k:k + 1])
            for cc in range(4):
                puv, pug = bank(3)[:, 0:CW], bank(4)[:, 0:CW]
                for k in range(KD):
                    mm(puv, w_own[:, k, C_UV + cc * 128:C_UV + (cc + 1) * 128], hT[:, k, :], k == 0, k == KD - 1)
                for k in range(KD):
                    mm(pug, w_own[:, k, C_UG + cc * 128:C_UG + (cc + 1) * 128], hT[:, k, :], k == 0, k == KD - 1)
                act(sg, pug, AF.Sigmoid)
                tt("vector", a_bf[:, cc, :], puv, sg, ALU.mult)
                tt("gpsimd", a_bf[:, cc, 0:HALO], a_bf[:, cc, 0:HALO], hv[:, lc * HALO:(lc + 1) * HALO], ALU.mult)
                pconv = bank(5)[:, 0:256]
                for kk in range(31):
                    mm(pconv, diag[:, cc * 31 + kk, :], a_bf[:, cc, 2 + kk:2 + kk + 256], kk == 0, kk == 30)
                act(y_f[:, cc, :], pconv, AF.Identity, bias=convb[:, cc:cc + 1])
                cp("vector", y_b[:, cc, :], y_f[:, cc, :])
                tt("gpsimd", ysq[:, cc, :], y_f[:, cc, :], y_f[:, cc, :], ALU.mult)
            pmean, pmsq = bank(6)[:, 0:256], bank(7)[:, 0:256]
            for cc in range(4):
                mm(pmean, ones_b, y_b[:, cc, :], cc == 0, cc == 3)
            for cc in range(4):
                mm(pmsq, ones_b, ysq[:, cc, :], cc == 0, cc == 3)
            act(mean_s, pmean, AF.Copy, scale=1.0 / 512)
            tt("vector", var_s, mean_s, mean_s, ALU.mult)
            stt(var_s, pmsq, 1.0 / 512, var_s, ALU.mult, ALU.subtract)
            tsc("vector", var_s, var_s, 1e-5, None, ALU.add)
            act(var_s, var_s, AF.Sqrt)
            recip(var_s, var_s)
            for cc in range(4):
                tt("vector", yn, y_f[:, cc, :], mean_s, ALU.subtract)
                tt("vector", yn, yn, var_s, ALU.mult)
                act(actv[:, cc, :], yn, AF.Silu, scale=lng[:, cc:cc + 1], bias=lnb[:, cc:cc + 1])
            for dmc in range(KD):
                pco, pgc = bank(3)[:, 0:256], bank(4)[:, 0:256]
                for cc in range(4):
                    mm(pco, wco[:, cc, dmc * 128:(dmc + 1) * 128], actv[:, cc, :], cc == 0, cc == 3)
                for k in range(KD):
                    mm(pgc, w_own[:, k, O_GC + dmc * 128:O_GC + (dmc + 1) * 128], hT[:, k, HALO:CW], k == 0, k == KD - 1)
                act(sgate, pgc, AF.Sigmoid)
                tt("vector", cg_s[:, dmc, :], pco, sgate, ALU.mult)
            dma("scalar", cg_d[lc], cg_s.rearrange("p a b -> p (a b)"))
            for dmc in range(KD):
                pga = bank(5 + dmc % 2)[:, 0:256]
                for k in range(KD):
                    mm(pga, w_own[:, k, O_GA + dmc * 128:O_GA + (dmc + 1) * 128], hT[:, k, HALO:CW], k == 0, k == KD - 1)
                act(sga_s[:, dmc, :], pga, AF.Sigmoid)
            dma("scalar", sga_d[lc], sga_s.rearrange("p a b -> p (a b)"))
            for hp in range(4):
                pq = bank(3 + hp % 2)[:, 0:256]
                for k in range(KD):
                    mm(pq, w_own[:, k, C_Q + hp * 128:C_Q + (hp + 1) * 128], hT[:, k, HALO:CW], k == 0, k == KD - 1)
                cp("vector", qT[:, hp, lc * 256:(lc + 1) * 256], pq)
        if debug:
            dma("sync", dbg_out("qT", [128, 4 * OWN], BF16), qT.rearrange("p a b -> p (a b)"))
        A.reset(m1)
        if stop_after <= 1:
            P.emit()
            return nc, dbg

        attn = A.alloc([128, NQB, 512], BF16)
        KT = A.alloc([128, 2, S], BF16)
        V = A.alloc([128, NKB, 4, 65], BF16)
        Fneg = A.alloc([128, NKB, 8], F32)
        FrefB = A.alloc([128, NQB, 8], F32)
        m2 = A.mark()
        w_kv = A.alloc([128, KD, 1024], BF16)
        wf = A.alloc([128, KD, 128], BF16)
        refs = A.alloc([128, NKB, NQB], F32)
        fst = [A.alloc([8, 256], F32) for _ in range(2)]
        ssq2 = A.alloc([128, 2], F32)
        rstd2 = A.alloc([128, 2], F32)
        junk2 = A.alloc([128, D], BF16)
        bfc = A.alloc([8, 1], F32)
        GrefT = A.alloc([8, NQB], F32)
        Eexp = A.alloc([8, NQB, 8], F32)
        kst = [A.alloc([128, 2, 256], BF16) for _ in range(2)]
        vst = [A.alloc([128, 2, 4, 65], BF16) for _ in range(2)]
        m_alias = A.mark()
        xf = [A.alloc([128, 2, D], F32) for _ in range(2)]
        xs2 = [A.alloc([128, 2, D], BF16) for _ in range(2)]
        hT2 = [A.alloc([128, KD, 256], BF16) for _ in range(2)]
        if opts:
            mset("gpsimd", KT, 0.0)
            mset("gpsimd", V, 0.0)
            mset("gpsimd", attn, 0.0)
        dma("gpsimd", w_kv, win_v[:, :, C_K:C_K + 1024])
        mset("vector", wf, 0.0)
        dma("gpsimd", wf[:, :, 0:8], win_v[:, :, C_F:C_F + 8])
        dma("sync", refs.rearrange("p a b -> p (a b)"), refsel)
        dma("sync", bfc, bf_col)
        mset("gpsimd", V[:, :, :, 64:65], 1.0)
        for i in range(2):
            mset("gpsimd", vst[i][:, :, :, 64:65], 1.0)
        kvB_K = kvB_d[:, 0:2 * S].rearrange("p (a b) -> p a b", a=2)
        kvB_V = kvB_d[:, 2 * S:2 * S + NKB * 260].rearrange("p (a b) -> p a b", a=NKB)
        pTv = ps_all[:, 0:1024].bitcast(BF16).rearrange("p (b k t) -> p b k t", b=2, k=KD)
        NG = opts.get("g", 32)
        for gi in range(NG):
            i2 = gi % 2
            xb, xsb_, hb = xf[i2], xs2[i2], hT2[i2]
            dma("sync" if i2 == 0 else "scalar", xb, xfull[gi * 256:(gi + 1) * 256, :].rearrange("(b p) d -> p b d", p=128))
            for b in range(2):
                act(junk2, xb[:, b, :], AF.Square, accum=ssq2[:, b:b + 1])
            rms_rstd(ssq2, rstd2)
            for b in range(2):
                tsc("vector", xsb_[:, b, :], xb[:, b, :], rstd2[:, b:b + 1], None, ALU.mult)
            for b in range(2):
                for k in range(KD):
                    tr(pTv[:, b, k, :], xsb_[:, b, k * 128:(k + 1) * 128], ident_b, signal=(b == 1 and k == KD - 1))
            for k in range(KD):
                act(hb[:, k, :].rearrange("p (b t) -> p b t", b=2), pTv[:, :, k, :], AF.Identity, scale=a1col[:, k:k + 1], bias=sh1col[:, k:k + 1])
            NSK = opts.get("n", 0)
            for hp in range(4 if not (NSK & 2) else 0):
                pk = bank(2 + hp % 2)[:, 0:256]
                for k in range(KD):
                    mm(pk, w_kv[:, k, hp * 128:(hp + 1) * 128], hb[:, k, :], k == 0, k == KD - 1)
                if hp < 2:
                    cp("vector", KT[:, hp, gi * 256:(gi + 1) * 256], pk)
                else:
                    cp("vector", kst[i2][:, hp - 2, :], pk)
            if not (NSK & 2):
                dma("sync", kvB_K[:, :, gi * 256:(gi + 1) * 256], kst[i2])
            for b in range(2 if not (NSK & 4) else 0):
                pv = bank(4 + b)
                for k in range(KD):
                    mm(pv, hb[:, k, b * 128:(b + 1) * 128], w_kv[:, k, 512:1024], k == 0, k == KD - 1)
                kb = gi * 2 + b
                if not (NSK & 16):
                    cp("scalar", V[:, kb, :, 0:64], pv[:, 0:256].rearrange("p (h d) -> p h d", h=4))
                if not (NSK & 32):
                    cp("vector", vst[i2][:, b, :, 0:64], pv[:, 256:512].rearrange("p (h d) -> p h d", h=4))
            if not (NSK & 4) and not (NSK & 64):
                dma("sync", kvB_V[:, gi * 2:gi * 2 + 2, :], vst[i2].rearrange("p a b c -> p a (b c)"))
            if NSK & 8:
                continue
            pf = bank(6)[0:8, 0:256]
            pf_full = bank(6)[:, 0:256]
            for k in range(KD):
                mm(pf_full, wf[:, k, :], hb[:, k, :], k == 0, k == KD - 1)
            cp("vector", fst[i2], pf)
            dma("sync", f_d[:, gi * 256:(gi + 1) * 256], fst[i2])
        if opts.get("n", 0) & 1:
            if debug:
                dma("sync", dbg_out("KT", [128, 2 * S], BF16), KT.rearrange("p a b -> p (a b)"))
                dma("sync", dbg_out("V", [128, NKB * 260], BF16), V.rearrange("p a b c -> p (a b c)"))
            P.emit()
            return nc, dbg
        A.reset(m_alias)
        fT = A.alloc([8, S], F32)
        dma("sync", fT, f_d)
        tsc("vector", bfc, bfc, -1.0, None, ALU.mult)
        act(fT, fT, AF.Exp, scale=-1.0, bias=bfc[:, 0:1])
        act(fT, fT, AF.Ln, bias=1.0)
        P.op("vector", lambda e: e.tensor_tensor_scan(out=fT, data0=fT, data1=fT, initial=0.0, op0=ALU.add, op1=ALU.max), [fT], [fT])
        pF = bank(7)
        for kb in range(NKB):
            tr(pF[:, kb * 8:(kb + 1) * 8], fT[0:8, kb * 128:(kb + 1) * 128], ident_f[0:8, 0:8], signal=(kb == NKB - 1))
        cp("vector", Fneg.rearrange("p a b -> p (a b)"), pF)
        pG = bank(6)[0:8, 0:NQB]
        for kb in range(NKB):
            mm(pG, Fneg[:, kb, :], refs[:, kb, :], kb == 0, kb == NKB - 1)
        cp("vector", GrefT, pG)
        tt("vector", Eexp, GrefT.unsqueeze(2).to_broadcast([8, NQB, 8]), ident_f[0:8, 0:8].unsqueeze(1).to_broadcast([8, NQB, 8]), ALU.mult)
        pB = bank(5)[:, 0:128]
        mm(pB, ones_f[0:8, :], Eexp.rearrange("p a b -> p (a b)"), True, True)
        cp("vector", FrefB.rearrange("p a b -> p (a b)"), pB)
        if debug:
            dma("sync", dbg_out("KT", [128, 2 * S], BF16), KT.rearrange("p a b -> p (a b)"))
            dma("sync", dbg_out("V", [128, NKB * 260], BF16), V.rearrange("p a b c -> p (a b c)"))
            dma("sync", dbg_out("G", [128, NKB * 8]), Fneg.rearrange("p a b -> p (a b)"))
            dma("sync", dbg_out("Gref", [128, NQB * 8]), FrefB.rearrange("p a b -> p (a b)"))
        A.reset(m2)
        if stop_after <= 2:
            P.emit()
            return nc, dbg

        qpos = A.alloc([128, OWN], F32)
        kpos = A.alloc([128, NKB], F32)
        biasq = [A.alloc([128, NKB, 4], F32) for _ in range(2)]
        masks = [A.alloc([128, 8, 128], BF16) for _ in range(2)]
        Pt = A.alloc([128, 8, 128], BF16)
        rden = A.alloc([128, 4], F32)
        dma("sync", qpos, qposB)
        dma("sync", kpos, kposc)
        ztile = A.alloc([128, 2, D], BF16)
        ztile_f = A.alloc([128, 2, D], F32)
        mset("gpsimd", ztile, 0.0)
        mset("gpsimd", ztile_f, 0.0)
        nrows = NE * cap + 128
        for r0 in range(0, nrows, 256):
            nr = min(256, nrows - r0)
            dma("sync", xs_d[r0:r0 + nr, :].rearrange("(r p) d -> p r d", p=128), ztile[:, 0:nr // 128, :])
        Sps = ps_all[:, 0:2048].rearrange("p (s t) -> p s t", s=4)[:, :, 0:128]
        NQR = opts.get("q", NQB)
        for half in range(2):
            if half == 1:
                dma("sync", KT, kvB_K)
                dma("scalar", V.rearrange("p a b c -> p a (b c)"), kvB_V)
                for r0 in range(0, nrows, 256):
                    nr = min(256, nrows - r0)
                    dma("sync", ys_d[r0:r0 + nr, :].rearrange("(r p) d -> p r d", p=128), ztile_f[:, 0:nr // 128, :])
            tile_no = 0
            for qb in range(NQR):
                lc = qb // 2
                nkb, ws = kb_end(lc), win_start(lc)
                bq, mk = biasq[qb % 2], masks[qb % 2]
                tt("vector", bq, Fneg[:, :, half * 4:(half + 1) * 4], FrefB[:, qb, half * 4:(half + 1) * 4].unsqueeze(1).to_broadcast([128, NKB, 4]), ALU.subtract)
                tsc("vector", bq, bq, 0.0, None, ALU.min)
                for w in range(8):
                    tsc("gpsimd", mk[:, w, :], qpos[:, qb * 128:(qb + 1) * 128], kpos[:, ws + w:ws + w + 1], None, ALU.is_ge)
                tiles = [(hl, kb) for hl in range(4) for kb in range(nkb)]
                LA = 2

                def emit_S(i, tiles=tiles, qb=qb, half=half, tile_no=tile_no):
                    hl, kb = tiles[i]
                    n = tile_no + i
                    prt = (hl % 2) * 64
                    hpl = hl // 2
                    mm(Sps[:, n % 4, :], KT[prt:prt + 64, hpl, kb * 128:(kb + 1) * 128],
                       qT[prt:prt + 64, half * 2 + hpl, qb * 128:(qb + 1) * 128], True, True)

                for i in range(min(LA, len(tiles))):
                    emit_S(i)
                for i, (hl, kb) in enumerate(tiles):
                    n = tile_no + i
                    if i + LA < len(tiles):
                        emit_S(i + LA)
                    Ob = bank(4 + (hl % 2) + 2 * (qb % 2))
                    act(Pt[:, n % 8, :], Sps[:, n % 4, :], AF.Exp, scale=0.125, bias=bq[:, kb, hl:hl + 1])
                    if kb >= ws:
                        tt("gpsimd", Pt[:, n % 8, :], Pt[:, n % 8, :], mk[:, kb - ws, :], ALU.mult)
                    mm(Ob[:, 0:65], Pt[:, n % 8, :], V[:, kb, hl, :], kb == 0, kb == nkb - 1, signal=True)
                    if kb == nkb - 1:
                        h = half * 4 + hl
                        recip(rden[:, hl:hl + 1], Ob[:, 64:65])
                        tsc("vector", attn[:, qb, h * 64:(h + 1) * 64], Ob[:, 0:64], rden[:, hl:hl + 1], None, ALU.mult)
                tile_no += len(tiles)
        if debug:
            dma("sync", dbg_out("attn", [128, NQB * 512], BF16), attn.rearrange("p a b -> p (a b)"))
        A.reset(m2)
        if stop_after <= 3:
            P.emit()
            return nc, dbg

        wao = A.alloc([128, 4, D], BF16)
        wout = A.alloc([128, KD, D], BF16)
        wr = A.alloc([128, KD, NE], F32)
        brp = A.alloc([128, NE], F32)
        ecp = A.alloc([128, NE], F32)
        maskb = A.alloc([128, NQB, NE], BF16)
        sga_b = [A.alloc([128, KD, 128], BF16) for _ in range(2)]
        cg_b = [A.alloc([128, KD, 128], BF16) for _ in range(2)]
        xblk = [A.alloc([128, D], F32) for _ in range(2)]
        attnT = A.alloc([128, 4, 128], BF16)
        tmpf = A.alloc([128, D], F32)
        mixT = A.alloc([128, KD, 128], BF16)
        x1 = [A.alloc([128, D], F32) for _ in range(2)]
        h2f = A.alloc([128, D], F32)
        h2b = [A.alloc([128, D], BF16) for _ in range(2)]
        h2T = A.alloc([128, KD, 128], F32)
        junk3 = A.alloc([128, D], BF16)
        ssq3 = A.alloc([128, 1], F32)
        rstd3 = A.alloc([128, 1], F32)
        lg = A.alloc([128, NE], F32)
        top8 = A.alloc([128, 8], F32)
        msk = A.alloc([128, NE], F32)
        negm = A.alloc([128, 1], F32)
        ex = A.alloc([128, NE], F32)
        ssum = A.alloc([128, 1], F32)
        g32 = A.alloc([128, NE], F32)
        destf = A.alloc([128, NE], F32)
        oh = A.alloc([128, NE], F32)
        jk32 = A.alloc([128, NE], F32)
        dkf = A.alloc([128, 4], F32)
        dma("gpsimd", wao, w_ao.rearrange("(k p) n -> p k n", p=128))
        dma("gpsimd", wout, w_out.rearrange("(k p) n -> p k n", p=128))
        dma("sync", wr, w_router.rearrange("(k p) n -> p k n", p=128))
        dma("sync", brp, br_rep)
        dma("sync", ecp, ecap)
        for qb in range(NQR):
            lc, off = qb // 2, (qb % 2) * 128
            i2 = qb % 2
            dma("sync", sga_b[i2], sga_d[lc].rearrange("p (a b) -> p a b", a=KD)[:, :, off:off + 128])
            dma("sync", cg_b[i2], cg_d[lc].rearrange("p (a b) -> p a b", a=KD)[:, :, off:off + 128])
            dma("scalar", xblk[i2], xown[lc, HALO + off:HALO + off + 128, :])
            pAT = bank(0).bitcast(BF16)[:, 0:512].rearrange("p (c t) -> p c t", c=4)
            for c4 in range(4):
                tr(pAT[:, c4, :], attn[:, qb, c4 * 128:(c4 + 1) * 128], ident_b, signal=(c4 == 3))
            cp("vector", attnT, pAT)
            pAO = ps_all[:, 512:1536]
            for dmc in range(KD):
                for c4 in range(4):
                    mm(pAO[:, dmc * 128:(dmc + 1) * 128], wao[:, c4, dmc * 128:(dmc + 1) * 128], attnT[:, c4, :], c4 == 0, c4 == 3)
            tt("vector", tmpf, pAO, sga_b[i2].rearrange("p a b -> p (a b)"), ALU.mult)
            tt("gpsimd", mixT.rearrange("p a b -> p (a b)"), tmpf, cg_b[i2].rearrange("p a b -> p (a b)"), ALU.add)
            pD = ps_all[:, 1536:2560]
            for nh in range(2):
                for dk in range(KD):
                    mm(pD[:, nh * 512:(nh + 1) * 512], mixT[:, dk, :], wout[:, dk, nh * 512:(nh + 1) * 512], dk == 0, dk == KD - 1)
            x1b = x1[i2]
            tt("vector", tmpf, pD, modrow[:, 0, :], ALU.mult)
            tt("gpsimd", x1b, tmpf, xblk[i2], ALU.add)
            dma("scalar", x1_d[qb], x1b)
            act(junk3, x1b, AF.Square, accum=ssq3)
            rms_rstd(ssq3, rstd3)
            stt(h2f, x1b, rstd3[:, 0:1], a2row, ALU.mult, ALU.mult)
            tt("vector", h2f, h2f, modrow[:, 1, :], ALU.add)
            cp("scalar", h2b[i2], h2f)
            pHT = ps_all[:, 2560:3584].rearrange("p (k t) -> p k t", k=KD)
            for k in range(KD):
                tr(pHT[:, k, :], h2f[:, k * 128:(k + 1) * 128], ident_f, signal=(k == KD - 1))
            cp("vector", h2T.rearrange("p a b -> p (a b)"), ps_all[:, 2560:3584])
            plog = bank(7)[:, 0:NE]
            for k in range(KD):
                mm(plog, h2T[:, k, :], wr[:, k, :], k == 0, k == KD - 1)
            tt("vector", lg, plog, brp, ALU.add)
            P.op("vector", lambda e: e.max(out=top8, in_=lg), [lg], [top8])
            tsc("vector", msk, lg, top8[:, 3:4], None, ALU.is_ge)
            cp("vector", maskb[:, qb, :], msk)
            tsc("vector", negm, top8[:, 0:1], -1.0, None, ALU.mult)
            act(ex, lg, AF.Exp, bias=negm[:, 0:1])
            ttr(ex, ex, msk, ssum)
            recip(ssum, ssum)
            tsc("vector", g32, ex, ssum[:, 0:1], None, ALU.mult)
            ppos = bank(7)[:, 64:64 + NE]
            for b2 in range(qb + 1):
                mm(ppos, ones_b if b2 < qb else ustrict, maskb[:, b2, :], b2 == 0, b2 == qb)
            tt("vector", destf, ppos, ecp, ALU.add)
            for k in range(4):
                tsc("vector", oh, lg, top8[:, k:k + 1], None, ALU.is_equal)
                ttr(jk32, oh, g32, gates[:, qb, k:k + 1])
                ttr(jk32, oh, destf, dkf[:, k:k + 1])
            cp("vector", dests[:, qb, :], dkf)
            for k in range(4):
                P.dma("gpsimd", lambda e, qb=qb, k=k, i2=i2: e.indirect_dma_start(
                    out=xs_d[:, :], out_offset=bass.IndirectOffsetOnAxis(ap=dests[:, qb, k:k + 1], axis=0),
                    in_=h2b[i2], in_offset=None), [h2b[i2], dests[:, qb, k:k + 1]], [xs_d[:, :]])
        pcnt = bank(7)[:, 128:128 + NE]
        for b2 in range(NQR):
            mm(pcnt, ones_b, maskb[:, b2, :], b2 == 0, b2 == NQR - 1)
        t_cnt = cp("vector", cnt_i, pcnt)
        if debug:
            dma("sync", dbg_out("cnt", [128, NE], I32), cnt_i)
            dma("sync", dbg_out("lg", [128, NE]), lg)
            dma("sync", dbg_out("top8", [128, 8]), top8)
            dma("sync", dbg_out("h2f", [128, D]), h2f)
            dma("sync", dbg_out("gates", [128, NQB * 4]), gates.rearrange("p a b -> p (a b)"))
            dma("sync", dbg_out("dests", [128, NQB * 4], I32), dests.rearrange("p a b -> p (a b)"))
            d_x1 = dbg_out("x1", [NQB, 128, D])
            for qb in range(NQR):
                dma("sync", xblk[0], x1_d[qb])
                dma("sync", d_x1[qb], xblk[0])
        A.reset(A_BASE)
        if stop_after <= 4:
            P.emit()
            return nc, dbg

        W1 = [A.alloc([128, KD, 2 * D], BF16) for _ in range(2)]
        W2 = [A.alloc([128, KD, D], BF16) for _ in range(2)]
        b1s = A.alloc([128, NE * 16], F32)
        b2r = [A.alloc([128, D], F32) for _ in range(2)]
        xbk = [A.alloc([128, D], BF16) for _ in range(2)]
        xT = [A.alloc([128, KD, 128], BF16) for _ in range(2)]
        gm = [A.alloc([128, 512], F32) for _ in range(2)]
        sgm = [A.alloc([128, 512], F32) for _ in range(2)]
        uc = [A.alloc([128, 512], F32) for _ in range(2)]
        hmT = [A.alloc([128, 8, 128], BF16) for _ in range(2)]
        yb = [A.alloc([128, D], F32) for _ in range(2)]
        dma("sync", b1s, b1c)
        NEr = opts.get("e", NE)
        stg = [A.alloc([128, KD, 256], F32) for _ in range(4)]
        stg_i = [0]

        def expert_chunks(ei):
            wb1_, wb2_ = W1[ei % 2], W2[ei % 2]
            w1v = w_e1[ei].rearrange("(k p) n -> p k n", p=128)
            w2v = w_e2[ei].rearrange("(k p) n -> p k n", p=128)
            ch = []
            for c in range(12):
                if c < 8:
                    ch.append((w1v[:, :, c * 256:(c + 1) * 256], wb1_[:, :, c * 256:(c + 1) * 256]))
                else:
                    ch.append((w2v[:, :, (c - 8) * 256:(c - 7) * 256], wb2_[:, :, (c - 8) * 256:(c - 7) * 256]))
            return ch

        def w_dma(ei, c):
            src, dst = expert_chunks(ei)[c]
            dma("sync", stg[(ei * 8 + c) % 4], src)

        def w_cast(ei, c):
            src, dst = expert_chunks(ei)[c]
            cp("scalar", dst, stg[(ei * 8 + c) % 4])

        NCHK = 8

        def w2_dma(ei):
            dma("gpsimd", W2[ei % 2], w_e2[ei].rearrange("(k p) n -> p k n", p=128))

        dma("sync", b2r[0], b_e2[0:1, :].to_broadcast([128, D]))
        w2_dma(0)
        for c in range(4):
            w_dma(0, c)
        for c in range(NCHK):
            w_cast(0, c)
            if c + 4 < NCHK:
                w_dma(0, c + 4)
        for e_ in range(NEr):
            wb1, wb2 = W1[e_ % 2], W2[e_ % 2]
            nxt = e_ + 1 if e_ + 1 < NEr else None
            if nxt is not None:
                dma("sync", b2r[nxt % 2], b_e2[nxt:nxt + 1, :].to_broadcast([128, D]))
                w2_dma(nxt)
                for c in range(4):
                    w_dma(nxt, c)
            P.regload(e_, [t_cnt])
            dma("scalar", xbk[0], xs_d[e_ * cap:e_ * cap + 128, :])

            def st_T(kb_):
                P.guard = (e_, kb_ * 128)
                xk = xbk[kb_ % 2]
                xTk = xT[kb_ % 2]
                pXT = bank(0).bitcast(BF16).rearrange("p (k t) -> p k t", k=KD)
                for k in range(KD):
                    tr(pXT[:, k, :], xk[:, k * 128:(k + 1) * 128], ident_b, signal=(k == KD - 1))
                cp("vector", xTk.rearrange("p a b -> p (a b)"), bank(0).bitcast(BF16))
                P.guard = None

            def st_H(kb_, half2):
                P.guard = (e_, kb_ * 128)
                xTk = xT[kb_ % 2]
                gm_, uc_, sgm_ = gm[half2], uc[half2], sgm[half2]
                pg, pu = bank(1 + half2), bank(3 + half2)
                for j4 in range(4):
                    fc = half2 * 4 + j4
                    for k in range(KD):
                        mm(pg[:, j4 * 128:(j4 + 1) * 128], wb1[:, k, fc * 128:(fc + 1) * 128], xTk[:, k, :], k == 0, k == KD - 1)
                for j4 in range(4):
                    fc = 8 + half2 * 4 + j4
                    for k in range(KD):
                        mm(pu[:, j4 * 128:(j4 + 1) * 128], wb1[:, k, fc * 128:(fc + 1) * 128], xTk[:, k, :], k == 0, k == KD - 1)
                for j4 in range(4):
                    fcg = half2 * 4 + j4
                    tsc("vector", gm_[:, j4 * 128:(j4 + 1) * 128], pg[:, j4 * 128:(j4 + 1) * 128], b1s[:, e_ * 16 + fcg:e_ * 16 + fcg + 1], 7.0, ALU.add, ALU.min)
                    tsc("vector", uc_[:, j4 * 128:(j4 + 1) * 128], pu[:, j4 * 128:(j4 + 1) * 128], b1s[:, e_ * 16 + 8 + fcg:e_ * 16 + 8 + fcg + 1], 7.0, ALU.add, ALU.min)
                act(sgm_, gm_, AF.Silu, scale=1.702)
                tsc("vector", uc_, uc_, -7.0, 1.0, ALU.max, ALU.add)
                stt(hmT[kb_ % 2][:, half2 * 4:(half2 + 1) * 4, :].rearrange("p a b -> p (a b)"), uc_, 1.0 / 1.702, sgm_, ALU.mult, ALU.mult)
                P.guard = None

            def st_Y(kb_):
                P.guard = (e_, kb_ * 128)
                row0 = e_ * cap + kb_ * 128
                pY = ps_all[:, 2560:3584]
                for nh in range(2):
                    for fc in range(8):
                        mm(pY[:, nh * 512:(nh + 1) * 512], hmT[kb_ % 2][:, fc, :], wb2[:, fc, nh * 512:(nh + 1) * 512], fc == 0, fc == 7)
                ybk = yb[kb_ % 2]
                tt("vector", ybk, pY, b2r[e_ % 2], ALU.add)
                dma("scalar", ys_d[row0:row0 + 128, :], ybk)
                P.guard = None

            def st_prefetch(kb_):
                if kb_ < kmax:
                    P.guard = (e_, kb_ * 128)
                    dma("scalar", xbk[kb_ % 2], xs_d[e_ * cap + kb_ * 128:e_ * cap + kb_ * 128 + 128, :])
                    P.guard = None

            st_prefetch(1)
            st_T(0)
            st_H(0, 0)
            for kblk in range(kmax):
                st_H(kblk, 1)
                if kblk + 1 < kmax:
                    st_prefetch(kblk + 2)
                    st_T(kblk + 1)
                    st_H(kblk + 1, 0)
                st_Y(kblk)
                if nxt is not None:
                    for c in (2 * kblk, 2 * kblk + 1):
                        if c < NCHK:
                            w_cast(nxt, c)
                            if c + 4 < NCHK:
                                w_dma(nxt, c + 4)
        A.reset(A_BASE)
        if stop_after <= 5:
            P.emit()
            return nc, dbg

        yk = [A.alloc([128, D], F32) for _ in range(4)]
        acc = A.alloc([128, D], F32)
        x1r = [A.alloc([128, D], F32) for _ in range(2)]
        finr = A.alloc([128, D], F32)
        junk5 = A.alloc([128, D], BF16)
        ssq5 = A.alloc([128, 1], F32)
        rstd5 = A.alloc([128, 1], F32)
        ob = [A.alloc([128, D], F32) for _ in range(2)]
        dma("sync", finr, fin_rep)
        for qb in range(NQR):
            i2 = qb % 2
            dma("sync", x1r[i2], x1_d[qb])
            for k in range(4):
                P.dma("gpsimd", lambda e, qb=qb, k=k: e.indirect_dma_start(
                    out=yk[k], out_offset=None, in_=ys_d[:, :],
                    in_offset=bass.IndirectOffsetOnAxis(ap=dests[:, qb, k:k + 1], axis=0)), [ys_d[:, :], dests[:, qb, k:k + 1]], [yk[k]])
            tsc("vector", acc, yk[0], gates[:, qb, 0:1], None, ALU.mult)
            for k in range(1, 4):
                stt(acc, yk[k], gates[:, qb, k:k + 1], acc, ALU.mult, ALU.add)
            tt("gpsimd", acc, acc, modrow[:, 3, :], ALU.mult)
            tt("gpsimd", acc, acc, x1r[i2], ALU.add)
            act(junk5, acc, AF.Square, accum=ssq5)
            rms_rstd(ssq5, rstd5)
            stt(ob[i2], acc, rstd5[:, 0:1], finr, ALU.mult, ALU.mult)
            dma("sync", out[qb * 128:(qb + 1) * 128, :], ob[i2])
        P.emit()
    return nc, dbg


def host_inputs(inp, kmax=KMAX):
    cap = kmax * 128
    f = lambda a: np.ascontiguousarray(a, dtype=np.float32)
    x = inp["x"]
    col = lambda v: f(v.reshape(-1, 128).T)
    rep = lambda v: f(np.broadcast_to(v.reshape(1, -1), (128, v.size)))
    shared = {
        "w_ada": f(inp["w_ada"][0]),
        "b_ada_col": col(inp["b_ada"][0]),
        "n1g_col": col(inp["norm1_g"][0]),
        "n2g_rep": rep(inp["norm2_g"][0]),
        "fin_rep": rep(inp["final_g"]),
        "w_in": f(inp["w_in"][0]),
        "bf_col": f(inp["b_forget"][0].reshape(8, 1)),
        "cwT": f(inp["conv_w"][0].T.reshape(4, 128, 31).transpose(1, 0, 2).reshape(128, 124)),
        "cvecs": f(np.concatenate([col(inp["conv_b"][0]), col(inp["conv_ln_g"][0]), col(inp["conv_ln_b"][0])], axis=1)),
        "w_co": f(inp["w_conv_out"][0]),
        "w_ao": f(inp["w_attn_out"][0]),
        "w_out": f(inp["w_out"][0]),
        "w_router": f(inp["w_router"][0]),
        "br_rep": rep(inp["b_router"][0]),
        "w_e1": f(inp["w_exp_in"][0]),
        "b1c": f(inp["b_exp_in"][0].reshape(NE, 16, 128).transpose(2, 0, 1).reshape(128, NE * 16)),
        "w_e2": f(inp["w_exp_out"][0]),
        "b_e2": f(inp["b_exp_out"][0]),
        "kposc": f((np.arange(NKB)[None, :] * 128 + np.arange(128)[:, None])),
        "ecap": rep(np.arange(NE, dtype=np.float32) * cap),
    }
    maps = []
    poss = []
    for c in range(NCORES):
        b, j = c // 4, c % 4
        ch = chunks_of(j)
        xo = np.zeros((NCH, CW, D), np.float32)
        hv = np.zeros((128, NCH * HALO), np.float32)
        pos = np.zeros(OWN, np.int64)
        for lc, m in enumerate(ch):
            s0 = m * 256
            xo[lc, HALO:] = x[b, s0:s0 + 256]
            if m > 0:
                xo[lc, :HALO] = x[b, s0 - HALO:s0]
                hv[:, lc * HALO:(lc + 1) * HALO] = 1.0
            pos[lc * 256:(lc + 1) * 256] = np.arange(s0, s0 + 256)
        refs = np.zeros((128, NKB, NQB), np.float32)
        for qb in range(NQB):
            pl = pos[qb * 128 + 127]
            refs[pl % 128, pl // 128, qb] = 1.0
        d = dict(shared)
        d.update({
            "xfull": f(x[b]), "xown": xo, "halov": hv, "cvec": col(inp["c"][b]),
            "qposB": rep(pos.astype(np.float32)), "refsel": refs.reshape(128, NKB * NQB),
        })
        maps.append(d)
        poss.append((b, pos))
    return maps, poss


def kernel(**inp):
    inp = {k: np.asarray(v) for k, v in inp.items()}
    nc, _ = build()
    maps, poss = host_inputs(inp)
    res = run_bass_kernel_spmd(nc, maps, core_ids=list(range(NCORES)))
    out = np.zeros((2, S, D), np.float32)
    for c in range(NCORES):
        b, pos = poss[c]
        out[b, pos] = res.results[c]["out"]
    return out
```

```python
import numpy as np
from contextlib import ExitStack
import concourse.bass as bass
import concourse.mybir as mybir
from concourse.bass_utils import run_bass_kernel_spmd

F32 = mybir.dt.float32
BF16 = mybir.dt.bfloat16
I32 = mybir.dt.int32
U32 = mybir.dt.uint32
AF = mybir.ActivationFunctionType
ALU = mybir.AluOpType

ENGS = ("sync", "scalar", "vector", "gpsimd", "tensor")
NCORES = 8
D = 1024
KD = 8
S = 8192
NKB = 64
OWN = 2048
NCH = 8
NQB = 16
HALO = 32
CW = 256 + HALO
NE = 32
KMAX = 7
C_UV, C_UG, C_Q, C_K, C_V, C_F, C_GC, C_GA = 0, 512, 1024, 1536, 2048, 2560, 2568, 3592
_DT_SIZE = {F32: 4, BF16: 2, I32: 4, U32: 4}
SEM_ROT = 2000
NPOOL = 24
NSW = 12


class Tok:
    __slots__ = ("sem", "val", "gen")

    def __init__(self, sem=None, val=None, gen=0):
        self.sem = sem
        self.val = val
        self.gen = gen


class Rec:
    __slots__ = ("p0", "p1", "lo", "hi", "w", "tok", "eng", "alive", "name")


def region(ap):
    es = _DT_SIZE[ap.dtype]
    pairs = ap.ap
    off = ap.offset
    sp = str(ap.space)
    if sp in ("SB", "PSUM"):
        pstride, npart = pairs[0]
        if pstride <= 0:
            pstride = 1 << 40
        p0 = off // pstride
        col = off % pstride
        ext = 1 + sum((c - 1) * abs(s) for s, c in pairs[1:])
        return (ap.name, p0, p0 + npart, col * es, (col + ext) * es, 4096)
    ext = 1 + sum((c - 1) * abs(s) for s, c in pairs)
    return (ap.name, 0, 1, off * es, (off + ext) * es, 1 << 20)


class Prog:
    def __init__(self, nc, es):
        self.nc = nc
        self.es = es
        self.streams = {e: [] for e in ENGS}
        self.esem = {}
        self.ecount = {}
        self.nsem = 0
        self.pending = {e: [] for e in ENGS}
        self.bins = {}
        self.pool = []
        self.pool_last = []
        self.pool_i = 0
        self.dma_uid = 0
        self.noguard = 0
        self.guard = None
        self.fake = {}
        self.cnt_ap = None
        for e in ENGS:
            self._new_esem(e)
        self.qpool = {q: [self._mksem(f"dq_{q}{i}") for i in range(n)] for q, n in (("sync", 16), ("scalar", 12))}
        self.qlast = {q: [None] * len(v) for q, v in self.qpool.items()}
        self.qi = {q: 0 for q in self.qpool}
        self.swpool = [self._mksem(f"sq{i}") for i in range(NSW)]
        self.sw_last = [None] * NSW
        self.sw_i = 0

    def _mksem(self, name):
        self.nsem += 1
        return self.es.enter_context(self.nc.semaphore(name))

    def _new_esem(self, e):
        self.esem[e] = self._mksem(f"s_{e}_{self.nsem}")
        self.ecount[e] = 0

    def _bins(self, reg):
        name, p0, p1, lo, hi, bs = reg
        return [(name, b) for b in range(lo // bs, (hi - 1) // bs + 1)]

    def _query(self, reg):
        name, p0, p1, lo, hi, bs = reg
        seen = set()
        out = []
        for key in self._bins(reg):
            lst = self.bins.get(key)
            if not lst:
                continue
            dead = 0
            for r in lst:
                if not r.alive:
                    dead += 1
                    continue
                if id(r) in seen:
                    continue
                if r.lo < hi and lo < r.hi and r.p0 < p1 and p0 < r.p1:
                    seen.add(id(r))
                    out.append(r)
            if dead > 16:
                self.bins[key] = [r for r in lst if r.alive]
        return out

    def _add(self, reg, w, tok, eng):
        name, p0, p1, lo, hi, bs = reg
        r = Rec()
        r.p0, r.p1, r.lo, r.hi, r.w, r.tok, r.eng, r.alive, r.name = p0, p1, lo, hi, w, tok, eng, True, name
        for key in self._bins(reg):
            self.bins.setdefault(key, []).append(r)

    def _deps(self, eng, reads, writes, tok, engkey):
        deps = []
        rregs = [a if isinstance(a, tuple) else region(a) for a in reads]
        wregs = [a if isinstance(a, tuple) else region(a) for a in writes]
        for reg in rregs:
            for r in self._query(reg):
                if r.w and not (eng == "tensor" and r.eng == "tensor"):
                    deps.append(r.tok)
            if reg[0] == "parena":
                name, p0, p1, lo, hi, bs = reg
                breg = (name, 0, 128, (lo // 2048) * 2048, ((hi + 2047) // 2048) * 2048, bs)
                for r in self._query(breg):
                    if (not r.w) and r.eng != engkey and r.eng != "tensor":
                        deps.append(r.tok)
        for reg in wregs:
            for r in self._query(reg):
                if not (eng == "tensor" and r.eng == "tensor"):
                    deps.append(r.tok)
        for reg in rregs:
            name, p0, p1, lo, hi, bs = reg
            rep = False
            for r in self._query(reg):
                if (not r.w) and r.eng == engkey and r.p0 == p0 and r.p1 == p1 and r.lo == lo and r.hi == hi:
                    r.tok = tok
                    rep = True
                    break
            if not rep:
                self._add(reg, False, tok, engkey)
        for reg in wregs:
            name, p0, p1, lo, hi, bs = reg
            for r in self._query(reg):
                if r.p0 >= p0 and r.p1 <= p1 and r.lo >= lo and r.hi <= hi:
                    r.alive = False
            self._add(reg, True, tok, engkey)
        return deps

    def op(self, eng, fn, r=(), w=(), signal=True):
        if signal:
            if self.ecount[eng] >= SEM_ROT:
                self._new_esem(eng)
            self.ecount[eng] += 1
            tok = Tok(self.esem[eng], self.ecount[eng])
            for p in self.pending[eng]:
                p.sem, p.val = tok.sem, tok.val
            self.pending[eng] = []
        else:
            tok = Tok()
            self.pending[eng].append(tok)
        deps = self._deps(eng, r, w, tok, eng)
        self.streams[eng].append((fn, deps, tok if signal else None, 1, self.guard))
        return tok

    def regload(self, key, deps):
        for e in ENGS:
            self.streams[e].append((("regload", key), list(deps), None, 0, None))

    def dma(self, eng, fn, r=(), w=()):
        if eng == "gpsimd":
            i = self.sw_i
            self.sw_i = (self.sw_i + 1) % NSW
            prev = self.sw_last[i]
            val = (prev.val if prev is not None else 0) + 16
            tok = Tok(self.swpool[i], val)
            self.sw_last[i] = tok
            self.dma_uid += 1
            deps = self._deps(eng, r, w, tok, "dma%d" % self.dma_uid)
            if prev is not None:
                deps.append(prev)
            self.streams[eng].append((fn, deps, tok, 16, self.guard))
            return tok
        pool, last = self.qpool[eng], self.qlast[eng]
        i = self.qi[eng]
        self.qi[eng] = (i + 1) % len(pool)
        prev = last[i]
        val = (prev.val if prev is not None else 0) + 16
        tok = Tok(pool[i], val)
        last[i] = tok
        self.dma_uid += 1
        deps = self._deps(eng, r, w, tok, "dma%d" % self.dma_uid)
        if prev is not None:
            deps.append(prev)
        self.streams[eng].append((fn, deps, tok, 16, self.guard))
        return tok

    def emit(self):
        nc = self.nc
        final = [t for q in self.qlast for t in self.qlast[q] if t is not None] + [t for t in self.sw_last if t is not None]
        with nc.Block() as block:
            for e in ENGS:
                stream = self.streams[e]
                fw = final if e == "sync" else []

                def body(engine, stream=stream, fw=fw, ename=e):
                    waited = {}
                    lastfake = [None]
                    reg = engine.alloc_register("gcnt")

                    def dowait(d):
                        assert d.sem is not None, "unresolved token"
                        k = (id(d.sem), d.gen)
                        if waited.get(k, 0) >= d.val:
                            return
                        waited[k] = d.val
                        engine.wait_ge(d.sem, d.val)

                    def emit_one(ent):
                        fn, deps, tok, inc, g = ent
                        for d in deps:
                            dowait(d)
                        if isinstance(fn, tuple):
                            if self.noguard != 2:
                                engine.reg_load(reg, self.cnt_ap(fn[1]))
                            return
                        ins = fn(engine)
                        if tok is not None:
                            ins.then_inc(tok.sem, inc)

                    i = 0
                    n = len(stream)
                    while i < n:
                        g = stream[i][4]
                        if g is None or self.noguard:
                            emit_one(stream[i])
                            i += 1
                            continue
                        j = i
                        while j < n and stream[j][4] == g:
                            j += 1
                        grp = stream[i:j]
                        saved = dict(waited)
                        with engine.If_lt(reg, g[1] + 1):
                            for fn, deps, tok, inc, _ in grp:
                                for d in deps:
                                    dowait(d)
                                if tok is None:
                                    continue
                                if inc == 16:
                                    engine.sem_inc(tok.sem, 16)
                                else:
                                    if ename == "scalar" and lastfake[0] is not None:
                                        engine.wait_ge(lastfake[0].sem, lastfake[0].val)
                                    self.fake[ename](engine).then_inc(tok.sem, 1)
                                    lastfake[0] = tok
                        waited.clear()
                        waited.update(saved)
                        with engine.Else():
                            for ent in grp:
                                emit_one(ent)
                        waited.clear()
                        waited.update(saved)
                        i = j
                    for d in fw:
                        dowait(d)

                getattr(block, e)(body)


class Arena:
    def __init__(self, ap, nwords):
        self.ap = ap
        self.n = nwords
        self.off = 0

    def mark(self):
        return self.off

    def reset(self, off=0):
        self.off = off

    def alloc(self, shape, dt):
        free = int(np.prod(shape[1:]))
        words = (free * _DT_SIZE[dt] + 3) // 4
        words += words % 2
        assert self.off + words <= self.n, ("arena overflow", self.off, words, self.n)
        v = self.ap[0:shape[0], self.off:self.off + words]
        self.off += words
        if dt != F32:
            v = v.bitcast(dt)
        v = v[:, 0:free]
        if len(shape) > 2:
            names = [f"a{i}" for i in range(len(shape) - 1)]
            pat = "p (" + " ".join(names) + ") -> p " + " ".join(names)
            v = v.rearrange(pat, **{n: s for n, s in zip(names[:-1], shape[1:-1])})
        return v


def chunks_of(j):
    ch = []
    for g in range(4):
        ch += [8 * g + j, 8 * g + 7 - j]
    return ch


def kb_end(lc):
    g, s = lc // 2, lc % 2
    return 16 * g + 8 * s + 8


def win_start(lc):
    g, s = lc // 2, lc % 2
    return 16 * g + 8 * s


def isap(x):
    return hasattr(x, "ap") and hasattr(x, "dtype") and hasattr(x, "offset")


def build(debug=False, kmax=KMAX, stop_after=99, variant=''):
    cap = kmax * 128
    opts = {}
    for vv in variant.split("_"):
        if len(vv) > 1 and vv[1:].isdigit():
            opts[vv[0]] = int(vv[1:])
    nc = bass.Bass("TRN2", target_bir_lowering=False)
    dram_in = lambda n, shp, dt=F32: nc.dram_tensor(n, list(shp), dt, kind="ExternalInput").ap()
    xfull = dram_in("xfull", [S, D])
    xown = dram_in("xown", [NCH, CW, D])
    halov = dram_in("halov", [128, NCH * HALO])
    cvec = dram_in("cvec", [128, KD])
    w_ada = dram_in("w_ada", [D, 6 * D])
    b_ada_col = dram_in("b_ada_col", [128, 48])
    n1g_col = dram_in("n1g_col", [128, KD])
    n2g_rep = dram_in("n2g_rep", [128, D])
    fin_rep = dram_in("fin_rep", [128, D])
    w_in = dram_in("w_in", [D, 4616])
    bf_col = dram_in("bf_col", [8, 1])
    cwT = dram_in("cwT", [128, 4 * 31])
    cvecs = dram_in("cvecs", [128, 12])
    w_co = dram_in("w_co", [512, D])
    w_ao = dram_in("w_ao", [512, D])
    w_out = dram_in("w_out", [D, D])
    w_router = dram_in("w_router", [D, NE])
    br_rep = dram_in("br_rep", [128, NE])
    qposB = dram_in("qposB", [128, OWN])
    refsel = dram_in("refsel", [128, NKB * NQB])
    kposc = dram_in("kposc", [128, NKB])
    ecap = dram_in("ecap", [128, NE])
    if stop_after >= 5:
        w_e1 = dram_in("w_e1", [NE, D, 2 * D])
        b1c = dram_in("b1c", [128, NE * 16])
        w_e2 = dram_in("w_e2", [NE, D, D])
        b_e2 = dram_in("b_e2", [NE, D])
    out = nc.dram_tensor("out", [OWN, D], F32, kind="ExternalOutput").ap()
    dbg = {}

    def dbg_out(name, shp, dt=F32):
        dbg[name] = nc.dram_tensor("dbg_" + name, list(shp), dt, kind="ExternalOutput").ap()
        return dbg[name]

    scr = lambda n, shp, dt: nc.dram_tensor(n, list(shp), dt, kind="Internal").ap()
    cg_d = scr("cg_d", [NCH, 128, KD * 256], BF16)
    sga_d = scr("sga_d", [NCH, 128, KD * 256], BF16)
    kvB_d = scr("kvB_d", [128, 2 * S + NKB * 4 * 65], BF16)
    x1_d = scr("x1_d", [NQB, 128, D], F32)
    xs_d = scr("xs_d", [NE * cap + 128, D], BF16)
    ys_d = scr("ys_d", [NE * cap + 128, D], F32)
    f_d = scr("f_d", [8, S], F32)

    with ExitStack() as es:
        SBW = 51 * 1024
        sb_all = es.enter_context(nc.sbuf_tensor("arena", [128, SBW], F32))
        ps_all = es.enter_context(nc.psum_tensor("parena", [128, 4096], F32))
        A = Arena(sb_all, SBW)
        P = Prog(nc, es)

        def bank(i, n=1):
            return ps_all[:, i * 512:(i + n) * 512]

        def act(out, in_, func, scale=None, bias=None, accum=None):
            r = [in_] + [x for x in (scale, bias) if isap(x)]
            w = [out] + ([accum] if accum is not None else [])
            kw = {}
            if scale is not None:
                kw["scale"] = scale
            if bias is not None:
                kw["bias"] = bias
            if accum is not None:
                kw["accum_out"] = accum
            return P.op("scalar", lambda e: e.activation(out=out, in_=in_, func=func, **kw), r, w)

        def tsc(eng, out, in0, s1, s2=None, op0=ALU.mult, op1=None):
            r = [in0] + [x for x in (s1, s2) if isap(x)]
            kw = {} if op1 is None else {"op1": op1}
            return P.op(eng, lambda e: e.tensor_scalar(out=out, in0=in0, scalar1=s1, scalar2=s2, op0=op0, **kw), r, [out])

        def tt(eng, out, in0, in1, op):
            return P.op(eng, lambda e: e.tensor_tensor(out=out, in0=in0, in1=in1, op=op), [in0, in1], [out])

        def stt(out, in0, scalar, in1, op0, op1):
            r = [in0, in1] + ([scalar] if isap(scalar) else [])
            return P.op("vector", lambda e: e.scalar_tensor_tensor(out=out, in0=in0, scalar=scalar, in1=in1, op0=op0, op1=op1), r, [out])

        def ttr(out, in0, in1, accum):
            return P.op("vector", lambda e: e.scalar_tensor_tensor(out=out, in0=in0, scalar=1.0, in1=in1, op0=ALU.mult, op1=ALU.mult, accum_out=accum),
                        [in0, in1], [out, accum])

        def cp(eng, out, in_):
            if eng == "scalar":
                return P.op("scalar", lambda e: e.activation(out=out, in_=in_, func=AF.Copy), [in_], [out])
            return P.op(eng, lambda e: e.tensor_copy(out=out, in_=in_), [in_], [out])

        def mset(eng, ap, v):
            return P.op(eng, lambda e: e.memset(ap, v), [], [ap])

        def recip(out, in_):
            return P.op("vector", lambda e: e.reciprocal(out=out, in_=in_), [in_], [out])

        def bankreg(ap):
            name, p0, p1, lo, hi, bs = region(ap)
            return (name, 0, 128, (lo // 2048) * 2048, ((hi + 2047) // 2048) * 2048, bs)

        def mm(out, lhsT, rhs, start, stop, signal=None):
            if signal is None:
                signal = stop
            return P.op("tensor", lambda e: e.matmul(out, lhsT=lhsT, rhs=rhs, start=start, stop=stop), [lhsT, rhs], [bankreg(out)], signal=signal)

        def tr(out, in_, ident, signal=True):
            return P.op("tensor", lambda e: e.transpose(out=out, in_=in_, identity=ident), [in_, ident], [bankreg(out)], signal=signal)

        def dma(eng, out, in_):
            return P.dma(eng, lambda e: e.dma_start(out=out, in_=in_), [in_], [out])

        def rms_rstd(ssq_ap, rstd_ap):
            tsc("vector", rstd_ap, ssq_ap, 1.0 / D, 1e-5, ALU.mult, ALU.add)
            act(rstd_ap, rstd_ap, AF.Sqrt)
            recip(rstd_ap, rstd_ap)

        identi = A.alloc([128, 128], I32)
        ident_f = A.alloc([128, 128], F32)
        ident_b = A.alloc([128, 128], BF16)
        ones_f = A.alloc([128, 128], F32)
        ones_b = A.alloc([128, 128], BF16)
        ustrict = A.alloc([128, 128], BF16)
        modcol = A.alloc([128, 48], F32)
        a1col = A.alloc([128, KD], F32)
        modrow = A.alloc([128, 4, D], F32)
        a2row = A.alloc([128, D], F32)
        gates = A.alloc([128, NQB, 4], F32)
        dests = A.alloc([128, NQB, 4], I32)
        cnt_i = A.alloc([128, NE], I32)
        fsb = {en: A.alloc([128, 2], F32) for en in ("scalar", "vector", "gpsimd")}
        A_BASE = A.mark()
        qT = A.alloc([128, 4, OWN], BF16)

        P.op("gpsimd", lambda e: e.iota(identi, pattern=[[1, 128]], base=0, channel_multiplier=-1), [], [identi])
        tsc("vector", ident_f, identi, 0, None, ALU.is_equal)
        tsc("vector", ident_b, identi, 0, None, ALU.is_equal)
        tsc("vector", ustrict, identi, 0, None, ALU.is_gt)
        mset("vector", ones_f, 1.0)
        mset("vector", ones_b, 1.0)
        for en in ("scalar", "vector", "gpsimd"):
            mset(en if en != "scalar" else "vector", fsb[en], 0.0)
        fps = bank(7)[:, 510:511]
        P.fake = {
            "tensor": lambda e: e.matmul(fps, lhsT=ones_b, rhs=ones_b[:, 0:1], start=True, stop=True),
            "scalar": lambda e: e.drain(),
            "vector": lambda e: e.engine_nop(),
            "gpsimd": lambda e: e.engine_nop(),
        }
        P.cnt_ap = lambda ei: cnt_i[0:1, ei:ei + 1]
        P.noguard = opts.get("d", 0)
        if opts:
            mset("gpsimd", qT, 0.0)
            mset("gpsimd", gates, 0.0)
            mset("gpsimd", dests, 0)

        m0 = A.mark()
        cv = A.alloc([128, KD], F32)
        scv = A.alloc([128, KD], F32)
        ydiag = A.alloc([128, D], F32)
        bcol = A.alloc([128, 48], F32)
        n1g = A.alloc([128, KD], F32)
        n2g = A.alloc([128, D], F32)
        wab = [A.alloc([128, KD, 512], F32) for _ in range(2)]
        dma("sync", cv, cvec)
        dma("sync", bcol, b_ada_col)
        dma("sync", n1g, n1g_col)
        dma("sync", n2g, n2g_rep)
        act(scv, cv, AF.Silu)
        pmod = bank(0)[:, 0:48]
        wa_v = w_ada.rearrange("(k p) n -> p k n", p=128)
        for gi in range(12):
            buf = wab[gi % 2]
            dma("sync" if gi % 2 == 0 else "scalar", buf, wa_v[:, :, gi * 512:(gi + 1) * 512])
            for oc in range(4):
                col = gi * 4 + oc
                for k in range(KD):
                    mm(pmod[:, col:col + 1], buf[:, k, oc * 128:(oc + 1) * 128], scv[:, k:k + 1], k == 0, k == KD - 1)
        tt("vector", modcol, pmod, bcol, ALU.add)
        stt(a1col, modcol[:, 8:16], 1.0, n1g, ALU.add, ALU.mult)
        sh1col = modcol[:, 0:8]
        for r_, mi in enumerate((2, 3, 4, 5)):
            for k in range(KD):
                tsc("vector", ydiag[:, k * 128:(k + 1) * 128], ident_f, modcol[:, mi * 8 + k:mi * 8 + k + 1], None, ALU.mult)
            for hf in range(2):
                pr = bank(1 + hf)
                mm(pr, ones_f, ydiag[:, hf * 512:(hf + 1) * 512], True, True)
                cp("vector", modrow[:, r_, hf * 512:(hf + 1) * 512], pr)
        stt(a2row, modrow[:, 2, :], 1.0, n2g, ALU.add, ALU.mult)
        if debug:
            dma("sync", dbg_out("modcol", [128, 48]), modcol)
            dma("sync", dbg_out("modrow", [128, 4 * D]), modrow.rearrange("p a b -> p (a b)"))
        A.reset(m0)
        if stop_after <= 0:
            P.emit()
            return nc, dbg

        m1 = A.mark()
        w_own = A.alloc([128, KD, 3584], BF16)
        O_GC, O_GA = 1536, 2560
        wco = A.alloc([128, 4, D], BF16)
        cw_sb = A.alloc([128, 4 * 31], F32)
        cvs = A.alloc([128, 12], F32)
        diag = A.alloc([128, 4 * 31, 128], BF16)
        hv = A.alloc([128, NCH * HALO], F32)
        xo = [A.alloc([128, 3, D], F32) for _ in range(2)]
        xsb = A.alloc([128, 3, D], BF16)
        junk = A.alloc([128, D], BF16)
        ssq = A.alloc([128, 4], F32)
        rstd = A.alloc([128, 4], F32)
        hT = A.alloc([128, KD, CW], BF16)
        sg = A.alloc([128, CW], F32)
        a_bf = A.alloc([128, 4, CW], BF16)
        y_f = A.alloc([128, 4, 256], F32)
        y_b = A.alloc([128, 4, 256], BF16)
        ysq = A.alloc([128, 4, 256], BF16)
        mean_s = A.alloc([128, 256], F32)
        var_s = A.alloc([128, 256], F32)
        yn = A.alloc([128, 256], F32)
        actv = A.alloc([128, 4, 256], BF16)
        sgate = A.alloc([128, 256], F32)
        cg_s = A.alloc([128, KD, 256], BF16)
        sga_s = A.alloc([128, KD, 256], BF16)
        win_v = w_in.rearrange("(k p) n -> p k n", p=128)
        dma("gpsimd", w_own[:, :, 0:1536], win_v[:, :, 0:1536])
        dma("gpsimd", w_own[:, :, 1536:2560], win_v[:, :, C_GC:C_GC + 1024])
        dma("gpsimd", w_own[:, :, 2560:3584], win_v[:, :, C_GA:C_GA + 1024])
        dma("gpsimd", wco, w_co.rearrange("(k p) n -> p k n", p=128))
        dma("sync", cw_sb, cwT)
        dma("sync", cvs, cvecs)
        dma("sync", hv, halov)
        mset("vector", ssq, 1.0)
        for i in range(4 * 31):
            tsc("gpsimd", diag[:, i, :], ident_f, cw_sb[:, i:i + 1], None, ALU.mult)
        convb, lng, lnb = cvs[:, 0:4], cvs[:, 4:8], cvs[:, 8:12]
        pT = [bank(b).bitcast(BF16).rearrange("p (k t) -> p k t", k=KD) for b in range(3)]
        for lc in range(opts.get("c", NCH)):
            xb = xo[lc % 2]
            dma("sync", xb[:, 0:2, :], xown[lc, HALO:CW, :].rearrange("(b p) d -> p b d", p=128))
            dma("sync", xb[0:HALO, 2, :], xown[lc, 0:HALO, :])
            for b in range(3):
                np_ = 128 if b < 2 else HALO
                act(junk[0:np_, :], xb[0:np_, b, :], AF.Square, accum=ssq[0:np_, b:b + 1])
            rms_rstd(ssq, rstd)
            for b in range(3):
                np_ = 128 if b < 2 else HALO
                tsc("vector", xsb[0:np_, b, :], xb[0:np_, b, :], rstd[0:np_, b:b + 1], None, ALU.mult)
            for b in range(3):
                np_ = 128 if b < 2 else HALO
                for k in range(KD):
                    tr(pT[b][:, k, 0:np_], xsb[0:np_, b, k * 128:(k + 1) * 128], ident_b[0:np_, 0:np_], signal=(k == KD - 1))
            for k in range(KD):
                for b in range(3):
                    np_ = 128 if b < 2 else HALO
                    c0 = HALO + b * 128 if b < 2 else 0
                    act(hT[:, k, c0:c0 + np_], pT[b][:, k, 0:np_], AF.Identity, scale=a1col[:, k:k + 1], bias=sh1col[:, k:k + 1])
            for cc in range(4):
                puv, pug = bank(3)[:, 0:CW], bank(4)[:, 0:CW]
                for k in range(KD):
                    mm(puv, w_own[:, k, C_UV + cc * 128:C_UV + (cc + 1) * 128], hT[:, k, :], k == 0, k == KD - 1)
                for k in range(KD):
                    mm(pug, w_own[:, k, C_UG + cc * 128:C_UG + (cc + 1) * 128], hT[:, k, :], k == 0, k == KD - 1)
                act(sg, pug, AF.Sigmoid)
                tt("vector", a_bf[:, cc, :], puv, sg, ALU.mult)
                tt("gpsimd", a_bf[:, cc, 0:HALO], a_bf[:, cc, 0:HALO], hv[:, lc * HALO:(lc + 1) * HALO], ALU.mult)
                pconv = bank(5)[:, 0:256]
                for kk in range(31):
                    mm(pconv, diag[:, cc * 31 + kk, :], a_bf[:, cc, 2 + kk:2 + kk + 256], kk == 0, kk == 30)
                act(y_f[:, cc, :], pconv, AF.Identity, bias=convb[:, cc:cc + 1])
                cp("vector", y_b[:, cc, :], y_f[:, cc, :])
                tt("gpsimd", ysq[:, cc, :], y_f[:, cc, :], y_f[:, cc, :], ALU.mult)
            pmean, pmsq = bank(6)[:, 0:256], bank(7)[:, 0:256]
            for cc in range(4):
                mm(pmean, ones_b, y_b[:, cc, :], cc == 0, cc == 3)
            for cc in range(4):
                mm(pmsq, ones_b, ysq[:, cc, :], cc == 0, cc == 3)
            act(mean_s, pmean, AF.Copy, scale=1.0 / 512)
            tt("vector", var_s, mean_s, mean_s, ALU.mult)
            stt(var_s, pmsq, 1.0 / 512, var_s, ALU.mult, ALU.subtract)
            tsc("vector", var_s, var_s, 1e-5, None, ALU.add)
            act(var_s, var_s, AF.Sqrt)
            recip(var_s, var_s)
            for cc in range(4):
                tt("vector", yn, y_f[:, cc, :], mean_s, ALU.subtract)
                tt("vector", yn, yn, var_s, ALU.mult)
                act(actv[:, cc, :], yn, AF.Silu, scale=lng[:, cc:cc + 1], bias=lnb[:, cc:cc + 1])
            for dmc in range(KD):
                pco, pgc = bank(3)[:, 0:256], bank(4)[:, 0:256]
                for cc in range(4):
                    mm(pco, wco[:, cc, dmc * 128:(dmc + 1) * 128], actv[:, cc, :], cc == 0, cc == 3)
                for k in range(KD):
                    mm(pgc, w_own[:, k, O_GC + dmc * 128:O_GC + (dmc + 1) * 128], hT[:, k, HALO:CW], k == 0, k == KD - 1)
                act(sgate, pgc, AF.Sigmoid)
                tt("vector", cg_s[:, dmc, :], pco, sgate, ALU.mult)
            dma("scalar", cg_d[lc], cg_s.rearrange("p a b -> p (a b)"))
            for dmc in range(KD):
                pga = bank(5 + dmc % 2)[:, 0:256]
                for k in range(KD):
                    mm(pga, w_own[:, k, O_GA + dmc * 128:O_GA + (dmc + 1) * 128], hT[:, k, HALO:CW], k == 0, k == KD - 1)
                act(sga_s[:, dmc, :], pga, AF.Sigmoid)
            dma("scalar", sga_d[lc], sga_s.rearrange("p a b -> p (a b)"))
            for hp in range(4):
                pq = bank(3 + hp % 2)[:, 0:256]
                for k in range(KD):
                    mm(pq, w_own[:, k, C_Q + hp * 128:C_Q + (hp + 1) * 128], hT[:, k, HALO:CW], k == 0, k == KD - 1)
                cp("vector", qT[:, hp, lc * 256:(lc + 1) * 256], pq)
        if debug:
            dma("sync", dbg_out("qT", [128, 4 * OWN], BF16), qT.rearrange("p a b -> p (a b)"))
        A.reset(m1)
        if stop_after <= 1:
            P.emit()
            return nc, dbg

        attn = A.alloc([128, NQB, 512], BF16)
        KT = A.alloc([128, 2, S], BF16)
        V = A.alloc([128, NKB, 4, 65], BF16)
        Fneg = A.alloc([128, NKB, 8], F32)
        FrefB = A.alloc([128, NQB, 8], F32)
        m2 = A.mark()
        w_kv = A.alloc([128, KD, 1024], BF16)
        wf = A.alloc([128, KD, 128], BF16)
        refs = A.alloc([128, NKB, NQB], F32)
        fst = [A.alloc([8, 256], F32) for _ in range(2)]
        ssq2 = A.alloc([128, 2], F32)
        rstd2 = A.alloc([128, 2], F32)
        junk2 = A.alloc([128, D], BF16)
        bfc = A.alloc([8, 1], F32)
        GrefT = A.alloc([8, NQB], F32)
        Eexp = A.alloc([8, NQB, 8], F32)
        kst = [A.alloc([128, 2, 256], BF16) for _ in range(2)]
        vst = [A.alloc([128, 2, 4, 65], BF16) for _ in range(2)]
        m_alias = A.mark()
        xf = [A.alloc([128, 2, D], F32) for _ in range(2)]
        xs2 = [A.alloc([128, 2, D], BF16) for _ in range(2)]
        hT2 = [A.alloc([128, KD, 256], BF16) for _ in range(2)]
        if opts:
            mset("gpsimd", KT, 0.0)
            mset("gpsimd", V, 0.0)
            mset("gpsimd", attn, 0.0)
        dma("gpsimd", w_kv, win_v[:, :, C_K:C_K + 1024])
        mset("vector", wf, 0.0)
        dma("gpsimd", wf[:, :, 0:8], win_v[:, :, C_F:C_F + 8])
        dma("sync", refs.rearrange("p a b -> p (a b)"), refsel)
        dma("sync", bfc, bf_col)
        mset("gpsimd", V[:, :, :, 64:65], 1.0)
        for i in range(2):
            mset("gpsimd", vst[i][:, :, :, 64:65], 1.0)
        kvB_K = kvB_d[:, 0:2 * S].rearrange("p (a b) -> p a b", a=2)
        kvB_V = kvB_d[:, 2 * S:2 * S + NKB * 260].rearrange("p (a b) -> p a b", a=NKB)
        pTv = ps_all[:, 0:1024].bitcast(BF16).rearrange("p (b k t) -> p b k t", b=2, k=KD)
        NG = opts.get("g", 32)
        for gi in range(NG):
            i2 = gi % 2
            xb, xsb_, hb = xf[i2], xs2[i2], hT2[i2]
            dma("sync" if i2 == 0 else "scalar", xb, xfull[gi * 256:(gi + 1) * 256, :].rearrange("(b p) d -> p b d", p=128))
            for b in range(2):
                act(junk2, xb[:, b, :], AF.Square, accum=ssq2[:, b:b + 1])
            rms_rstd(ssq2, rstd2)
            for b in range(2):
                tsc("vector", xsb_[:, b, :], xb[:, b, :], rstd2[:, b:b + 1], None, ALU.mult)
            for b in range(2):
                for k in range(KD):
                    tr(pTv[:, b, k, :], xsb_[:, b, k * 128:(k + 1) * 128], ident_b, signal=(b == 1 and k == KD - 1))
            for k in range(KD):
                act(hb[:, k, :].rearrange("p (b t) -> p b t", b=2), pTv[:, :, k, :], AF.Identity, scale=a1col[:, k:k + 1], bias=sh1col[:, k:k + 1])
            NSK = opts.get("n", 0)
            for hp in range(4 if not (NSK & 2) else 0):
                pk = bank(2 + hp % 2)[:, 0:256]
                for k in range(KD):
                    mm(pk, w_kv[:, k, hp * 128:(hp + 1) * 128], hb[:, k, :], k == 0, k == KD - 1)
                if hp < 2:
                    cp("vector", KT[:, hp, gi * 256:(gi + 1) * 256], pk)
                else:
                    cp("vector", kst[i2][:, hp - 2, :], pk)
            if not (NSK & 2):
                dma("sync", kvB_K[:, :, gi * 256:(gi + 1) * 256], kst[i2])
            for b in range(2 if not (NSK & 4) else 0):
                pv = bank(4 + b)
                for k in range(KD):
                    mm(pv, hb[:, k, b * 128:(b + 1) * 128], w_kv[:, k, 512:1024], k == 0, k == KD - 1)
                kb = gi * 2 + b
                if not (NSK & 16):
                    cp("scalar", V[:, kb, :, 0:64], pv[:, 0:256].rearrange("p (h d) -> p h d", h=4))
                if not (NSK & 32):
                    cp("vector", vst[i2][:, b, :, 0:64], pv[:, 256:512].rearrange("p (h d) -> p h d", h=4))
            if not (NSK & 4) and not (NSK & 64):
                dma("sync", kvB_V[:, gi * 2:gi * 2 + 2, :], vst[i2].rearrange("p a b c -> p a (b c)"))
            if NSK & 8:
                continue
            pf = bank(6)[0:8, 0:256]
            pf_full = bank(6)[:, 0:256]
            for k in range(KD):
                mm(pf_full, wf[:, k, :], hb[:, k, :], k == 0, k == KD - 1)
            cp("vector", fst[i2], pf)
            dma("sync", f_d[:, gi * 256:(gi + 1) * 256], fst[i2])
        if opts.get("n", 0) & 1:
            if debug:
                dma("sync", dbg_out("KT", [128, 2 * S], BF16), KT.rearrange("p a b -> p (a b)"))
                dma("sync", dbg_out("V", [128, NKB * 260], BF16), V.rearrange("p a b c -> p (a b c)"))
            P.emit()
            return nc, dbg
        A.reset(m_alias)
        fT = A.alloc([8, S], F32)
        dma("sync", fT, f_d)
        tsc("vector", bfc, bfc, -1.0, None, ALU.mult)
        act(fT, fT, AF.Exp, scale=-1.0, bias=bfc[:, 0:1])
        act(fT, fT, AF.Ln, bias=1.0)
        P.op("vector", lambda e: e.tensor_tensor_scan(out=fT, data0=fT, data1=fT, initial=0.0, op0=ALU.add, op1=ALU.max), [fT], [fT])
        pF = bank(7)
        for kb in range(NKB):
            tr(pF[:, kb * 8:(kb + 1) * 8], fT[0:8, kb * 128:(kb + 1) * 128], ident_f[0:8, 0:8], signal=(kb == NKB - 1))
        cp("vector", Fneg.rearrange("p a b -> p (a b)"), pF)
        pG = bank(6)[0:8, 0:NQB]
        for kb in range(NKB):
            mm(pG, Fneg[:, kb, :], refs[:, kb, :], kb == 0, kb == NKB - 1)
        cp("vector", GrefT, pG)
        tt("vector", Eexp, GrefT.unsqueeze(2).to_broadcast([8, NQB, 8]), ident_f[0:8, 0:8].unsqueeze(1).to_broadcast([8, NQB, 8]), ALU.mult)
        pB = bank(5)[:, 0:128]
        mm(pB, ones_f[0:8, :], Eexp.rearrange("p a b -> p (a b)"), True, True)
        cp("vector", FrefB.rearrange("p a b -> p (a b)"), pB)
        if debug:
            dma("sync", dbg_out("KT", [128, 2 * S], BF16), KT.rearrange("p a b -> p (a b)"))
            dma("sync", dbg_out("V", [128, NKB * 260], BF16), V.rearrange("p a b c -> p (a b c)"))
            dma("sync", dbg_out("G", [128, NKB * 8]), Fneg.rearrange("p a b -> p (a b)"))
            dma("sync", dbg_out("Gref", [128, NQB * 8]), FrefB.rearrange("p a b -> p (a b)"))
        A.reset(m2)
        if stop_after <= 2:
            P.emit()
            return nc, dbg

        qpos = A.alloc([128, OWN], F32)
        kpos = A.alloc([128, NKB], F32)
        biasq = [A.alloc([128, NKB, 4], F32) for _ in range(2)]
        masks = [A.alloc([128, 8, 128], BF16) for _ in range(2)]
        Pt = A.alloc([128, 8, 128], BF16)
        rden = A.alloc([128, 4], F32)
        dma("sync", qpos, qposB)
        dma("sync", kpos, kposc)
        ztile = A.alloc([128, 2, D], BF16)
        ztile_f = A.alloc([128, 2, D], F32)
        mset("gpsimd", ztile, 0.0)
        mset("gpsimd", ztile_f, 0.0)
        nrows = NE * cap + 128
        for r0 in range(0, nrows, 256):
            nr = min(256, nrows - r0)
            dma("sync", xs_d[r0:r0 + nr, :].rearrange("(r p) d -> p r d", p=128), ztile[:, 0:nr // 128, :])
        Sps = ps_all[:, 0:2048].rearrange("p (s t) -> p s t", s=4)[:, :, 0:128]
        NQR = opts.get("q", NQB)
        for half in range(2):
            if half == 1:
                dma("sync", KT, kvB_K)
                dma("scalar", V.rearrange("p a b c -> p a (b c)"), kvB_V)
                for r0 in range(0, nrows, 256):
                    nr = min(256, nrows - r0)
                    dma("sync", ys_d[r0:r0 + nr, :].rearrange("(r p) d -> p r d", p=128), ztile_f[:, 0:nr // 128, :])
            tile_no = 0
            for qb in range(NQR):
                lc = qb // 2
                nkb, ws = kb_end(lc), win_start(lc)
                bq, mk = biasq[qb % 2], masks[qb % 2]
                tt("vector", bq, Fneg[:, :, half * 4:(half + 1) * 4], FrefB[:, qb, half * 4:(half + 1) * 4].unsqueeze(1).to_broadcast([128, NKB, 4]), ALU.subtract)
                tsc("vector", bq, bq, 0.0, None, ALU.min)
                for w in range(8):
                    tsc("gpsimd", mk[:, w, :], qpos[:, qb * 128:(qb + 1) * 128], kpos[:, ws + w:ws + w + 1], None, ALU.is_ge)
                tiles = [(hl, kb) for hl in range(4) for kb in range(nkb)]
                LA = 3

                def emit_S(i, tiles=tiles, qb=qb, half=half, tile_no=tile_no):
                    hl, kb = tiles[i]
                    n = tile_no + i
                    prt = (hl % 2) * 64
                    hpl = hl // 2
                    mm(Sps[:, n % 4, :], KT[prt:prt + 64, hpl, kb * 128:(kb + 1) * 128],
                       qT[prt:prt + 64, half * 2 + hpl, qb * 128:(qb + 1) * 128], True, True)

                for i in range(min(LA, len(tiles))):
                    emit_S(i)
                for i, (hl, kb) in enumerate(tiles):
                    n = tile_no + i
                    if i + LA < len(tiles):
                        emit_S(i + LA)
                    Ob = bank(4 + (hl % 2) + 2 * (qb % 2))
                    act(Pt[:, n % 8, :], Sps[:, n % 4, :], AF.Exp, scale=0.125, bias=bq[:, kb, hl:hl + 1])
                    if kb >= ws:
                        tt("gpsimd", Pt[:, n % 8, :], Pt[:, n % 8, :], mk[:, kb - ws, :], ALU.mult)
                    mm(Ob[:, 0:65], Pt[:, n % 8, :], V[:, kb, hl, :], kb == 0, kb == nkb - 1, signal=True)
                    if kb == nkb - 1:
                        h = half * 4 + hl
                        recip(rden[:, hl:hl + 1], Ob[:, 64:65])
                        tsc("vector", attn[:, qb, h * 64:(h + 1) * 64], Ob[:, 0:64], rden[:, hl:hl + 1], None, ALU.mult)
                tile_no += len(tiles)
        if debug:
            dma("sync", dbg_out("attn", [128, NQB * 512], BF16), attn.rearrange("p a b -> p (a b)"))
        A.reset(m2)
        if stop_after <= 3:
            P.emit()
            return nc, dbg

        wao = A.alloc([128, 4, D], BF16)
        wout = A.alloc([128, KD, D], BF16)
        wr = A.alloc([128, KD, NE], F32)
        brp = A.alloc([128, NE], F32)
        ecp = A.alloc([128, NE], F32)
        maskb = A.alloc([128, NQB, NE], BF16)
        sga_b = [A.alloc([128, KD, 128], BF16) for _ in range(2)]
        cg_b = [A.alloc([128, KD, 128], BF16) for _ in range(2)]
        xblk = [A.alloc([128, D], F32) for _ in range(2)]
        attnT = A.alloc([128, 4, 128], BF16)
        tmpf = A.alloc([128, D], F32)
        mixT = A.alloc([128, KD, 128], BF16)
        x1 = [A.alloc([128, D], F32) for _ in range(2)]
        h2f = A.alloc([128, D], F32)
        h2b = [A.alloc([128, D], BF16) for _ in range(2)]
        h2T = A.alloc([128, KD, 128], F32)
        junk3 = A.alloc([128, D], BF16)
        ssq3 = A.alloc([128, 1], F32)
        rstd3 = A.alloc([128, 1], F32)
        lg = A.alloc([128, NE], F32)
        top8 = A.alloc([128, 8], F32)
        msk = A.alloc([128, NE], F32)
        negm = A.alloc([128, 1], F32)
        ex = A.alloc([128, NE], F32)
        ssum = A.alloc([128, 1], F32)
        g32 = A.alloc([128, NE], F32)
        destf = A.alloc([128, NE], F32)
        oh = A.alloc([128, NE], F32)
        jk32 = A.alloc([128, NE], F32)
        dkf = A.alloc([128, 4], F32)
        dma("gpsimd", wao, w_ao.rearrange("(k p) n -> p k n", p=128))
        dma("gpsimd", wout, w_out.rearrange("(k p) n -> p k n", p=128))
        dma("sync", wr, w_router.rearrange("(k p) n -> p k n", p=128))
        dma("sync", brp, br_rep)
        dma("sync", ecp, ecap)
        for qb in range(NQR):
            lc, off = qb // 2, (qb % 2) * 128
            i2 = qb % 2
            dma("sync", sga_b[i2], sga_d[lc].rearrange("p (a b) -> p a b", a=KD)[:, :, off:off + 128])
            dma("sync", cg_b[i2], cg_d[lc].rearrange("p (a b) -> p a b", a=KD)[:, :, off:off + 128])
            dma("scalar", xblk[i2], xown[lc, HALO + off:HALO + off + 128, :])
            pAT = bank(0).bitcast(BF16)[:, 0:512].rearrange("p (c t) -> p c t", c=4)
            for c4 in range(4):
                tr(pAT[:, c4, :], attn[:, qb, c4 * 128:(c4 + 1) * 128], ident_b, signal=(c4 == 3))
            cp("vector", attnT, pAT)
            pAO = ps_all[:, 512:1536]
            for dmc in range(KD):
                for c4 in range(4):
                    mm(pAO[:, dmc * 128:(dmc + 1) * 128], wao[:, c4, dmc * 128:(dmc + 1) * 128], attnT[:, c4, :], c4 == 0, c4 == 3)
            tt("vector", tmpf, pAO, sga_b[i2].rearrange("p a b -> p (a b)"), ALU.mult)
            tt("gpsimd", mixT.rearrange("p a b -> p (a b)"), tmpf, cg_b[i2].rearrange("p a b -> p (a b)"), ALU.add)
            pD = ps_all[:, 1536:2560]
            for nh in range(2):
                for dk in range(KD):
                    mm(pD[:, nh * 512:(nh + 1) * 512], mixT[:, dk, :], wout[:, dk, nh * 512:(nh + 1) * 512], dk == 0, dk == KD - 1)
            x1b = x1[i2]
            tt("vector", tmpf, pD, modrow[:, 0, :], ALU.mult)
            tt("gpsimd", x1b, tmpf, xblk[i2], ALU.add)
            dma("scalar", x1_d[qb], x1b)
            act(junk3, x1b, AF.Square, accum=ssq3)
            rms_rstd(ssq3, rstd3)
            stt(h2f, x1b, rstd3[:, 0:1], a2row, ALU.mult, ALU.mult)
            tt("vector", h2f, h2f, modrow[:, 1, :], ALU.add)
            cp("scalar", h2b[i2], h2f)
            pHT = ps_all[:, 2560:3584].rearrange("p (k t) -> p k t", k=KD)
            for k in range(KD):
                tr(pHT[:, k, :], h2f[:, k * 128:(k + 1) * 128], ident_f, signal=(k == KD - 1))
            cp("vector", h2T.rearrange("p a b -> p (a b)"), ps_all[:, 2560:3584])
            plog = bank(7)[:, 0:NE]
            for k in range(KD):
                mm(plog, h2T[:, k, :], wr[:, k, :], k == 0, k == KD - 1)
            tt("vector", lg, plog, brp, ALU.add)
            P.op("vector", lambda e: e.max(out=top8, in_=lg), [lg], [top8])
            tsc("vector", msk, lg, top8[:, 3:4], None, ALU.is_ge)
            cp("vector", maskb[:, qb, :], msk)
            tsc("vector", negm, top8[:, 0:1], -1.0, None, ALU.mult)
            act(ex, lg, AF.Exp, bias=negm[:, 0:1])
            ttr(ex, ex, msk, ssum)
            recip(ssum, ssum)
            tsc("vector", g32, ex, ssum[:, 0:1], None, ALU.mult)
            ppos = bank(7)[:, 64:64 + NE]
            for b2 in range(qb + 1):
                mm(ppos, ones_b if b2 < qb else ustrict, maskb[:, b2, :], b2 == 0, b2 == qb)
            tt("vector", destf, ppos, ecp, ALU.add)
            for k in range(4):
                tsc("vector", oh, lg, top8[:, k:k + 1], None, ALU.is_equal)
                ttr(jk32, oh, g32, gates[:, qb, k:k + 1])
                ttr(jk32, oh, destf, dkf[:, k:k + 1])
            cp("vector", dests[:, qb, :], dkf)
            for k in range(4):
                P.dma("gpsimd", lambda e, qb=qb, k=k, i2=i2: e.indirect_dma_start(
                    out=xs_d[:, :], out_offset=bass.IndirectOffsetOnAxis(ap=dests[:, qb, k:k + 1], axis=0),
                    in_=h2b[i2], in_offset=None), [h2b[i2], dests[:, qb, k:k + 1]], [xs_d[:, :]])
        pcnt = bank(7)[:, 128:128 + NE]
        for b2 in range(NQR):
            mm(pcnt, ones_b, maskb[:, b2, :], b2 == 0, b2 == NQR - 1)
        t_cnt = cp("vector", cnt_i, pcnt)
        if debug:
            dma("sync", dbg_out("cnt", [128, NE], I32), cnt_i)
            dma("sync", dbg_out("lg", [128, NE]), lg)
            dma("sync", dbg_out("top8", [128, 8]), top8)
            dma("sync", dbg_out("h2f", [128, D]), h2f)
            dma("sync", dbg_out("gates", [128, NQB * 4]), gates.rearrange("p a b -> p (a b)"))
            dma("sync", dbg_out("dests", [128, NQB * 4], I32), dests.rearrange("p a b -> p (a b)"))
            d_x1 = dbg_out("x1", [NQB, 128, D])
            for qb in range(NQR):
                dma("sync", xblk[0], x1_d[qb])
                dma("sync", d_x1[qb], xblk[0])
        A.reset(A_BASE)
        if stop_after <= 4:
            P.emit()
            return nc, dbg

        W1 = [A.alloc([128, KD, 2 * D], BF16) for _ in range(2)]
        W2 = [A.alloc([128, KD, D], BF16) for _ in range(2)]
        b1s = A.alloc([128, NE * 16], F32)
        b2r = [A.alloc([128, D], F32) for _ in range(2)]
        xbk = [A.alloc([128, D], BF16) for _ in range(2)]
        xT = [A.alloc([128, KD, 128], BF16) for _ in range(2)]
        gm = [A.alloc([128, 512], F32) for _ in range(2)]
        sgm = [A.alloc([128, 512], F32) for _ in range(2)]
        uc = [A.alloc([128, 512], F32) for _ in range(2)]
        hmT = [A.alloc([128, 8, 128], BF16) for _ in range(2)]
        yb = [A.alloc([128, D], F32) for _ in range(2)]
        dma("sync", b1s, b1c)
        NEr = opts.get("e", NE)
        stg = [A.alloc([128, KD, 256], F32) for _ in range(4)]
        stg_i = [0]

        def expert_chunks(ei):
            wb1_, wb2_ = W1[ei % 2], W2[ei % 2]
            w1v = w_e1[ei].rearrange("(k p) n -> p k n", p=128)
            w2v = w_e2[ei].rearrange("(k p) n -> p k n", p=128)
            ch = []
            for c in range(12):
                if c < 8:
                    ch.append((w1v[:, :, c * 256:(c + 1) * 256], wb1_[:, :, c * 256:(c + 1) * 256]))
                else:
                    ch.append((w2v[:, :, (c - 8) * 256:(c - 7) * 256], wb2_[:, :, (c - 8) * 256:(c - 7) * 256]))
            return ch

        def w_dma(ei, c):
            src, dst = expert_chunks(ei)[c]
            dma("sync", stg[(ei * 8 + c) % 4], src)

        def w_cast(ei, c):
            src, dst = expert_chunks(ei)[c]
            cp("scalar", dst, stg[(ei * 8 + c) % 4])

        NCHK = 8

        def w2_dma(ei):
            dma("gpsimd", W2[ei % 2], w_e2[ei].rearrange("(k p) n -> p k n", p=128))

        dma("sync", b2r[0], b_e2[0:1, :].to_broadcast([128, D]))
        w2_dma(0)
        for c in range(4):
            w_dma(0, c)
        for c in range(NCHK):
            w_cast(0, c)
            if c + 4 < NCHK:
                w_dma(0, c + 4)
        for e_ in range(NEr):
            wb1, wb2 = W1[e_ % 2], W2[e_ % 2]
            nxt = e_ + 1 if e_ + 1 < NEr else None
            if nxt is not None:
                dma("sync", b2r[nxt % 2], b_e2[nxt:nxt + 1, :].to_broadcast([128, D]))
                w2_dma(nxt)
                for c in range(4):
                    w_dma(nxt, c)
            P.regload(e_, [t_cnt])
            dma("scalar", xbk[0], xs_d[e_ * cap:e_ * cap + 128, :])

            def st_T(kb_):
                P.guard = (e_, kb_ * 128)
                xk = xbk[kb_ % 2]
                xTk = xT[kb_ % 2]
                pXT = bank(0).bitcast(BF16).rearrange("p (k t) -> p k t", k=KD)
                for k in range(KD):
                    tr(pXT[:, k, :], xk[:, k * 128:(k + 1) * 128], ident_b, signal=(k == KD - 1))
                cp("vector", xTk.rearrange("p a b -> p (a b)"), bank(0).bitcast(BF16))
                P.guard = None

            def st_H(kb_, half2):
                P.guard = (e_, kb_ * 128)
                xTk = xT[kb_ % 2]
                gm_, uc_, sgm_ = gm[half2], uc[half2], sgm[half2]
                pg, pu = bank(1 + half2), bank(3 + half2)
                for j4 in range(4):
                    fc = half2 * 4 + j4
                    for k in range(KD):
                        mm(pg[:, j4 * 128:(j4 + 1) * 128], wb1[:, k, fc * 128:(fc + 1) * 128], xTk[:, k, :], k == 0, k == KD - 1)
                for j4 in range(4):
                    fc = 8 + half2 * 4 + j4
                    for k in range(KD):
                        mm(pu[:, j4 * 128:(j4 + 1) * 128], wb1[:, k, fc * 128:(fc + 1) * 128], xTk[:, k, :], k == 0, k == KD - 1)
                for j4 in range(4):
                    fcg = half2 * 4 + j4
                    tsc("vector", gm_[:, j4 * 128:(j4 + 1) * 128], pg[:, j4 * 128:(j4 + 1) * 128], b1s[:, e_ * 16 + fcg:e_ * 16 + fcg + 1], 7.0, ALU.add, ALU.min)
                    tsc("vector", uc_[:, j4 * 128:(j4 + 1) * 128], pu[:, j4 * 128:(j4 + 1) * 128], b1s[:, e_ * 16 + 8 + fcg:e_ * 16 + 8 + fcg + 1], 7.0, ALU.add, ALU.min)
                act(sgm_, gm_, AF.Silu, scale=1.702)
                tsc("vector", uc_, uc_, -7.0, 1.0, ALU.max, ALU.add)
                stt(hmT[kb_ % 2][:, half2 * 4:(half2 + 1) * 4, :].rearrange("p a b -> p (a b)"), uc_, 1.0 / 1.702, sgm_, ALU.mult, ALU.mult)
                P.guard = None

            def st_Y(kb_):
                P.guard = (e_, kb_ * 128)
                row0 = e_ * cap + kb_ * 128
                pY = ps_all[:, 2560:3584]
                for nh in range(2):
                    for fc in range(8):
                        mm(pY[:, nh * 512:(nh + 1) * 512], hmT[kb_ % 2][:, fc, :], wb2[:, fc, nh * 512:(nh + 1) * 512], fc == 0, fc == 7)
                ybk = yb[kb_ % 2]
                tt("vector", ybk, pY, b2r[e_ % 2], ALU.add)
                dma("scalar", ys_d[row0:row0 + 128, :], ybk)
                P.guard = None

            def st_prefetch(kb_):
                if kb_ < kmax:
                    P.guard = (e_, kb_ * 128)
                    dma("scalar", xbk[kb_ % 2], xs_d[e_ * cap + kb_ * 128:e_ * cap + kb_ * 128 + 128, :])
                    P.guard = None

            st_prefetch(1)
            st_T(0)
            st_H(0, 0)
            for kblk in range(kmax):
                st_H(kblk, 1)
                if kblk + 1 < kmax:
                    st_prefetch(kblk + 2)
                    st_T(kblk + 1)
                    st_H(kblk + 1, 0)
                st_Y(kblk)
                if nxt is not None:
                    for c in (3 * kblk, 3 * kblk + 1, 3 * kblk + 2):
                        if c < NCHK:
                            w_cast(nxt, c)
                            if c + 4 < NCHK:
                                w_dma(nxt, c + 4)
        A.reset(A_BASE)
        if stop_after <= 5:
            P.emit()
            return nc, dbg

        yk = [A.alloc([128, D], F32) for _ in range(4)]
        acc = A.alloc([128, D], F32)
        x1r = [A.alloc([128, D], F32) for _ in range(2)]
        finr = A.alloc([128, D], F32)
        junk5 = A.alloc([128, D], BF16)
        ssq5 = A.alloc([128, 1], F32)
        rstd5 = A.alloc([128, 1], F32)
        ob = [A.alloc([128, D], F32) for _ in range(2)]
        dma("sync", finr, fin_rep)
        for qb in range(NQR):
            i2 = qb % 2
            dma("sync", x1r[i2], x1_d[qb])
            for k in range(4):
                P.dma("gpsimd", lambda e, qb=qb, k=k: e.indirect_dma_start(
                    out=yk[k], out_offset=None, in_=ys_d[:, :],
                    in_offset=bass.IndirectOffsetOnAxis(ap=dests[:, qb, k:k + 1], axis=0)), [ys_d[:, :], dests[:, qb, k:k + 1]], [yk[k]])
            tsc("vector", acc, yk[0], gates[:, qb, 0:1], None, ALU.mult)
            for k in range(1, 4):
                stt(acc, yk[k], gates[:, qb, k:k + 1], acc, ALU.mult, ALU.add)
            tt("gpsimd", acc, acc, modrow[:, 3, :], ALU.mult)
            tt("gpsimd", acc, acc, x1r[i2], ALU.add)
            act(junk5, acc, AF.Square, accum=ssq5)
            rms_rstd(ssq5, rstd5)
            stt(ob[i2], acc, rstd5[:, 0:1], finr, ALU.mult, ALU.mult)
            dma("sync", out[qb * 128:(qb + 1) * 128, :], ob[i2])
        P.emit()
    return nc, dbg


def host_inputs(inp, kmax=KMAX):
    cap = kmax * 128
    f = lambda a: np.ascontiguousarray(a, dtype=np.float32)
    x = inp["x"]
    col = lambda v: f(v.reshape(-1, 128).T)
    rep = lambda v: f(np.broadcast_to(v.reshape(1, -1), (128, v.size)))
    shared = {
        "w_ada": f(inp["w_ada"][0]),
        "b_ada_col": col(inp["b_ada"][0]),
        "n1g_col": col(inp["norm1_g"][0]),
        "n2g_rep": rep(inp["norm2_g"][0]),
        "fin_rep": rep(inp["final_g"]),
        "w_in": f(inp["w_in"][0]),
        "bf_col": f(inp["b_forget"][0].reshape(8, 1)),
        "cwT": f(inp["conv_w"][0].T.reshape(4, 128, 31).transpose(1, 0, 2).reshape(128, 124)),
        "cvecs": f(np.concatenate([col(inp["conv_b"][0]), col(inp["conv_ln_g"][0]), col(inp["conv_ln_b"][0])], axis=1)),
        "w_co": f(inp["w_conv_out"][0]),
        "w_ao": f(inp["w_attn_out"][0]),
        "w_out": f(inp["w_out"][0]),
        "w_router": f(inp["w_router"][0]),
        "br_rep": rep(inp["b_router"][0]),
        "w_e1": f(inp["w_exp_in"][0]),
        "b1c": f(inp["b_exp_in"][0].reshape(NE, 16, 128).transpose(2, 0, 1).reshape(128, NE * 16)),
        "w_e2": f(inp["w_exp_out"][0]),
        "b_e2": f(inp["b_exp_out"][0]),
        "kposc": f((np.arange(NKB)[None, :] * 128 + np.arange(128)[:, None])),
        "ecap": rep(np.arange(NE, dtype=np.float32) * cap),
    }
    maps = []
    poss = []
    for c in range(NCORES):
        b, j = c // 4, c % 4
        ch = chunks_of(j)
        xo = np.zeros((NCH, CW, D), np.float32)
        hv = np.zeros((128, NCH * HALO), np.float32)
        pos = np.zeros(OWN, np.int64)
        for lc, m in enumerate(ch):
            s0 = m * 256
            xo[lc, HALO:] = x[b, s0:s0 + 256]
            if m > 0:
                xo[lc, :HALO] = x[b, s0 - HALO:s0]
                hv[:, lc * HALO:(lc + 1) * HALO] = 1.0
            pos[lc * 256:(lc + 1) * 256] = np.arange(s0, s0 + 256)
        refs = np.zeros((128, NKB, NQB), np.float32)
        for qb in range(NQB):
            pl = pos[qb * 128 + 127]
            refs[pl % 128, pl // 128, qb] = 1.0
        d = dict(shared)
        d.update({
            "xfull": f(x[b]), "xown": xo, "halov": hv, "cvec": col(inp["c"][b]),
            "qposB": rep(pos.astype(np.float32)), "refsel": refs.reshape(128, NKB * NQB),
        })
        maps.append(d)
        poss.append((b, pos))
    return maps, poss


def kernel(**inp):
    inp = {k: np.asarray(v) for k, v in inp.items()}
    nc, _ = build()
    maps, poss = host_inputs(inp)
    res = run_bass_kernel_spmd(nc, maps, core_ids=list(range(NCORES)))
    out = np.zeros((2, S, D), np.float32)
    for c in range(NCORES):
        b, pos = poss[c]
        out[b, pos] = res.results[c]["out"]
    return out
```

```python
import numpy as np
from contextlib import ExitStack
import concourse.bass as bass
import concourse.mybir as mybir
from concourse.bass_utils import run_bass_kernel_spmd

F32 = mybir.dt.float32
BF16 = mybir.dt.bfloat16
I32 = mybir.dt.int32
U32 = mybir.dt.uint32
AF = mybir.ActivationFunctionType
ALU = mybir.AluOpType

ENGS = ("sync", "scalar", "vector", "gpsimd", "tensor")
NCORES = 8
D = 1024
KD = 8
S = 8192
NKB = 64
OWN = 2048
NCH = 8
NQB = 16
HALO = 32
CW = 256 + HALO
NE = 32
KMAX = 7
C_UV, C_UG, C_Q, C_K, C_V, C_F, C_GC, C_GA = 0, 512, 1024, 1536, 2048, 2560, 2568, 3592
_DT_SIZE = {F32: 4, BF16: 2, I32: 4, U32: 4}
SEM_ROT = 2000
NPOOL = 24
NSW = 12


class Tok:
    __slots__ = ("sem", "val", "gen")

    def __init__(self, sem=None, val=None, gen=0):
        self.sem = sem
        self.val = val
        self.gen = gen


class Rec:
    __slots__ = ("p0", "p1", "lo", "hi", "w", "tok", "eng", "alive", "name")


def region(ap):
    es = _DT_SIZE[ap.dtype]
    pairs = ap.ap
    off = ap.offset
    sp = str(ap.space)
    if sp in ("SB", "PSUM"):
        pstride, npart = pairs[0]
        if pstride <= 0:
            pstride = 1 << 40
        p0 = off // pstride
        col = off % pstride
        ext = 1 + sum((c - 1) * abs(s) for s, c in pairs[1:])
        return (ap.name, p0, p0 + npart, col * es, (col + ext) * es, 4096)
    ext = 1 + sum((c - 1) * abs(s) for s, c in pairs)
    return (ap.name, 0, 1, off * es, (off + ext) * es, 1 << 20)


class Prog:
    def __init__(self, nc, es):
        self.nc = nc
        self.es = es
        self.streams = {e: [] for e in ENGS}
        self.esem = {}
        self.ecount = {}
        self.nsem = 0
        self.pending = {e: [] for e in ENGS}
        self.bins = {}
        self.pool = []
        self.pool_last = []
        self.pool_i = 0
        self.dma_uid = 0
        self.noguard = 0
        self.guard = None
        self.fake = {}
        self.cnt_ap = None
        for e in ENGS:
            self._new_esem(e)
        self.qpool = {q: [self._mksem(f"dq_{q}{i}") for i in range(n)] for q, n in (("sync", 16), ("scalar", 12))}
        self.qlast = {q: [None] * len(v) for q, v in self.qpool.items()}
        self.qi = {q: 0 for q in self.qpool}
        self.swpool = [self._mksem(f"sq{i}") for i in range(NSW)]
        self.sw_last = [None] * NSW
        self.sw_i = 0

    def _mksem(self, name):
        self.nsem += 1
        return self.es.enter_context(self.nc.semaphore(name))

    def _new_esem(self, e):
        self.esem[e] = self._mksem(f"s_{e}_{self.nsem}")
        self.ecount[e] = 0

    def _bins(self, reg):
        name, p0, p1, lo, hi, bs = reg
        return [(name, b) for b in range(lo // bs, (hi - 1) // bs + 1)]

    def _query(self, reg):
        name, p0, p1, lo, hi, bs = reg
        seen = set()
        out = []
        for key in self._bins(reg):
            lst = self.bins.get(key)
            if not lst:
                continue
            dead = 0
            for r in lst:
                if not r.alive:
                    dead += 1
                    continue
                if id(r) in seen:
                    continue
                if r.lo < hi and lo < r.hi and r.p0 < p1 and p0 < r.p1:
                    seen.add(id(r))
                    out.append(r)
            if dead > 16:
                self.bins[key] = [r for r in lst if r.alive]
        return out

    def _add(self, reg, w, tok, eng):
        name, p0, p1, lo, hi, bs = reg
        r = Rec()
        r.p0, r.p1, r.lo, r.hi, r.w, r.tok, r.eng, r.alive, r.name = p0, p1, lo, hi, w, tok, eng, True, name
        for key in self._bins(reg):
            self.bins.setdefault(key, []).append(r)

    def _deps(self, eng, reads, writes, tok, engkey):
        deps = []
        rregs = [a if isinstance(a, tuple) else region(a) for a in reads]
        wregs = [a if isinstance(a, tuple) else region(a) for a in writes]
        for reg in rregs:
            for r in self._query(reg):
                if r.w and not (eng == "tensor" and r.eng == "tensor"):
                    deps.append(r.tok)
            if reg[0] == "parena":
                name, p0, p1, lo, hi, bs = reg
                breg = (name, 0, 128, (lo // 2048) * 2048, ((hi + 2047) // 2048) * 2048, bs)
                for r in self._query(breg):
                    if (not r.w) and r.eng != engkey and r.eng != "tensor":
                        deps.append(r.tok)
        for reg in wregs:
            for r in self._query(reg):
                if not (eng == "tensor" and r.eng == "tensor"):
                    deps.append(r.tok)
        for reg in rregs:
            name, p0, p1, lo, hi, bs = reg
            rep = False
            for r in self._query(reg):
                if (not r.w) and r.eng == engkey and r.p0 == p0 and r.p1 == p1 and r.lo == lo and r.hi == hi:
                    r.tok = tok
                    rep = True
                    break
            if not rep:
                self._add(reg, False, tok, engkey)
        for reg in wregs:
            name, p0, p1, lo, hi, bs = reg
            for r in self._query(reg):
                if r.p0 >= p0 and r.p1 <= p1 and r.lo >= lo and r.hi <= hi:
                    r.alive = False
            self._add(reg, True, tok, engkey)
        return deps

    def op(self, eng, fn, r=(), w=(), signal=True):
        if signal:
            if self.ecount[eng] >= SEM_ROT:
                self._new_esem(eng)
            self.ecount[eng] += 1
            tok = Tok(self.esem[eng], self.ecount[eng])
            for p in self.pending[eng]:
                p.sem, p.val = tok.sem, tok.val
            self.pending[eng] = []
        else:
            tok = Tok()
            self.pending[eng].append(tok)
        deps = self._deps(eng, r, w, tok, eng)
        self.streams[eng].append((fn, deps, tok if signal else None, 1, self.guard))
        return tok

    def regload(self, key, deps):
        for e in ENGS:
            self.streams[e].append((("regload", key), list(deps), None, 0, None))

    def dma(self, eng, fn, r=(), w=()):
        if eng == "gpsimd":
            i = self.sw_i
            self.sw_i = (self.sw_i + 1) % NSW
            prev = self.sw_last[i]
            val = (prev.val if prev is not None else 0) + 16
            tok = Tok(self.swpool[i], val)
            self.sw_last[i] = tok
            self.dma_uid += 1
            deps = self._deps(eng, r, w, tok, "dma%d" % self.dma_uid)
            if prev is not None:
                deps.append(prev)
            self.streams[eng].append((fn, deps, tok, 16, self.guard))
            return tok
        pool, last = self.qpool[eng], self.qlast[eng]
        i = self.qi[eng]
        self.qi[eng] = (i + 1) % len(pool)
        prev = last[i]
        val = (prev.val if prev is not None else 0) + 16
        tok = Tok(pool[i], val)
        last[i] = tok
        self.dma_uid += 1
        deps = self._deps(eng, r, w, tok, "dma%d" % self.dma_uid)
        if prev is not None:
            deps.append(prev)
        self.streams[eng].append((fn, deps, tok, 16, self.guard))
        return tok

    def emit(self):
        nc = self.nc
        final = [t for q in self.qlast for t in self.qlast[q] if t is not None] + [t for t in self.sw_last if t is not None]
        with nc.Block() as block:
            for e in ENGS:
                stream = self.streams[e]
                fw = final if e == "sync" else []

                def body(engine, stream=stream, fw=fw, ename=e):
                    waited = {}
                    lastfake = [None]
                    reg = engine.alloc_register("gcnt")

                    def dowait(d):
                        assert d.sem is not None, "unresolved token"
                        k = (id(d.sem), d.gen)
                        if waited.get(k, 0) >= d.val:
                            return
                        waited[k] = d.val
                        engine.wait_ge(d.sem, d.val)

                    def emit_one(ent):
                        fn, deps, tok, inc, g = ent
                        for d in deps:
                            dowait(d)
                        if isinstance(fn, tuple):
                            if self.noguard != 2:
                                engine.reg_load(reg, self.cnt_ap(fn[1]))
                            return
                        ins = fn(engine)
                        if tok is not None:
                            ins.then_inc(tok.sem, inc)

                    i = 0
                    n = len(stream)
                    while i < n:
                        g = stream[i][4]
                        if g is None or self.noguard:
                            emit_one(stream[i])
                            i += 1
                            continue
                        j = i
                        while j < n and stream[j][4] == g:
                            j += 1
                        grp = stream[i:j]
                        saved = dict(waited)
                        with engine.If_lt(reg, g[1] + 1):
                            for fn, deps, tok, inc, _ in grp:
                                for d in deps:
                                    dowait(d)
                                if tok is None:
                                    continue
                                if inc == 16:
                                    engine.sem_inc(tok.sem, 16)
                                else:
                                    if ename == "scalar" and lastfake[0] is not None:
                                        engine.wait_ge(lastfake[0].sem, lastfake[0].val)
                                    self.fake[ename](engine).then_inc(tok.sem, 1)
                                    lastfake[0] = tok
                        waited.clear()
                        waited.update(saved)
                        with engine.Else():
                            for ent in grp:
                                emit_one(ent)
                        waited.clear()
                        waited.update(saved)
                        i = j
                    for d in fw:
                        dowait(d)

                getattr(block, e)(body)


class Arena:
    def __init__(self, ap, nwords):
        self.ap = ap
        self.n = nwords
        self.off = 0

    def mark(self):
        return self.off

    def reset(self, off=0):
        self.off = off

    def alloc(self, shape, dt):
        free = int(np.prod(shape[1:]))
        words = (free * _DT_SIZE[dt] + 3) // 4
        words += words % 2
        assert self.off + words <= self.n, ("arena overflow", self.off, words, self.n)
        v = self.ap[0:shape[0], self.off:self.off + words]
        self.off += words
        if dt != F32:
            v = v.bitcast(dt)
        v = v[:, 0:free]
        if len(shape) > 2:
            names = [f"a{i}" for i in range(len(shape) - 1)]
            pat = "p (" + " ".join(names) + ") -> p " + " ".join(names)
            v = v.rearrange(pat, **{n: s for n, s in zip(names[:-1], shape[1:-1])})
        return v


def chunks_of(j):
    ch = []
    for g in range(4):
        ch += [8 * g + j, 8 * g + 7 - j]
    return ch


def kb_end(lc):
    g, s = lc // 2, lc % 2
    return 16 * g + 8 * s + 8


def win_start(lc):
    g, s = lc // 2, lc % 2
    return 16 * g + 8 * s


def isap(x):
    return hasattr(x, "ap") and hasattr(x, "dtype") and hasattr(x, "offset")


def build(debug=False, kmax=KMAX, stop_after=99, variant=''):
    cap = kmax * 128
    opts = {}
    for vv in variant.split("_"):
        if len(vv) > 1 and vv[1:].isdigit():
            opts[vv[0]] = int(vv[1:])
    nc = bass.Bass("TRN2", target_bir_lowering=False)
    dram_in = lambda n, shp, dt=F32: nc.dram_tensor(n, list(shp), dt, kind="ExternalInput").ap()
    xfull = dram_in("xfull", [S, D])
    xown = dram_in("xown", [NCH, CW, D])
    halov = dram_in("halov", [128, NCH * HALO])
    cvec = dram_in("cvec", [128, KD])
    w_ada = dram_in("w_ada", [D, 6 * D])
    b_ada_col = dram_in("b_ada_col", [128, 48])
    n1g_col = dram_in("n1g_col", [128, KD])
    n2g_rep = dram_in("n2g_rep", [128, D])
    fin_rep = dram_in("fin_rep", [128, D])
    w_in = dram_in("w_in", [D, 4616])
    bf_col = dram_in("bf_col", [8, 1])
    cwT = dram_in("cwT", [128, 4 * 31])
    cvecs = dram_in("cvecs", [128, 12])
    w_co = dram_in("w_co", [512, D])
    w_ao = dram_in("w_ao", [512, D])
    w_out = dram_in("w_out", [D, D])
    w_router = dram_in("w_router", [D, NE])
    br_rep = dram_in("br_rep", [128, NE])
    qposB = dram_in("qposB", [128, OWN])
    refsel = dram_in("refsel", [128, NKB * NQB])
    kposc = dram_in("kposc", [128, NKB])
    ecap = dram_in("ecap", [128, NE])
    if stop_after >= 5:
        w_e1 = dram_in("w_e1", [NE, D, 2 * D])
        b1c = dram_in("b1c", [128, NE * 16])
        w_e2 = dram_in("w_e2", [NE, D, D])
        b_e2 = dram_in("b_e2", [NE, D])
    out = nc.dram_tensor("out", [OWN, D], F32, kind="ExternalOutput").ap()
    dbg = {}

    def dbg_out(name, shp, dt=F32):
        dbg[name] = nc.dram_tensor("dbg_" + name, list(shp), dt, kind="ExternalOutput").ap()
        return dbg[name]

    scr = lambda n, shp, dt: nc.dram_tensor(n, list(shp), dt, kind="Internal").ap()
    cg_d = scr("cg_d", [NCH, 128, KD * 256], BF16)
    sga_d = scr("sga_d", [NCH, 128, KD * 256], BF16)
    kvB_d = scr("kvB_d", [128, 2 * S + NKB * 4 * 65], BF16)
    x1_d = scr("x1_d", [NQB, 128, D], F32)
    xs_d = scr("xs_d", [NE * cap + 128, D], BF16)
    ys_d = scr("ys_d", [NE * cap + 128, D], F32)
    f_d = scr("f_d", [8, S], F32)

    with ExitStack() as es:
        SBW = 51 * 1024
        sb_all = es.enter_context(nc.sbuf_tensor("arena", [128, SBW], F32))
        ps_all = es.enter_context(nc.psum_tensor("parena", [128, 4096], F32))
        A = Arena(sb_all, SBW)
        P = Prog(nc, es)

        def bank(i, n=1):
            return ps_all[:, i * 512:(i + n) * 512]

        def act(out, in_, func, scale=None, bias=None, accum=None):
            r = [in_] + [x for x in (scale, bias) if isap(x)]
            w = [out] + ([accum] if accum is not None else [])
            kw = {}
            if scale is not None:
                kw["scale"] = scale
            if bias is not None:
                kw["bias"] = bias
            if accum is not None:
                kw["accum_out"] = accum
            return P.op("scalar", lambda e: e.activation(out=out, in_=in_, func=func, **kw), r, w)

        def tsc(eng, out, in0, s1, s2=None, op0=ALU.mult, op1=None):
            r = [in0] + [x for x in (s1, s2) if isap(x)]
            kw = {} if op1 is None else {"op1": op1}
            return P.op(eng, lambda e: e.tensor_scalar(out=out, in0=in0, scalar1=s1, scalar2=s2, op0=op0, **kw), r, [out])

        def tt(eng, out, in0, in1, op):
            return P.op(eng, lambda e: e.tensor_tensor(out=out, in0=in0, in1=in1, op=op), [in0, in1], [out])

        def stt(out, in0, scalar, in1, op0, op1):
            r = [in0, in1] + ([scalar] if isap(scalar) else [])
            return P.op("vector", lambda e: e.scalar_tensor_tensor(out=out, in0=in0, scalar=scalar, in1=in1, op0=op0, op1=op1), r, [out])

        def ttr(out, in0, in1, accum):
            return P.op("vector", lambda e: e.scalar_tensor_tensor(out=out, in0=in0, scalar=1.0, in1=in1, op0=ALU.mult, op1=ALU.mult, accum_out=accum),
                        [in0, in1], [out, accum])

        def cp(eng, out, in_):
            if eng == "scalar":
                return P.op("scalar", lambda e: e.activation(out=out, in_=in_, func=AF.Copy), [in_], [out])
            return P.op(eng, lambda e: e.tensor_copy(out=out, in_=in_), [in_], [out])

        def mset(eng, ap, v):
            return P.op(eng, lambda e: e.memset(ap, v), [], [ap])

        def recip(out, in_):
            return P.op("vector", lambda e: e.reciprocal(out=out, in_=in_), [in_], [out])

        def bankreg(ap):
            name, p0, p1, lo, hi, bs = region(ap)
            return (name, 0, 128, (lo // 2048) * 2048, ((hi + 2047) // 2048) * 2048, bs)

        def mm(out, lhsT, rhs, start, stop, signal=None):
            if signal is None:
                signal = stop
            return P.op("tensor", lambda e: e.matmul(out, lhsT=lhsT, rhs=rhs, start=start, stop=stop), [lhsT, rhs], [bankreg(out)], signal=signal)

        def tr(out, in_, ident, signal=True):
            return P.op("tensor", lambda e: e.transpose(out=out, in_=in_, identity=ident), [in_, ident], [bankreg(out)], signal=signal)

        def dma(eng, out, in_):
            return P.dma(eng, lambda e: e.dma_start(out=out, in_=in_), [in_], [out])

        def rms_rstd(ssq_ap, rstd_ap):
            tsc("vector", rstd_ap, ssq_ap, 1.0 / D, 1e-5, ALU.mult, ALU.add)
            act(rstd_ap, rstd_ap, AF.Sqrt)
            recip(rstd_ap, rstd_ap)

        identi = A.alloc([128, 128], I32)
        ident_f = A.alloc([128, 128], F32)
        ident_b = A.alloc([128, 128], BF16)
        ones_f = A.alloc([128, 128], F32)
        ones_b = A.alloc([128, 128], BF16)
        ustrict = A.alloc([128, 128], BF16)
        modcol = A.alloc([128, 48], F32)
        a1col = A.alloc([128, KD], F32)
        modrow = A.alloc([128, 4, D], F32)
        a2row = A.alloc([128, D], F32)
        gates = A.alloc([128, NQB, 4], F32)
        dests = A.alloc([128, NQB, 4], I32)
        cnt_i = A.alloc([128, NE], I32)
        fsb = {en: A.alloc([128, 2], F32) for en in ("scalar", "vector", "gpsimd")}
        A_BASE = A.mark()
        qT = A.alloc([128, 4, OWN], BF16)

        P.op("gpsimd", lambda e: e.iota(identi, pattern=[[1, 128]], base=0, channel_multiplier=-1), [], [identi])
        tsc("vector", ident_f, identi, 0, None, ALU.is_equal)
        tsc("vector", ident_b, identi, 0, None, ALU.is_equal)
        tsc("vector", ustrict, identi, 0, None, ALU.is_gt)
        mset("vector", ones_f, 1.0)
        mset("vector", ones_b, 1.0)
        for en in ("scalar", "vector", "gpsimd"):
            mset(en if en != "scalar" else "vector", fsb[en], 0.0)
        fps = bank(7)[:, 510:511]
        P.fake = {
            "tensor": lambda e: e.matmul(fps, lhsT=ones_b, rhs=ones_b[:, 0:1], start=True, stop=True),
            "scalar": lambda e: e.drain(),
            "vector": lambda e: e.engine_nop(),
            "gpsimd": lambda e: e.engine_nop(),
        }
        P.cnt_ap = lambda ei: cnt_i[0:1, ei:ei + 1]
        P.noguard = opts.get("d", 0)
        if opts:
            mset("gpsimd", qT, 0.0)
            mset("gpsimd", gates, 0.0)
            mset("gpsimd", dests, 0)

        m0 = A.mark()
        cv = A.alloc([128, KD], F32)
        scv = A.alloc([128, KD], F32)
        ydiag = A.alloc([128, D], F32)
        bcol = A.alloc([128, 48], F32)
        n1g = A.alloc([128, KD], F32)
        n2g = A.alloc([128, D], F32)
        wab = [A.alloc([128, KD, 512], F32) for _ in range(2)]
        dma("sync", cv, cvec)
        dma("sync", bcol, b_ada_col)
        dma("sync", n1g, n1g_col)
        dma("sync", n2g, n2g_rep)
        act(scv, cv, AF.Silu)
        pmod = bank(0)[:, 0:48]
        wa_v = w_ada.rearrange("(k p) n -> p k n", p=128)
        for gi in range(12):
            buf = wab[gi % 2]
            dma("sync" if gi % 2 == 0 else "scalar", buf, wa_v[:, :, gi * 512:(gi + 1) * 512])
            for oc in range(4):
                col = gi * 4 + oc
                for k in range(KD):
                    mm(pmod[:, col:col + 1], buf[:, k, oc * 128:(oc + 1) * 128], scv[:, k:k + 1], k == 0, k == KD - 1)
        tt("vector", modcol, pmod, bcol, ALU.add)
        stt(a1col, modcol[:, 8:16], 1.0, n1g, ALU.add, ALU.mult)
        sh1col = modcol[:, 0:8]
        for r_, mi in enumerate((2, 3, 4, 5)):
            for k in range(KD):
                tsc("vector", ydiag[:, k * 128:(k + 1) * 128], ident_f, modcol[:, mi * 8 + k:mi * 8 + k + 1], None, ALU.mult)
            for hf in range(2):
                pr = bank(1 + hf)
                mm(pr, ones_f, ydiag[:, hf * 512:(hf + 1) * 512], True, True)
                cp("vector", modrow[:, r_, hf * 512:(hf + 1) * 512], pr)
        stt(a2row, modrow[:, 2, :], 1.0, n2g, ALU.add, ALU.mult)
        if debug:
            dma("sync", dbg_out("modcol", [128, 48]), modcol)
            dma("sync", dbg_out("modrow", [128, 4 * D]), modrow.rearrange("p a b -> p (a b)"))
        A.reset(m0)
        if stop_after <= 0:
            P.emit()
            return nc, dbg

        m1 = A.mark()
        w_own = A.alloc([128, KD, 3584], BF16)
        O_GC, O_GA = 1536, 2560
        wco = A.alloc([128, 4, D], BF16)
        cw_sb = A.alloc([128, 4 * 31], F32)
        cvs = A.alloc([128, 12], F32)
        diag = A.alloc([128, 4 * 31, 128], BF16)
        hv = A.alloc([128, NCH * HALO], F32)
        xo = [A.alloc([128, 3, D], F32) for _ in range(2)]
        xsb = A.alloc([128, 3, D], BF16)
        junk = A.alloc([128, D], BF16)
        ssq = A.alloc([128, 4], F32)
        rstd = A.alloc([128, 4], F32)
        hT = A.alloc([128, KD, CW], BF16)
        sg = A.alloc([128, CW], F32)
        a_bf = A.alloc([128, 4, CW], BF16)
        y_f = A.alloc([128, 4, 256], F32)
        y_b = A.alloc([128, 4, 256], BF16)
        ysq = A.alloc([128, 4, 256], BF16)
        mean_s = A.alloc([128, 256], F32)
        var_s = A.alloc([128, 256], F32)
        yn = A.alloc([128, 256], F32)
        actv = A.alloc([128, 4, 256], BF16)
        sgate = A.alloc([128, 256], F32)
        cg_s = A.alloc([128, KD, 256], BF16)
        sga_s = A.alloc([128, KD, 256], BF16)
        win_v = w_in.rearrange("(k p) n -> p k n", p=128)
        dma("gpsimd", w_own[:, :, 0:1536], win_v[:, :, 0:1536])
        dma("gpsimd", w_own[:, :, 1536:2560], win_v[:, :, C_GC:C_GC + 1024])
        dma("gpsimd", w_own[:, :, 2560:3584], win_v[:, :, C_GA:C_GA + 1024])
        dma("gpsimd", wco, w_co.rearrange("(k p) n -> p k n", p=128))
        dma("sync", cw_sb, cwT)
        dma("sync", cvs, cvecs)
        dma("sync", hv, halov)
        mset("vector", ssq, 1.0)
        for i in range(4 * 31):
            tsc("gpsimd", diag[:, i, :], ident_f, cw_sb[:, i:i + 1], None, ALU.mult)
        convb, lng, lnb = cvs[:, 0:4], cvs[:, 4:8], cvs[:, 8:12]
        pT = [bank(b).bitcast(BF16).rearrange("p (k t) -> p k t", k=KD) for b in range(3)]
        for lc in range(opts.get("c", NCH)):
            xb = xo[lc % 2]
            dma("sync", xb[:, 0:2, :], xown[lc, HALO:CW, :].rearrange("(b p) d -> p b d", p=128))
            dma("sync", xb[0:HALO, 2, :], xown[lc, 0:HALO, :])
            for b in range(3):
                np_ = 128 if b < 2 else HALO
                act(junk[0:np_, :], xb[0:np_, b, :], AF.Square, accum=ssq[0:np_, b:b + 1])
            rms_rstd(ssq, rstd)
            for b in range(3):
                np_ = 128 if b < 2 else HALO
                tsc("vector", xsb[0:np_, b, :], xb[0:np_, b, :], rstd[0:np_, b:b + 1], None, ALU.mult)
            for b in range(3):
                np_ = 128 if b < 2 else HALO
                for k in range(KD):
                    tr(pT[b][:, k, 0:np_], xsb[0:np_, b, k * 128:(k + 1) * 128], ident_b[0:np_, 0:np_], signal=(k == KD - 1))
            for k in range(KD):
                for b in range(3):
                    np_ = 128 if b < 2 else HALO
                    c0 = HALO + b * 128 if b < 2 else 0
                    act(hT[:, k, c0:c0 + np_], pT[b][:, k, 0:np_], AF.Identity, scale=a1col[:, k:k + 1], bias=sh1col[:, k:k + 1])
            for cc in range(4):
                puv, pug = bank(3)[:, 0:CW], bank(4)[:, 0:CW]
                for k in range(KD):
                    mm(puv, w_own[:, k, C_UV + cc * 128:C_UV + (cc + 1) * 128], hT[:, k, :], k == 0, k == KD - 1)
                for k in range(KD):
                    mm(pug, w_own[:, k, C_UG + cc * 128:C_UG + (cc + 1) * 128], hT[:, k, :], k == 0, k == KD - 1)
                act(sg, pug, AF.Sigmoid)
                tt("vector", a_bf[:, cc, :], puv, sg, ALU.mult)
                tt("gpsimd", a_bf[:, cc, 0:HALO], a_bf[:, cc, 0:HALO], hv[:, lc * HALO:(lc + 1) * HALO], ALU.mult)
                pconv = bank(5)[:, 0:256]
                for kk in range(31):
                    mm(pconv, diag[:, cc * 31 + kk, :], a_bf[:, cc, 2 + kk:2 + kk + 256], kk == 0, kk == 30)
                act(y_f[:, cc, :], pconv, AF.Identity, bias=convb[:, cc:cc + 1])
                cp("vector", y_b[:, cc, :], y_f[:, cc, :])
                tt("gpsimd", ysq[:, cc, :], y_f[:, cc, :], y_f[:, cc, :], ALU.mult)
            pmean, pmsq = bank(6)[:, 0:256], bank(7)[:, 0:256]
            for cc in range(4):
                mm(pmean, ones_b, y_b[:, cc, :], cc == 0, cc == 3)
            for cc in range(4):
                mm(pmsq, ones_b, ysq[:, cc, :], cc == 0, cc == 3)
            act(mean_s, pmean, AF.Copy, scale=1.0 / 512)
            tt("vector", var_s, mean_s, mean_s, ALU.mult)
            stt(var_s, pmsq, 1.0 / 512, var_s, ALU.mult, ALU.subtract)
            tsc("vector", var_s, var_s, 1e-5, None, ALU.add)
            act(var_s, var_s, AF.Sqrt)
            recip(var_s, var_s)
            for cc in range(4):
                tt("vector", yn, y_f[:, cc, :], mean_s, ALU.subtract)
                tt("vector", yn, yn, var_s, ALU.mult)
                act(actv[:, cc, :], yn, AF.Silu, scale=lng[:, cc:cc + 1], bias=lnb[:, cc:cc + 1])
            for dmc in range(KD):
                pco, pgc = bank(3)[:, 0:256], bank(4)[:, 0:256]
                for cc in range(4):
                    mm(pco, wco[:, cc, dmc * 128:(dmc + 1) * 128], actv[:, cc, :], cc == 0, cc == 3)
                for k in range(KD):
                    mm(pgc, w_own[:, k, O_GC + dmc * 128:O_GC + (dmc + 1) * 128], hT[:, k, HALO:CW], k == 0, k == KD - 1)
                act(sgate, pgc, AF.Sigmoid)
                tt("vector", cg_s[:, dmc, :], pco, sgate, ALU.mult)
            dma("scalar", cg_d[lc], cg_s.rearrange("p a b -> p (a b)"))
            for dmc in range(KD):
                pga = bank(5 + dmc % 2)[:, 0:256]
                for k in range(KD):
                    mm(pga, w_own[:, k, O_GA + dmc * 128:O_GA + (dmc + 1) * 128], hT[:, k, HALO:CW], k == 0, k == KD - 1)
                act(sga_s[:, dmc, :], pga, AF.Sigmoid)
            dma("scalar", sga_d[lc], sga_s.rearrange("p a b -> p (a b)"))
            for hp in range(4):
                pq = bank(3 + hp % 2)[:, 0:256]
                for k in range(KD):
                    mm(pq, w_own[:, k, C_Q + hp * 128:C_Q + (hp + 1) * 128], hT[:, k, HALO:CW], k == 0, k == KD - 1)
                cp("vector", qT[:, hp, lc * 256:(lc + 1) * 256], pq)
        if debug:
            dma("sync", dbg_out("qT", [128, 4 * OWN], BF16), qT.rearrange("p a b -> p (a b)"))
        A.reset(m1)
        if stop_after <= 1:
            P.emit()
            return nc, dbg

        attn = A.alloc([128, NQB, 512], BF16)
        KT = A.alloc([128, 2, S], BF16)
        V = A.alloc([128, NKB, 4, 65], BF16)
        Fneg = A.alloc([128, NKB, 8], F32)
        FrefB = A.alloc([128, NQB, 8], F32)
        m2 = A.mark()
        w_kv = A.alloc([128, KD, 1024], BF16)
        wf = A.alloc([128, KD, 128], BF16)
        refs = A.alloc([128, NKB, NQB], F32)
        fst = [A.alloc([8, 256], F32) for _ in range(2)]
        ssq2 = A.alloc([128, 2], F32)
        rstd2 = A.alloc([128, 2], F32)
        junk2 = A.alloc([128, D], BF16)
        bfc = A.alloc([8, 1], F32)
        GrefT = A.alloc([8, NQB], F32)
        Eexp = A.alloc([8, NQB, 8], F32)
        kst = [A.alloc([128, 2, 256], BF16) for _ in range(2)]
        vst = [A.alloc([128, 2, 4, 65], BF16) for _ in range(2)]
        m_alias = A.mark()
        xf = [A.alloc([128, 2, D], F32) for _ in range(2)]
        xs2 = [A.alloc([128, 2, D], BF16) for _ in range(2)]
        hT2 = [A.alloc([128, KD, 256], BF16) for _ in range(2)]
        if opts:
            mset("gpsimd", KT, 0.0)
            mset("gpsimd", V, 0.0)
            mset("gpsimd", attn, 0.0)
        dma("gpsimd", w_kv, win_v[:, :, C_K:C_K + 1024])
        mset("vector", wf, 0.0)
        dma("gpsimd", wf[:, :, 0:8], win_v[:, :, C_F:C_F + 8])
        dma("sync", refs.rearrange("p a b -> p (a b)"), refsel)
        dma("sync", bfc, bf_col)
        mset("gpsimd", V[:, :, :, 64:65], 1.0)
        for i in range(2):
            mset("gpsimd", vst[i][:, :, :, 64:65], 1.0)
        kvB_K = kvB_d[:, 0:2 * S].rearrange("p (a b) -> p a b", a=2)
        kvB_V = kvB_d[:, 2 * S:2 * S + NKB * 260].rearrange("p (a b) -> p a b", a=NKB)
        pTv = ps_all[:, 0:1024].bitcast(BF16).rearrange("p (b k t) -> p b k t", b=2, k=KD)
        NG = opts.get("g", 32)
        for gi in range(NG):
            i2 = gi % 2
            xb, xsb_, hb = xf[i2], xs2[i2], hT2[i2]
            dma("sync" if i2 == 0 else "scalar", xb, xfull[gi * 256:(gi + 1) * 256, :].rearrange("(b p) d -> p b d", p=128))
            for b in range(2):
                act(junk2, xb[:, b, :], AF.Square, accum=ssq2[:, b:b + 1])
            rms_rstd(ssq2, rstd2)
            for b in range(2):
                tsc("vector", xsb_[:, b, :], xb[:, b, :], rstd2[:, b:b + 1], None, ALU.mult)
            for b in range(2):
                for k in range(KD):
                    tr(pTv[:, b, k, :], xsb_[:, b, k * 128:(k + 1) * 128], ident_b, signal=(b == 1 and k == KD - 1))
            for k in range(KD):
                act(hb[:, k, :].rearrange("p (b t) -> p b t", b=2), pTv[:, :, k, :], AF.Identity, scale=a1col[:, k:k + 1], bias=sh1col[:, k:k + 1])
            NSK = opts.get("n", 0)
            for hp in range(4 if not (NSK & 2) else 0):
                pk = bank(2 + hp % 2)[:, 0:256]
                for k in range(KD):
                    mm(pk, w_kv[:, k, hp * 128:(hp + 1) * 128], hb[:, k, :], k == 0, k == KD - 1)
                if hp < 2:
                    cp("vector", KT[:, hp, gi * 256:(gi + 1) * 256], pk)
                else:
                    cp("vector", kst[i2][:, hp - 2, :], pk)
            if not (NSK & 2):
                dma("sync", kvB_K[:, :, gi * 256:(gi + 1) * 256], kst[i2])
            for b in range(2 if not (NSK & 4) else 0):
                pv = bank(4 + b)
                for k in range(KD):
                    mm(pv, hb[:, k, b * 128:(b + 1) * 128], w_kv[:, k, 512:1024], k == 0, k == KD - 1)
                kb = gi * 2 + b
                if not (NSK & 16):
                    cp("scalar", V[:, kb, :, 0:64], pv[:, 0:256].rearrange("p (h d) -> p h d", h=4))
                if not (NSK & 32):
                    cp("vector", vst[i2][:, b, :, 0:64], pv[:, 256:512].rearrange("p (h d) -> p h d", h=4))
            if not (NSK & 4) and not (NSK & 64):
                dma("sync", kvB_V[:, gi * 2:gi * 2 + 2, :], vst[i2].rearrange("p a b c -> p a (b c)"))
            if NSK & 8:
                continue
            pf = bank(6)[0:8, 0:256]
            pf_full = bank(6)[:, 0:256]
            for k in range(KD):
                mm(pf_full, wf[:, k, :], hb[:, k, :], k == 0, k == KD - 1)
            cp("vector", fst[i2], pf)
            dma("sync", f_d[:, gi * 256:(gi + 1) * 256], fst[i2])
        if opts.get("n", 0) & 1:
            if debug:
                dma("sync", dbg_out("KT", [128, 2 * S], BF16), KT.rearrange("p a b -> p (a b)"))
                dma("sync", dbg_out("V", [128, NKB * 260], BF16), V.rearrange("p a b c -> p (a b c)"))
            P.emit()
            return nc, dbg
        A.reset(m_alias)
        fT = A.alloc([8, S], F32)
        dma("sync", fT, f_d)
        tsc("vector", bfc, bfc, -1.0, None, ALU.mult)
        act(fT, fT, AF.Exp, scale=-1.0, bias=bfc[:, 0:1])
        act(fT, fT, AF.Ln, bias=1.0)
        P.op("vector", lambda e: e.tensor_tensor_scan(out=fT, data0=fT, data1=fT, initial=0.0, op0=ALU.add, op1=ALU.max), [fT], [fT])
        pF = bank(7)
        for kb in range(NKB):
            tr(pF[:, kb * 8:(kb + 1) * 8], fT[0:8, kb * 128:(kb + 1) * 128], ident_f[0:8, 0:8], signal=(kb == NKB - 1))
        cp("vector", Fneg.rearrange("p a b -> p (a b)"), pF)
        pG = bank(6)[0:8, 0:NQB]
        for kb in range(NKB):
            mm(pG, Fneg[:, kb, :], refs[:, kb, :], kb == 0, kb == NKB - 1)
        cp("vector", GrefT, pG)
        tt("vector", Eexp, GrefT.unsqueeze(2).to_broadcast([8, NQB, 8]), ident_f[0:8, 0:8].unsqueeze(1).to_broadcast([8, NQB, 8]), ALU.mult)
        pB = bank(5)[:, 0:128]
        mm(pB, ones_f[0:8, :], Eexp.rearrange("p a b -> p (a b)"), True, True)
        cp("vector", FrefB.rearrange("p a b -> p (a b)"), pB)
        if debug:
            dma("sync", dbg_out("KT", [128, 2 * S], BF16), KT.rearrange("p a b -> p (a b)"))
            dma("sync", dbg_out("V", [128, NKB * 260], BF16), V.rearrange("p a b c -> p (a b c)"))
            dma("sync", dbg_out("G", [128, NKB * 8]), Fneg.rearrange("p a b -> p (a b)"))
            dma("sync", dbg_out("Gref", [128, NQB * 8]), FrefB.rearrange("p a b -> p (a b)"))
        A.reset(m2)
        if stop_after <= 2:
            P.emit()
            return nc, dbg

        qpos = A.alloc([128, OWN], F32)
        kpos = A.alloc([128, NKB], F32)
        biasq = [A.alloc([128, NKB, 4], F32) for _ in range(2)]
        masks = [A.alloc([128, 8, 128], BF16) for _ in range(2)]
        Pt = A.alloc([128, 8, 128], BF16)
        rden = A.alloc([128, 4], F32)
        dma("sync", qpos, qposB)
        dma("sync", kpos, kposc)
        ztile = A.alloc([128, 2, D], BF16)
        ztile_f = A.alloc([128, 2, D], F32)
        mset("gpsimd", ztile, 0.0)
        mset("gpsimd", ztile_f, 0.0)
        nrows = NE * cap + 128
        for r0 in range(0, nrows, 256):
            nr = min(256, nrows - r0)
            dma("sync", xs_d[r0:r0 + nr, :].rearrange("(r p) d -> p r d", p=128), ztile[:, 0:nr // 128, :])
        Sps = ps_all[:, 0:2048].rearrange("p (s t) -> p s t", s=4)[:, :, 0:128]
        NQR = opts.get("q", NQB)
        for half in range(2):
            if half == 1:
                dma("sync", KT, kvB_K)
                dma("scalar", V.rearrange("p a b c -> p a (b c)"), kvB_V)
                for r0 in range(0, nrows, 256):
                    nr = min(256, nrows - r0)
                    dma("sync", ys_d[r0:r0 + nr, :].rearrange("(r p) d -> p r d", p=128), ztile_f[:, 0:nr // 128, :])
            tile_no = 0
            for qb in range(NQR):
                lc = qb // 2
                nkb, ws = kb_end(lc), win_start(lc)
                bq, mk = biasq[qb % 2], masks[qb % 2]
                tt("vector", bq, Fneg[:, :, half * 4:(half + 1) * 4], FrefB[:, qb, half * 4:(half + 1) * 4].unsqueeze(1).to_broadcast([128, NKB, 4]), ALU.subtract)
                tsc("vector", bq, bq, 0.0, None, ALU.min)
                for w in range(8):
                    tsc("gpsimd", mk[:, w, :], qpos[:, qb * 128:(qb + 1) * 128], kpos[:, ws + w:ws + w + 1], None, ALU.is_ge)
                tiles = [(hl, kb) for hl in range(4) for kb in range(nkb)]
                LA = 3

                def emit_S(i, tiles=tiles, qb=qb, half=half, tile_no=tile_no):
                    hl, kb = tiles[i]
                    n = tile_no + i
                    prt = (hl % 2) * 64
                    hpl = hl // 2
                    mm(Sps[:, n % 4, :], KT[prt:prt + 64, hpl, kb * 128:(kb + 1) * 128],
                       qT[prt:prt + 64, half * 2 + hpl, qb * 128:(qb + 1) * 128], True, True)

                for i in range(min(LA, len(tiles))):
                    emit_S(i)
                for i, (hl, kb) in enumerate(tiles):
                    n = tile_no + i
                    if i + LA < len(tiles):
                        emit_S(i + LA)
                    Ob = bank(4 + (hl % 2) + 2 * (qb % 2))
                    act(Pt[:, n % 8, :], Sps[:, n % 4, :], AF.Exp, scale=0.125, bias=bq[:, kb, hl:hl + 1])
                    if kb >= ws:
                        tt("gpsimd", Pt[:, n % 8, :], Pt[:, n % 8, :], mk[:, kb - ws, :], ALU.mult)
                    mm(Ob[:, 0:65], Pt[:, n % 8, :], V[:, kb, hl, :], kb == 0, kb == nkb - 1, signal=True)
                    if kb == nkb - 1:
                        h = half * 4 + hl
                        recip(rden[:, hl:hl + 1], Ob[:, 64:65])
                        tsc("vector", attn[:, qb, h * 64:(h + 1) * 64], Ob[:, 0:64], rden[:, hl:hl + 1], None, ALU.mult)
                tile_no += len(tiles)
        if debug:
            dma("sync", dbg_out("attn", [128, NQB * 512], BF16), attn.rearrange("p a b -> p (a b)"))
        A.reset(m2)
        if stop_after <= 3:
            P.emit()
            return nc, dbg

        wao = A.alloc([128, 4, D], BF16)
        wout = A.alloc([128, KD, D], BF16)
        wr = A.alloc([128, KD, NE], F32)
        brp = A.alloc([128, NE], F32)
        ecp = A.alloc([128, NE], F32)
        maskb = A.alloc([128, NQB, NE], BF16)
        sga_b = [A.alloc([128, KD, 128], BF16) for _ in range(2)]
        cg_b = [A.alloc([128, KD, 128], BF16) for _ in range(2)]
        xblk = [A.alloc([128, D], F32) for _ in range(2)]
        attnT = A.alloc([128, 4, 128], BF16)
        tmpf = A.alloc([128, D], F32)
        mixT = A.alloc([128, KD, 128], BF16)
        x1 = [A.alloc([128, D], F32) for _ in range(2)]
        h2f = A.alloc([128, D], F32)
        h2b = [A.alloc([128, D], BF16) for _ in range(2)]
        h2T = A.alloc([128, KD, 128], F32)
        junk3 = A.alloc([128, D], BF16)
        ssq3 = A.alloc([128, 1], F32)
        rstd3 = A.alloc([128, 1], F32)
        lg = A.alloc([128, NE], F32)
        top8 = A.alloc([128, 8], F32)
        msk = A.alloc([128, NE], F32)
        negm = A.alloc([128, 1], F32)
        ex = A.alloc([128, NE], F32)
        ssum = A.alloc([128, 1], F32)
        g32 = A.alloc([128, NE], F32)
        destf = A.alloc([128, NE], F32)
        oh = A.alloc([128, NE], F32)
        jk32 = A.alloc([128, NE], F32)
        dkf = A.alloc([128, 4], F32)
        dma("gpsimd", wao, w_ao.rearrange("(k p) n -> p k n", p=128))
        dma("gpsimd", wout, w_out.rearrange("(k p) n -> p k n", p=128))
        dma("sync", wr, w_router.rearrange("(k p) n -> p k n", p=128))
        dma("sync", brp, br_rep)
        dma("sync", ecp, ecap)
        for qb in range(NQR):
            lc, off = qb // 2, (qb % 2) * 128
            i2 = qb % 2
            dma("sync", sga_b[i2], sga_d[lc].rearrange("p (a b) -> p a b", a=KD)[:, :, off:off + 128])
            dma("sync", cg_b[i2], cg_d[lc].rearrange("p (a b) -> p a b", a=KD)[:, :, off:off + 128])
            dma("scalar", xblk[i2], xown[lc, HALO + off:HALO + off + 128, :])
            pAT = bank(0).bitcast(BF16)[:, 0:512].rearrange("p (c t) -> p c t", c=4)
            for c4 in range(4):
                tr(pAT[:, c4, :], attn[:, qb, c4 * 128:(c4 + 1) * 128], ident_b, signal=(c4 == 3))
            cp("vector", attnT, pAT)
            pAO = ps_all[:, 512:1536]
            for dmc in range(KD):
                for c4 in range(4):
                    mm(pAO[:, dmc * 128:(dmc + 1) * 128], wao[:, c4, dmc * 128:(dmc + 1) * 128], attnT[:, c4, :], c4 == 0, c4 == 3)
            tt("vector", tmpf, pAO, sga_b[i2].rearrange("p a b -> p (a b)"), ALU.mult)
            tt("vector", mixT.rearrange("p a b -> p (a b)"), tmpf, cg_b[i2].rearrange("p a b -> p (a b)"), ALU.add)
            pD = ps_all[:, 1536:2560]
            for nh in range(2):
                for dk in range(KD):
                    mm(pD[:, nh * 512:(nh + 1) * 512], mixT[:, dk, :], wout[:, dk, nh * 512:(nh + 1) * 512], dk == 0, dk == KD - 1)
            x1b = x1[i2]
            tt("vector", tmpf, pD, modrow[:, 0, :], ALU.mult)
            tt("vector", x1b, tmpf, xblk[i2], ALU.add)
            dma("scalar", x1_d[qb], x1b)
            act(junk3, x1b, AF.Square, accum=ssq3)
            rms_rstd(ssq3, rstd3)
            stt(h2f, x1b, rstd3[:, 0:1], a2row, ALU.mult, ALU.mult)
            tt("vector", h2f, h2f, modrow[:, 1, :], ALU.add)
            cp("scalar", h2b[i2], h2f)
            pHT = ps_all[:, 2560:3584].rearrange("p (k t) -> p k t", k=KD)
            for k in range(KD):
                tr(pHT[:, k, :], h2f[:, k * 128:(k + 1) * 128], ident_f, signal=(k == KD - 1))
            cp("vector", h2T.rearrange("p a b -> p (a b)"), ps_all[:, 2560:3584])
            plog = bank(7)[:, 0:NE]
            for k in range(KD):
                mm(plog, h2T[:, k, :], wr[:, k, :], k == 0, k == KD - 1)
            tt("vector", lg, plog, brp, ALU.add)
            P.op("vector", lambda e: e.max(out=top8, in_=lg), [lg], [top8])
            tsc("vector", msk, lg, top8[:, 3:4], None, ALU.is_ge)
            cp("vector", maskb[:, qb, :], msk)
            tsc("vector", negm, top8[:, 0:1], -1.0, None, ALU.mult)
            act(ex, lg, AF.Exp, bias=negm[:, 0:1])
            ttr(ex, ex, msk, ssum)
            recip(ssum, ssum)
            tsc("vector", g32, ex, ssum[:, 0:1], None, ALU.mult)
            ppos = bank(7)[:, 64:64 + NE]
            for b2 in range(qb + 1):
                mm(ppos, ones_b if b2 < qb else ustrict, maskb[:, b2, :], b2 == 0, b2 == qb)
            tt("vector", destf, ppos, ecp, ALU.add)
            for k in range(4):
                tsc("vector", oh, lg, top8[:, k:k + 1], None, ALU.is_equal)
                ttr(jk32, oh, g32, gates[:, qb, k:k + 1])
                ttr(jk32, oh, destf, dkf[:, k:k + 1])
            cp("vector", dests[:, qb, :], dkf)
            for k in range(4):
                P.dma("gpsimd", lambda e, qb=qb, k=k, i2=i2: e.indirect_dma_start(
                    out=xs_d[:, :], out_offset=bass.IndirectOffsetOnAxis(ap=dests[:, qb, k:k + 1], axis=0),
                    in_=h2b[i2], in_offset=None), [h2b[i2], dests[:, qb, k:k + 1]], [xs_d[:, :]])
        pcnt = bank(7)[:, 128:128 + NE]
        for b2 in range(NQR):
            mm(pcnt, ones_b, maskb[:, b2, :], b2 == 0, b2 == NQR - 1)
        t_cnt = cp("vector", cnt_i, pcnt)
        if debug:
            dma("sync", dbg_out("cnt", [128, NE], I32), cnt_i)
            dma("sync", dbg_out("lg", [128, NE]), lg)
            dma("sync", dbg_out("top8", [128, 8]), top8)
            dma("sync", dbg_out("h2f", [128, D]), h2f)
            dma("sync", dbg_out("gates", [128, NQB * 4]), gates.rearrange("p a b -> p (a b)"))
            dma("sync", dbg_out("dests", [128, NQB * 4], I32), dests.rearrange("p a b -> p (a b)"))
            d_x1 = dbg_out("x1", [NQB, 128, D])
            for qb in range(NQR):
                dma("sync", xblk[0], x1_d[qb])
                dma("sync", d_x1[qb], xblk[0])
        A.reset(A_BASE)
        if stop_after <= 4:
            P.emit()
            return nc, dbg

        W1 = [A.alloc([128, KD, 2 * D], BF16) for _ in range(2)]
        W2 = [A.alloc([128, KD, D], BF16) for _ in range(2)]
        b1s = A.alloc([128, NE * 16], F32)
        b2r = [A.alloc([128, D], F32) for _ in range(2)]
        xbk = [A.alloc([128, D], BF16) for _ in range(2)]
        xT = [A.alloc([128, KD, 128], BF16) for _ in range(2)]
        gm = [A.alloc([128, 512], F32) for _ in range(2)]
        sgm = [A.alloc([128, 512], F32) for _ in range(2)]
        uc = [A.alloc([128, 512], F32) for _ in range(2)]
        hmT = [A.alloc([128, 8, 128], BF16) for _ in range(2)]
        yb = [A.alloc([128, D], F32) for _ in range(2)]
        dma("sync", b1s, b1c)
        NEr = opts.get("e", NE)
        stg = [A.alloc([128, KD, 256], F32) for _ in range(4)]
        stg_i = [0]

        def expert_chunks(ei):
            wb1_, wb2_ = W1[ei % 2], W2[ei % 2]
            w1v = w_e1[ei].rearrange("(k p) n -> p k n", p=128)
            w2v = w_e2[ei].rearrange("(k p) n -> p k n", p=128)
            ch = []
            for c in range(12):
                if c < 8:
                    ch.append((w1v[:, :, c * 256:(c + 1) * 256], wb1_[:, :, c * 256:(c + 1) * 256]))
                else:
                    ch.append((w2v[:, :, (c - 8) * 256:(c - 7) * 256], wb2_[:, :, (c - 8) * 256:(c - 7) * 256]))
            return ch

        def w_dma(ei, c):
            src, dst = expert_chunks(ei)[c]
            dma("sync", stg[(ei * 8 + c) % 4], src)

        def w_cast(ei, c):
            src, dst = expert_chunks(ei)[c]
            cp("scalar", dst, stg[(ei * 8 + c) % 4])

        NCHK = 8

        def w2_dma(ei):
            dma("gpsimd", W2[ei % 2], w_e2[ei].rearrange("(k p) n -> p k n", p=128))

        dma("sync", b2r[0], b_e2[0:1, :].to_broadcast([128, D]))
        w2_dma(0)
        for c in range(4):
            w_dma(0, c)
        for c in range(NCHK):
            w_cast(0, c)
            if c + 4 < NCHK:
                w_dma(0, c + 4)
        for e_ in range(NEr):
            wb1, wb2 = W1[e_ % 2], W2[e_ % 2]
            nxt = e_ + 1 if e_ + 1 < NEr else None
            if nxt is not None:
                dma("sync", b2r[nxt % 2], b_e2[nxt:nxt + 1, :].to_broadcast([128, D]))
                w2_dma(nxt)
                for c in range(4):
                    w_dma(nxt, c)
            P.regload(e_, [t_cnt])
            dma("scalar", xbk[0], xs_d[e_ * cap:e_ * cap + 128, :])

            def st_T(kb_):
                P.guard = (e_, kb_ * 128)
                xk = xbk[kb_ % 2]
                xTk = xT[kb_ % 2]
                pXT = bank(0).bitcast(BF16).rearrange("p (k t) -> p k t", k=KD)
                for k in range(KD):
                    tr(pXT[:, k, :], xk[:, k * 128:(k + 1) * 128], ident_b, signal=(k == KD - 1))
                cp("vector", xTk.rearrange("p a b -> p (a b)"), bank(0).bitcast(BF16))
                P.guard = None

            def st_H(kb_, half2):
                P.guard = (e_, kb_ * 128)
                xTk = xT[kb_ % 2]
                gm_, uc_, sgm_ = gm[half2], uc[half2], sgm[half2]
                pg, pu = bank(1 + half2), bank(3 + half2)
                for j4 in range(4):
                    fc = half2 * 4 + j4
                    for k in range(KD):
                        mm(pg[:, j4 * 128:(j4 + 1) * 128], wb1[:, k, fc * 128:(fc + 1) * 128], xTk[:, k, :], k == 0, k == KD - 1)
                for j4 in range(4):
                    fc = 8 + half2 * 4 + j4
                    for k in range(KD):
                        mm(pu[:, j4 * 128:(j4 + 1) * 128], wb1[:, k, fc * 128:(fc + 1) * 128], xTk[:, k, :], k == 0, k == KD - 1)
                for j4 in range(4):
                    fcg = half2 * 4 + j4
                    tsc("vector", gm_[:, j4 * 128:(j4 + 1) * 128], pg[:, j4 * 128:(j4 + 1) * 128], b1s[:, e_ * 16 + fcg:e_ * 16 + fcg + 1], 7.0, ALU.add, ALU.min)
                    tsc("vector", uc_[:, j4 * 128:(j4 + 1) * 128], pu[:, j4 * 128:(j4 + 1) * 128], b1s[:, e_ * 16 + 8 + fcg:e_ * 16 + 8 + fcg + 1], 7.0, ALU.add, ALU.min)
                act(sgm_, gm_, AF.Silu, scale=1.702)
                tsc("vector", uc_, uc_, -7.0, 1.0, ALU.max, ALU.add)
                stt(hmT[kb_ % 2][:, half2 * 4:(half2 + 1) * 4, :].rearrange("p a b -> p (a b)"), uc_, 1.0 / 1.702, sgm_, ALU.mult, ALU.mult)
                P.guard = None

            def st_Y(kb_):
                P.guard = (e_, kb_ * 128)
                row0 = e_ * cap + kb_ * 128
                pY = ps_all[:, 2560:3584]
                for nh in range(2):
                    for fc in range(8):
                        mm(pY[:, nh * 512:(nh + 1) * 512], hmT[kb_ % 2][:, fc, :], wb2[:, fc, nh * 512:(nh + 1) * 512], fc == 0, fc == 7)
                ybk = yb[kb_ % 2]
                tt("vector", ybk, pY, b2r[e_ % 2], ALU.add)
                dma("scalar", ys_d[row0:row0 + 128, :], ybk)
                P.guard = None

            def st_prefetch(kb_):
                if kb_ < kmax:
                    P.guard = (e_, kb_ * 128)
                    dma("scalar", xbk[kb_ % 2], xs_d[e_ * cap + kb_ * 128:e_ * cap + kb_ * 128 + 128, :])
                    P.guard = None

            st_prefetch(1)
            st_T(0)
            st_H(0, 0)
            for kblk in range(kmax):
                st_H(kblk, 1)
                if kblk + 1 < kmax:
                    st_prefetch(kblk + 2)
                    st_T(kblk + 1)
                    st_H(kblk + 1, 0)
                st_Y(kblk)
                if nxt is not None:
                    for c in (3 * kblk, 3 * kblk + 1, 3 * kblk + 2):
                        if c < NCHK:
                            w_cast(nxt, c)
                            if c + 4 < NCHK:
                                w_dma(nxt, c + 4)
        A.reset(A_BASE)
        if stop_after <= 5:
            P.emit()
            return nc, dbg

        yk = [A.alloc([128, D], F32) for _ in range(4)]
        acc = A.alloc([128, D], F32)
        x1r = [A.alloc([128, D], F32) for _ in range(2)]
        finr = A.alloc([128, D], F32)
        junk5 = A.alloc([128, D], BF16)
        ssq5 = A.alloc([128, 1], F32)
        rstd5 = A.alloc([128, 1], F32)
        ob = [A.alloc([128, D], F32) for _ in range(2)]
        dma("sync", finr, fin_rep)
        for qb in range(NQR):
            i2 = qb % 2
            dma("sync", x1r[i2], x1_d[qb])
            for k in range(4):
                P.dma("gpsimd", lambda e, qb=qb, k=k: e.indirect_dma_start(
                    out=yk[k], out_offset=None, in_=ys_d[:, :],
                    in_offset=bass.IndirectOffsetOnAxis(ap=dests[:, qb, k:k + 1], axis=0)), [ys_d[:, :], dests[:, qb, k:k + 1]], [yk[k]])
            tsc("vector", acc, yk[0], gates[:, qb, 0:1], None, ALU.mult)
            for k in range(1, 4):
                stt(acc, yk[k], gates[:, qb, k:k + 1], acc, ALU.mult, ALU.add)
            tt("vector", acc, acc, modrow[:, 3, :], ALU.mult)
            tt("vector", acc, acc, x1r[i2], ALU.add)
            act(junk5, acc, AF.Square, accum=ssq5)
            rms_rstd(ssq5, rstd5)
            stt(ob[i2], acc, rstd5[:, 0:1], finr, ALU.mult, ALU.mult)
            dma("sync", out[qb * 128:(qb + 1) * 128, :], ob[i2])
        P.emit()
    return nc, dbg


def host_inputs(inp, kmax=KMAX):
    cap = kmax * 128
    f = lambda a: np.ascontiguousarray(a, dtype=np.float32)
    x = inp["x"]
    col = lambda v: f(v.reshape(-1, 128).T)
    rep = lambda v: f(np.broadcast_to(v.reshape(1, -1), (128, v.size)))
    shared = {
        "w_ada": f(inp["w_ada"][0]),
        "b_ada_col": col(inp["b_ada"][0]),
        "n1g_col": col(inp["norm1_g"][0]),
        "n2g_rep": rep(inp["norm2_g"][0]),
        "fin_rep": rep(inp["final_g"]),
        "w_in": f(inp["w_in"][0]),
        "bf_col": f(inp["b_forget"][0].reshape(8, 1)),
        "cwT": f(inp["conv_w"][0].T.reshape(4, 128, 31).transpose(1, 0, 2).reshape(128, 124)),
        "cvecs": f(np.concatenate([col(inp["conv_b"][0]), col(inp["conv_ln_g"][0]), col(inp["conv_ln_b"][0])], axis=1)),
        "w_co": f(inp["w_conv_out"][0]),
        "w_ao": f(inp["w_attn_out"][0]),
        "w_out": f(inp["w_out"][0]),
        "w_router": f(inp["w_router"][0]),
        "br_rep": rep(inp["b_router"][0]),
        "w_e1": f(inp["w_exp_in"][0]),
        "b1c": f(inp["b_exp_in"][0].reshape(NE, 16, 128).transpose(2, 0, 1).reshape(128, NE * 16)),
        "w_e2": f(inp["w_exp_out"][0]),
        "b_e2": f(inp["b_exp_out"][0]),
        "kposc": f((np.arange(NKB)[None, :] * 128 + np.arange(128)[:, None])),
        "ecap": rep(np.arange(NE, dtype=np.float32) * cap),
    }
    maps = []
    poss = []
    for c in range(NCORES):
        b, j = c // 4, c % 4
        ch = chunks_of(j)
        xo = np.zeros((NCH, CW, D), np.float32)
        hv = np.zeros((128, NCH * HALO), np.float32)
        pos = np.zeros(OWN, np.int64)
        for lc, m in enumerate(ch):
            s0 = m * 256
            xo[lc, HALO:] = x[b, s0:s0 + 256]
            if m > 0:
                xo[lc, :HALO] = x[b, s0 - HALO:s0]
                hv[:, lc * HALO:(lc + 1) * HALO] = 1.0
            pos[lc * 256:(lc + 1) * 256] = np.arange(s0, s0 + 256)
        refs = np.zeros((128, NKB, NQB), np.float32)
        for qb in range(NQB):
            pl = pos[qb * 128 + 127]
            refs[pl % 128, pl // 128, qb] = 1.0
        d = dict(shared)
        d.update({
            "xfull": f(x[b]), "xown": xo, "halov": hv, "cvec": col(inp["c"][b]),
            "qposB": rep(pos.astype(np.float32)), "refsel": refs.reshape(128, NKB * NQB),
        })
        maps.append(d)
        poss.append((b, pos))
    return maps, poss


def kernel(**inp):
    inp = {k: np.asarray(v) for k, v in inp.items()}
    nc, _ = build()
    maps, poss = host_inputs(inp)
    res = run_bass_kernel_spmd(nc, maps, core_ids=list(range(NCORES)))
    out = np.zeros((2, S, D), np.float32)
    for c in range(NCORES):
        b, pos = poss[c]
        out[b, pos] = res.results[c]["out"]
    return out
```

```python
import numpy as np
from contextlib import ExitStack
import concourse.bass as bass
import concourse.mybir as mybir
from concourse.bass_utils import run_bass_kernel_spmd

F32 = mybir.dt.float32
BF16 = mybir.dt.bfloat16
I32 = mybir.dt.int32
U32 = mybir.dt.uint32
AF = mybir.ActivationFunctionType
ALU = mybir.AluOpType

ENGS = ("sync", "scalar", "vector", "gpsimd", "tensor")
NCORES = 8
D = 1024
KD = 8
S = 8192
NKB = 64
OWN = 2048
NCH = 8
NQB = 16
HALO = 32
CW = 256 + HALO
NE = 32
KMAX = 7
C_UV, C_UG, C_Q, C_K, C_V, C_F, C_GC, C_GA = 0, 512, 1024, 1536, 2048, 2560, 2568, 3592
_DT_SIZE = {F32: 4, BF16: 2, I32: 4, U32: 4}
SEM_ROT = 2000
NPOOL = 24
NSW = 12


class Tok:
    __slots__ = ("sem", "val", "gen")

    def __init__(self, sem=None, val=None, gen=0):
        self.sem = sem
        self.val = val
        self.gen = gen


class Rec:
    __slots__ = ("p0", "p1", "lo", "hi", "w", "tok", "eng", "alive", "name")


def region(ap):
    es = _DT_SIZE[ap.dtype]
    pairs = ap.ap
    off = ap.offset
    sp = str(ap.space)
    if sp in ("SB", "PSUM"):
        pstride, npart = pairs[0]
        if pstride <= 0:
            pstride = 1 << 40
        p0 = off // pstride
        col = off % pstride
        ext = 1 + sum((c - 1) * abs(s) for s, c in pairs[1:])
        return (ap.name, p0, p0 + npart, col * es, (col + ext) * es, 4096)
    ext = 1 + sum((c - 1) * abs(s) for s, c in pairs)
    return (ap.name, 0, 1, off * es, (off + ext) * es, 1 << 20)


class Prog:
    def __init__(self, nc, es):
        self.nc = nc
        self.es = es
        self.streams = {e: [] for e in ENGS}
        self.esem = {}
        self.ecount = {}
        self.nsem = 0
        self.pending = {e: [] for e in ENGS}
        self.bins = {}
        self.pool = []
        self.pool_last = []
        self.pool_i = 0
        self.dma_uid = 0
        self.noguard = 0
        self.guard = None
        self.fake = {}
        self.cnt_ap = None
        for e in ENGS:
            self._new_esem(e)
        self.qpool = {q: [self._mksem(f"dq_{q}{i}") for i in range(n)] for q, n in (("sync", 16), ("scalar", 12))}
        self.qlast = {q: [None] * len(v) for q, v in self.qpool.items()}
        self.qi = {q: 0 for q in self.qpool}
        self.swpool = [self._mksem(f"sq{i}") for i in range(NSW)]
        self.sw_last = [None] * NSW
        self.sw_i = 0

    def _mksem(self, name):
        self.nsem += 1
        return self.es.enter_context(self.nc.semaphore(name))

    def _new_esem(self, e):
        self.esem[e] = self._mksem(f"s_{e}_{self.nsem}")
        self.ecount[e] = 0

    def _bins(self, reg):
        name, p0, p1, lo, hi, bs = reg
        return [(name, b) for b in range(lo // bs, (hi - 1) // bs + 1)]

    def _query(self, reg):
        name, p0, p1, lo, hi, bs = reg
        seen = set()
        out = []
        for key in self._bins(reg):
            lst = self.bins.get(key)
            if not lst:
                continue
            dead = 0
            for r in lst:
                if not r.alive:
                    dead += 1
                    continue
                if id(r) in seen:
                    continue
                if r.lo < hi and lo < r.hi and r.p0 < p1 and p0 < r.p1:
                    seen.add(id(r))
                    out.append(r)
            if dead > 16:
                self.bins[key] = [r for r in lst if r.alive]
        return out

    def _add(self, reg, w, tok, eng):
        name, p0, p1, lo, hi, bs = reg
        r = Rec()
        r.p0, r.p1, r.lo, r.hi, r.w, r.tok, r.eng, r.alive, r.name = p0, p1, lo, hi, w, tok, eng, True, name
        for key in self._bins(reg):
            self.bins.setdefault(key, []).append(r)

    def _deps(self, eng, reads, writes, tok, engkey):
        deps = []
        rregs = [a if isinstance(a, tuple) else region(a) for a in reads]
        wregs = [a if isinstance(a, tuple) else region(a) for a in writes]
        for reg in rregs:
            for r in self._query(reg):
                if r.w and not (eng == "tensor" and r.eng == "tensor"):
                    deps.append(r.tok)
            if reg[0] == "parena":
                name, p0, p1, lo, hi, bs = reg
                breg = (name, 0, 128, (lo // 2048) * 2048, ((hi + 2047) // 2048) * 2048, bs)
                for r in self._query(breg):
                    if (not r.w) and r.eng != engkey and r.eng != "tensor":
                        deps.append(r.tok)
        for reg in wregs:
            for r in self._query(reg):
                if not (eng == "tensor" and r.eng == "tensor"):
                    deps.append(r.tok)
        for reg in rregs:
            name, p0, p1, lo, hi, bs = reg
            rep = False
            for r in self._query(reg):
                if (not r.w) and r.eng == engkey and r.p0 == p0 and r.p1 == p1 and r.lo == lo and r.hi == hi:
                    r.tok = tok
                    rep = True
                    break
            if not rep:
                self._add(reg, False, tok, engkey)
        for reg in wregs:
            name, p0, p1, lo, hi, bs = reg
            for r in self._query(reg):
                if r.p0 >= p0 and r.p1 <= p1 and r.lo >= lo and r.hi <= hi:
                    r.alive = False
            self._add(reg, True, tok, engkey)
        return deps

    def op(self, eng, fn, r=(), w=(), signal=True):
        if signal:
            if self.ecount[eng] >= SEM_ROT:
                self._new_esem(eng)
            self.ecount[eng] += 1
            tok = Tok(self.esem[eng], self.ecount[eng])
            for p in self.pending[eng]:
                p.sem, p.val = tok.sem, tok.val
            self.pending[eng] = []
        else:
            tok = Tok()
            self.pending[eng].append(tok)
        deps = self._deps(eng, r, w, tok, eng)
        self.streams[eng].append((fn, deps, tok if signal else None, 1, self.guard))
        return tok

    def regload(self, key, deps):
        for e in ENGS:
            self.streams[e].append((("regload", key), list(deps), None, 0, None))

    def dma(self, eng, fn, r=(), w=()):
        if eng == "gpsimd":
            i = self.sw_i
            self.sw_i = (self.sw_i + 1) % NSW
            prev = self.sw_last[i]
            val = (prev.val if prev is not None else 0) + 16
            tok = Tok(self.swpool[i], val)
            self.sw_last[i] = tok
            self.dma_uid += 1
            deps = self._deps(eng, r, w, tok, "dma%d" % self.dma_uid)
            if prev is not None:
                deps.append(prev)
            self.streams[eng].append((fn, deps, tok, 16, self.guard))
            return tok
        pool, last = self.qpool[eng], self.qlast[eng]
        i = self.qi[eng]
        self.qi[eng] = (i + 1) % len(pool)
        prev = last[i]
        val = (prev.val if prev is not None else 0) + 16
        tok = Tok(pool[i], val)
        last[i] = tok
        self.dma_uid += 1
        deps = self._deps(eng, r, w, tok, "dma%d" % self.dma_uid)
        if prev is not None:
            deps.append(prev)
        self.streams[eng].append((fn, deps, tok, 16, self.guard))
        return tok

    def emit(self):
        nc = self.nc
        final = [t for q in self.qlast for t in self.qlast[q] if t is not None] + [t for t in self.sw_last if t is not None]
        with nc.Block() as block:
            for e in ENGS:
                stream = self.streams[e]
                fw = final if e == "sync" else []

                def body(engine, stream=stream, fw=fw, ename=e):
                    waited = {}
                    lastfake = [None]
                    reg = engine.alloc_register("gcnt")

                    def dowait(d):
                        assert d.sem is not None, "unresolved token"
                        k = (id(d.sem), d.gen)
                        if waited.get(k, 0) >= d.val:
                            return
                        waited[k] = d.val
                        engine.wait_ge(d.sem, d.val)

                    def emit_one(ent):
                        fn, deps, tok, inc, g = ent
                        for d in deps:
                            dowait(d)
                        if isinstance(fn, tuple):
                            if self.noguard != 2:
                                engine.reg_load(reg, self.cnt_ap(fn[1]))
                            return
                        ins = fn(engine)
                        if tok is not None:
                            ins.then_inc(tok.sem, inc)

                    i = 0
                    n = len(stream)
                    while i < n:
                        g = stream[i][4]
                        if g is None or self.noguard:
                            emit_one(stream[i])
                            i += 1
                            continue
                        j = i
                        while j < n and stream[j][4] == g:
                            j += 1
                        grp = stream[i:j]
                        saved = dict(waited)
                        with engine.If_lt(reg, g[1] + 1):
                            for fn, deps, tok, inc, _ in grp:
                                for d in deps:
                                    dowait(d)
                                if tok is None:
                                    continue
                                if inc == 16:
                                    engine.sem_inc(tok.sem, 16)
                                else:
                                    if ename == "scalar" and lastfake[0] is not None:
                                        engine.wait_ge(lastfake[0].sem, lastfake[0].val)
                                    self.fake[ename](engine).then_inc(tok.sem, 1)
                                    lastfake[0] = tok
                        waited.clear()
                        waited.update(saved)
                        with engine.Else():
                            for ent in grp:
                                emit_one(ent)
                        waited.clear()
                        waited.update(saved)
                        i = j
                    for d in fw:
                        dowait(d)

                getattr(block, e)(body)


class Arena:
    def __init__(self, ap, nwords):
        self.ap = ap
        self.n = nwords
        self.off = 0

    def mark(self):
        return self.off

    def reset(self, off=0):
        self.off = off

    def alloc(self, shape, dt):
        free = int(np.prod(shape[1:]))
        words = (free * _DT_SIZE[dt] + 3) // 4
        words += words % 2
        assert self.off + words <= self.n, ("arena overflow", self.off, words, self.n)
        v = self.ap[0:shape[0], self.off:self.off + words]
        self.off += words
        if dt != F32:
            v = v.bitcast(dt)
        v = v[:, 0:free]
        if len(shape) > 2:
            names = [f"a{i}" for i in range(len(shape) - 1)]
            pat = "p (" + " ".join(names) + ") -> p " + " ".join(names)
            v = v.rearrange(pat, **{n: s for n, s in zip(names[:-1], shape[1:-1])})
        return v


def chunks_of(j):
    ch = []
    for g in range(4):
        ch += [8 * g + j, 8 * g + 7 - j]
    return ch


def kb_end(lc):
    g, s = lc // 2, lc % 2
    return 16 * g + 8 * s + 8


def win_start(lc):
    g, s = lc // 2, lc % 2
    return 16 * g + 8 * s


def isap(x):
    return hasattr(x, "ap") and hasattr(x, "dtype") and hasattr(x, "offset")


def build(debug=False, kmax=KMAX, stop_after=99, variant=''):
    cap = kmax * 128
    opts = {}
    for vv in variant.split("_"):
        if len(vv) > 1 and vv[1:].isdigit():
            opts[vv[0]] = int(vv[1:])
    nc = bass.Bass("TRN2", target_bir_lowering=False)
    dram_in = lambda n, shp, dt=F32: nc.dram_tensor(n, list(shp), dt, kind="ExternalInput").ap()
    xfull = dram_in("xfull", [S, D])
    xown = dram_in("xown", [NCH, CW, D])
    halov = dram_in("halov", [128, NCH * HALO])
    cvec = dram_in("cvec", [128, KD])
    w_ada = dram_in("w_ada", [D, 6 * D])
    b_ada_col = dram_in("b_ada_col", [128, 48])
    n1g_col = dram_in("n1g_col", [128, KD])
    n2g_rep = dram_in("n2g_rep", [128, D])
    fin_rep = dram_in("fin_rep", [128, D])
    w_in = dram_in("w_in", [D, 4616])
    bf_col = dram_in("bf_col", [8, 1])
    cwT = dram_in("cwT", [128, 4 * 31])
    cvecs = dram_in("cvecs", [128, 12])
    w_co = dram_in("w_co", [512, D])
    w_ao = dram_in("w_ao", [512, D])
    w_out = dram_in("w_out", [D, D])
    w_router = dram_in("w_router", [D, NE])
    br_rep = dram_in("br_rep", [128, NE])
    qposB = dram_in("qposB", [128, OWN])
    refsel = dram_in("refsel", [128, NKB * NQB])
    kposc = dram_in("kposc", [128, NKB])
    ecap = dram_in("ecap", [128, NE])
    if stop_after >= 5:
        w_e1 = dram_in("w_e1", [NE, D, 2 * D])
        b1c = dram_in("b1c", [128, NE * 16])
        w_e2 = dram_in("w_e2", [NE, D, D])
        b_e2 = dram_in("b_e2", [NE, D])
    out = nc.dram_tensor("out", [OWN, D], F32, kind="ExternalOutput").ap()
    dbg = {}

    def dbg_out(name, shp, dt=F32):
        dbg[name] = nc.dram_tensor("dbg_" + name, list(shp), dt, kind="ExternalOutput").ap()
        return dbg[name]

    scr = lambda n, shp, dt: nc.dram_tensor(n, list(shp), dt, kind="Internal").ap()
    cg_d = scr("cg_d", [NCH, 128, KD * 256], BF16)
    sga_d = scr("sga_d", [NCH, 128, KD * 256], BF16)
    kvB_d = scr("kvB_d", [128, 2 * S + NKB * 4 * 65], BF16)
    x1_d = scr("x1_d", [NQB, 128, D], F32)
    xs_d = scr("xs_d", [NE * cap + 128, D], BF16)
    ys_d = scr("ys_d", [NE * cap + 128, D], F32)
    f_d = scr("f_d", [8, S], F32)

    with ExitStack() as es:
        SBW = 51 * 1024
        sb_all = es.enter_context(nc.sbuf_tensor("arena", [128, SBW], F32))
        ps_all = es.enter_context(nc.psum_tensor("parena", [128, 4096], F32))
        A = Arena(sb_all, SBW)
        P = Prog(nc, es)

        def bank(i, n=1):
            return ps_all[:, i * 512:(i + n) * 512]

        def act(out, in_, func, scale=None, bias=None, accum=None):
            r = [in_] + [x for x in (scale, bias) if isap(x)]
            w = [out] + ([accum] if accum is not None else [])
            kw = {}
            if scale is not None:
                kw["scale"] = scale
            if bias is not None:
                kw["bias"] = bias
            if accum is not None:
                kw["accum_out"] = accum
            return P.op("scalar", lambda e: e.activation(out=out, in_=in_, func=func, **kw), r, w)

        def tsc(eng, out, in0, s1, s2=None, op0=ALU.mult, op1=None):
            r = [in0] + [x for x in (s1, s2) if isap(x)]
            kw = {} if op1 is None else {"op1": op1}
            return P.op(eng, lambda e: e.tensor_scalar(out=out, in0=in0, scalar1=s1, scalar2=s2, op0=op0, **kw), r, [out])

        def tt(eng, out, in0, in1, op):
            return P.op(eng, lambda e: e.tensor_tensor(out=out, in0=in0, in1=in1, op=op), [in0, in1], [out])

        def stt(out, in0, scalar, in1, op0, op1):
            r = [in0, in1] + ([scalar] if isap(scalar) else [])
            return P.op("vector", lambda e: e.scalar_tensor_tensor(out=out, in0=in0, scalar=scalar, in1=in1, op0=op0, op1=op1), r, [out])

        def ttr(out, in0, in1, accum):
            return P.op("vector", lambda e: e.scalar_tensor_tensor(out=out, in0=in0, scalar=1.0, in1=in1, op0=ALU.mult, op1=ALU.mult, accum_out=accum),
                        [in0, in1], [out, accum])

        def cp(eng, out, in_):
            if eng == "scalar":
                return P.op("scalar", lambda e: e.activation(out=out, in_=in_, func=AF.Copy), [in_], [out])
            return P.op(eng, lambda e: e.tensor_copy(out=out, in_=in_), [in_], [out])

        def mset(eng, ap, v):
            return P.op(eng, lambda e: e.memset(ap, v), [], [ap])

        def recip(out, in_):
            return P.op("vector", lambda e: e.reciprocal(out=out, in_=in_), [in_], [out])

        def bankreg(ap):
            name, p0, p1, lo, hi, bs = region(ap)
            return (name, 0, 128, (lo // 2048) * 2048, ((hi + 2047) // 2048) * 2048, bs)

        def mm(out, lhsT, rhs, start, stop, signal=None):
            if signal is None:
                signal = stop
            return P.op("tensor", lambda e: e.matmul(out, lhsT=lhsT, rhs=rhs, start=start, stop=stop), [lhsT, rhs], [bankreg(out)], signal=signal)

        def tr(out, in_, ident, signal=True):
            return P.op("tensor", lambda e: e.transpose(out=out, in_=in_, identity=ident), [in_, ident], [bankreg(out)], signal=signal)

        def dma(eng, out, in_):
            return P.dma(eng, lambda e: e.dma_start(out=out, in_=in_), [in_], [out])

        def rms_rstd(ssq_ap, rstd_ap):
            tsc("vector", rstd_ap, ssq_ap, 1.0 / D, 1e-5, ALU.mult, ALU.add)
            act(rstd_ap, rstd_ap, AF.Sqrt)
            recip(rstd_ap, rstd_ap)

        identi = A.alloc([128, 128], I32)
        ident_f = A.alloc([128, 128], F32)
        ident_b = A.alloc([128, 128], BF16)
        ones_f = A.alloc([128, 128], F32)
        ones_b = A.alloc([128, 128], BF16)
        ustrict = A.alloc([128, 128], BF16)
        modcol = A.alloc([128, 48], F32)
        a1col = A.alloc([128, KD], F32)
        modrow = A.alloc([128, 4, D], F32)
        a2row = A.alloc([128, D], F32)
        gates = A.alloc([128, NQB, 4], F32)
        dests = A.alloc([128, NQB, 4], I32)
        cnt_i = A.alloc([128, NE], I32)
        fsb = {en: A.alloc([128, 2], F32) for en in ("scalar", "vector", "gpsimd")}
        A_BASE = A.mark()
        qT = A.alloc([128, 4, OWN], BF16)

        P.op("gpsimd", lambda e: e.iota(identi, pattern=[[1, 128]], base=0, channel_multiplier=-1), [], [identi])
        tsc("vector", ident_f, identi, 0, None, ALU.is_equal)
        tsc("vector", ident_b, identi, 0, None, ALU.is_equal)
        tsc("vector", ustrict, identi, 0, None, ALU.is_gt)
        mset("vector", ones_f, 1.0)
        mset("vector", ones_b, 1.0)
        for en in ("scalar", "vector", "gpsimd"):
            mset(en if en != "scalar" else "vector", fsb[en], 0.0)
        fps = bank(7)[:, 510:511]
        P.fake = {
            "tensor": lambda e: e.matmul(fps, lhsT=ones_b, rhs=ones_b[:, 0:1], start=True, stop=True),
            "scalar": lambda e: e.drain(),
            "vector": lambda e: e.engine_nop(),
            "gpsimd": lambda e: e.engine_nop(),
        }
        P.cnt_ap = lambda ei: cnt_i[0:1, ei:ei + 1]
        P.noguard = opts.get("d", 0)
        if opts:
            mset("gpsimd", qT, 0.0)
            mset("gpsimd", gates, 0.0)
            mset("gpsimd", dests, 0)

        m0 = A.mark()
        cv = A.alloc([128, KD], F32)
        scv = A.alloc([128, KD], F32)
        ydiag = A.alloc([128, D], F32)
        bcol = A.alloc([128, 48], F32)
        n1g = A.alloc([128, KD], F32)
        n2g = A.alloc([128, D], F32)
        wab = [A.alloc([128, KD, 512], F32) for _ in range(2)]
        dma("sync", cv, cvec)
        dma("sync", bcol, b_ada_col)
        dma("sync", n1g, n1g_col)
        dma("sync", n2g, n2g_rep)
        act(scv, cv, AF.Silu)
        pmod = bank(0)[:, 0:48]
        wa_v = w_ada.rearrange("(k p) n -> p k n", p=128)
        for gi in range(12):
            buf = wab[gi % 2]
            dma("sync" if gi % 2 == 0 else "scalar", buf, wa_v[:, :, gi * 512:(gi + 1) * 512])
            for oc in range(4):
                col = gi * 4 + oc
                for k in range(KD):
                    mm(pmod[:, col:col + 1], buf[:, k, oc * 128:(oc + 1) * 128], scv[:, k:k + 1], k == 0, k == KD - 1)
        tt("vector", modcol, pmod, bcol, ALU.add)
        stt(a1col, modcol[:, 8:16], 1.0, n1g, ALU.add, ALU.mult)
        sh1col = modcol[:, 0:8]
        for r_, mi in enumerate((2, 3, 4, 5)):
            for k in range(KD):
                tsc("vector", ydiag[:, k * 128:(k + 1) * 128], ident_f, modcol[:, mi * 8 + k:mi * 8 + k + 1], None, ALU.mult)
            for hf in range(2):
                pr = bank(1 + hf)
                mm(pr, ones_f, ydiag[:, hf * 512:(hf + 1) * 512], True, True)
                cp("vector", modrow[:, r_, hf * 512:(hf + 1) * 512], pr)
        stt(a2row, modrow[:, 2, :], 1.0, n2g, ALU.add, ALU.mult)
        if debug:
            dma("sync", dbg_out("modcol", [128, 48]), modcol)
            dma("sync", dbg_out("modrow", [128, 4 * D]), modrow.rearrange("p a b -> p (a b)"))
        A.reset(m0)
        if stop_after <= 0:
            P.emit()
            return nc, dbg

        m1 = A.mark()
        w_own = A.alloc([128, KD, 3584], BF16)
        O_GC, O_GA = 1536, 2560
        wco = A.alloc([128, 4, D], BF16)
        cw_sb = A.alloc([128, 4 * 31], F32)
        cvs = A.alloc([128, 12], F32)
        diag = A.alloc([128, 4 * 31, 128], BF16)
        hv = A.alloc([128, NCH * HALO], F32)
        xo = [A.alloc([128, 3, D], F32) for _ in range(2)]
        xsb = A.alloc([128, 3, D], BF16)
        junk = A.alloc([128, D], BF16)
        ssq = A.alloc([128, 4], F32)
        rstd = A.alloc([128, 4], F32)
        hT = A.alloc([128, KD, CW], BF16)
        sg = A.alloc([128, CW], F32)
        a_bf = A.alloc([128, 4, CW], BF16)
        y_f = A.alloc([128, 4, 256], F32)
        y_b = A.alloc([128, 4, 256], BF16)
        ysq = A.alloc([128, 4, 256], BF16)
        mean_s = A.alloc([128, 256], F32)
        var_s = A.alloc([128, 256], F32)
        yn = A.alloc([128, 256], F32)
        actv = A.alloc([128, 4, 256], BF16)
        sgate = A.alloc([128, 256], F32)
        cg_s = A.alloc([128, KD, 256], BF16)
        sga_s = A.alloc([128, KD, 256], BF16)
        win_v = w_in.rearrange("(k p) n -> p k n", p=128)
        dma("gpsimd", w_own[:, :, 0:1536], win_v[:, :, 0:1536])
        dma("gpsimd", w_own[:, :, 1536:2560], win_v[:, :, C_GC:C_GC + 1024])
        dma("gpsimd", w_own[:, :, 2560:3584], win_v[:, :, C_GA:C_GA + 1024])
        dma("gpsimd", wco, w_co.rearrange("(k p) n -> p k n", p=128))
        dma("sync", cw_sb, cwT)
        dma("sync", cvs, cvecs)
        dma("sync", hv, halov)
        mset("vector", ssq, 1.0)
        for i in range(4 * 31):
            tsc("gpsimd", diag[:, i, :], ident_f, cw_sb[:, i:i + 1], None, ALU.mult)
        convb, lng, lnb = cvs[:, 0:4], cvs[:, 4:8], cvs[:, 8:12]
        pT = [bank(b).bitcast(BF16).rearrange("p (k t) -> p k t", k=KD) for b in range(3)]
        for lc in range(opts.get("c", NCH)):
            xb = xo[lc % 2]
            dma("sync", xb[:, 0:2, :], xown[lc, HALO:CW, :].rearrange("(b p) d -> p b d", p=128))
            dma("sync", xb[0:HALO, 2, :], xown[lc, 0:HALO, :])
            for b in range(3):
                np_ = 128 if b < 2 else HALO
                act(junk[0:np_, :], xb[0:np_, b, :], AF.Square, accum=ssq[0:np_, b:b + 1])
            rms_rstd(ssq, rstd)
            for b in range(3):
                np_ = 128 if b < 2 else HALO
                tsc("vector", xsb[0:np_, b, :], xb[0:np_, b, :], rstd[0:np_, b:b + 1], None, ALU.mult)
            for b in range(3):
                np_ = 128 if b < 2 else HALO
                for k in range(KD):
                    tr(pT[b][:, k, 0:np_], xsb[0:np_, b, k * 128:(k + 1) * 128], ident_b[0:np_, 0:np_], signal=(k == KD - 1))
            for k in range(KD):
                for b in range(3):
                    np_ = 128 if b < 2 else HALO
                    c0 = HALO + b * 128 if b < 2 else 0
                    act(hT[:, k, c0:c0 + np_], pT[b][:, k, 0:np_], AF.Identity, scale=a1col[:, k:k + 1], bias=sh1col[:, k:k + 1])
            for cc in range(4):
                puv, pug = bank(3)[:, 0:CW], bank(4)[:, 0:CW]
                for k in range(KD):
                    mm(puv, w_own[:, k, C_UV + cc * 128:C_UV + (cc + 1) * 128], hT[:, k, :], k == 0, k == KD - 1)
                for k in range(KD):
                    mm(pug, w_own[:, k, C_UG + cc * 128:C_UG + (cc + 1) * 128], hT[:, k, :], k == 0, k == KD - 1)
                act(sg, pug, AF.Sigmoid)
                tt("vector", a_bf[:, cc, :], puv, sg, ALU.mult)
                tt("gpsimd", a_bf[:, cc, 0:HALO], a_bf[:, cc, 0:HALO], hv[:, lc * HALO:(lc + 1) * HALO], ALU.mult)
                pconv = bank(5)[:, 0:256]
                for kk in range(31):
                    mm(pconv, diag[:, cc * 31 + kk, :], a_bf[:, cc, 2 + kk:2 + kk + 256], kk == 0, kk == 30)
                act(y_f[:, cc, :], pconv, AF.Identity, bias=convb[:, cc:cc + 1])
                cp("vector", y_b[:, cc, :], y_f[:, cc, :])
                tt("vector", ysq[:, cc, :], y_f[:, cc, :], y_f[:, cc, :], ALU.mult)
            pmean, pmsq = bank(6)[:, 0:256], bank(7)[:, 0:256]
            for cc in range(4):
                mm(pmean, ones_b, y_b[:, cc, :], cc == 0, cc == 3)
            for cc in range(4):
                mm(pmsq, ones_b, ysq[:, cc, :], cc == 0, cc == 3)
            act(mean_s, pmean, AF.Copy, scale=1.0 / 512)
            tt("vector", var_s, mean_s, mean_s, ALU.mult)
            stt(var_s, pmsq, 1.0 / 512, var_s, ALU.mult, ALU.subtract)
            tsc("vector", var_s, var_s, 1e-5, None, ALU.add)
            act(var_s, var_s, AF.Sqrt)
            recip(var_s, var_s)
            for cc in range(4):
                tt("vector", yn, y_f[:, cc, :], mean_s, ALU.subtract)
                tt("vector", yn, yn, var_s, ALU.mult)
                act(actv[:, cc, :], yn, AF.Silu, scale=lng[:, cc:cc + 1], bias=lnb[:, cc:cc + 1])
            for dmc in range(KD):
                pco, pgc = bank(3)[:, 0:256], bank(4)[:, 0:256]
                for cc in range(4):
                    mm(pco, wco[:, cc, dmc * 128:(dmc + 1) * 128], actv[:, cc, :], cc == 0, cc == 3)
                for k in range(KD):
                    mm(pgc, w_own[:, k, O_GC + dmc * 128:O_GC + (dmc + 1) * 128], hT[:, k, HALO:CW], k == 0, k == KD - 1)
                act(sgate, pgc, AF.Sigmoid)
                tt("vector", cg_s[:, dmc, :], pco, sgate, ALU.mult)
            dma("scalar", cg_d[lc], cg_s.rearrange("p a b -> p (a b)"))
            for dmc in range(KD):
                pga = bank(5 + dmc % 2)[:, 0:256]
                for k in range(KD):
                    mm(pga, w_own[:, k, O_GA + dmc * 128:O_GA + (dmc + 1) * 128], hT[:, k, HALO:CW], k == 0, k == KD - 1)
                act(sga_s[:, dmc, :], pga, AF.Sigmoid)
            dma("scalar", sga_d[lc], sga_s.rearrange("p a b -> p (a b)"))
            for hp in range(4):
                pq = bank(3 + hp % 2)[:, 0:256]
                for k in range(KD):
                    mm(pq, w_own[:, k, C_Q + hp * 128:C_Q + (hp + 1) * 128], hT[:, k, HALO:CW], k == 0, k == KD - 1)
                cp("vector", qT[:, hp, lc * 256:(lc + 1) * 256], pq)
        if debug:
            dma("sync", dbg_out("qT", [128, 4 * OWN], BF16), qT.rearrange("p a b -> p (a b)"))
        A.reset(m1)
        if stop_after <= 1:
            P.emit()
            return nc, dbg

        attn = A.alloc([128, NQB, 512], BF16)
        KT = A.alloc([128, 2, S], BF16)
        V = A.alloc([128, NKB, 4, 65], BF16)
        Fneg = A.alloc([128, NKB, 8], F32)
        FrefB = A.alloc([128, NQB, 8], F32)
        m2 = A.mark()
        w_kv = A.alloc([128, KD, 1024], BF16)
        wf = A.alloc([128, KD, 128], BF16)
        refs = A.alloc([128, NKB, NQB], F32)
        fst = [A.alloc([8, 256], F32) for _ in range(2)]
        ssq2 = A.alloc([128, 2], F32)
        rstd2 = A.alloc([128, 2], F32)
        junk2 = A.alloc([128, D], BF16)
        bfc = A.alloc([8, 1], F32)
        GrefT = A.alloc([8, NQB], F32)
        Eexp = A.alloc([8, NQB, 8], F32)
        kst = [A.alloc([128, 2, 256], BF16) for _ in range(2)]
        vst = [A.alloc([128, 2, 4, 65], BF16) for _ in range(2)]
        m_alias = A.mark()
        xf = [A.alloc([128, 2, D], F32) for _ in range(2)]
        xs2 = [A.alloc([128, 2, D], BF16) for _ in range(2)]
        hT2 = [A.alloc([128, KD, 256], BF16) for _ in range(2)]
        if opts:
            mset("gpsimd", KT, 0.0)
            mset("gpsimd", V, 0.0)
            mset("gpsimd", attn, 0.0)
        dma("gpsimd", w_kv, win_v[:, :, C_K:C_K + 1024])
        mset("vector", wf, 0.0)
        dma("gpsimd", wf[:, :, 0:8], win_v[:, :, C_F:C_F + 8])
        dma("sync", refs.rearrange("p a b -> p (a b)"), refsel)
        dma("sync", bfc, bf_col)
        mset("gpsimd", V[:, :, :, 64:65], 1.0)
        for i in range(2):
            mset("gpsimd", vst[i][:, :, :, 64:65], 1.0)
        kvB_K = kvB_d[:, 0:2 * S].rearrange("p (a b) -> p a b", a=2)
        kvB_V = kvB_d[:, 2 * S:2 * S + NKB * 260].rearrange("p (a b) -> p a b", a=NKB)
        pTv = ps_all[:, 0:1024].bitcast(BF16).rearrange("p (b k t) -> p b k t", b=2, k=KD)
        NG = opts.get("g", 32)
        for gi in range(NG):
            i2 = gi % 2
            xb, xsb_, hb = xf[i2], xs2[i2], hT2[i2]
            dma("sync" if i2 == 0 else "scalar", xb, xfull[gi * 256:(gi + 1) * 256, :].rearrange("(b p) d -> p b d", p=128))
            for b in range(2):
                act(junk2, xb[:, b, :], AF.Square, accum=ssq2[:, b:b + 1])
            rms_rstd(ssq2, rstd2)
            for b in range(2):
                tsc("vector", xsb_[:, b, :], xb[:, b, :], rstd2[:, b:b + 1], None, ALU.mult)
            for b in range(2):
                for k in range(KD):
                    tr(pTv[:, b, k, :], xsb_[:, b, k * 128:(k + 1) * 128], ident_b, signal=(b == 1 and k == KD - 1))
            for k in range(KD):
                act(hb[:, k, :].rearrange("p (b t) -> p b t", b=2), pTv[:, :, k, :], AF.Identity, scale=a1col[:, k:k + 1], bias=sh1col[:, k:k + 1])
            NSK = opts.get("n", 0)
            for hp in range(4 if not (NSK & 2) else 0):
                pk = bank(2 + hp % 2)[:, 0:256]
                for k in range(KD):
                    mm(pk, w_kv[:, k, hp * 128:(hp + 1) * 128], hb[:, k, :], k == 0, k == KD - 1)
                if hp < 2:
                    cp("vector", KT[:, hp, gi * 256:(gi + 1) * 256], pk)
                else:
                    cp("vector", kst[i2][:, hp - 2, :], pk)
            if not (NSK & 2):
                dma("sync", kvB_K[:, :, gi * 256:(gi + 1) * 256], kst[i2])
            for b in range(2 if not (NSK & 4) else 0):
                pv = bank(4 + b)
                for k in range(KD):
                    mm(pv, hb[:, k, b * 128:(b + 1) * 128], w_kv[:, k, 512:1024], k == 0, k == KD - 1)
                kb = gi * 2 + b
                if not (NSK & 16):
                    cp("scalar", V[:, kb, :, 0:64], pv[:, 0:256].rearrange("p (h d) -> p h d", h=4))
                if not (NSK & 32):
                    cp("vector", vst[i2][:, b, :, 0:64], pv[:, 256:512].rearrange("p (h d) -> p h d", h=4))
            if not (NSK & 4) and not (NSK & 64):
                dma("sync", kvB_V[:, gi * 2:gi * 2 + 2, :], vst[i2].rearrange("p a b c -> p a (b c)"))
            if NSK & 8:
                continue
            pf = bank(6)[0:8, 0:256]
            pf_full = bank(6)[:, 0:256]
            for k in range(KD):
                mm(pf_full, wf[:, k, :], hb[:, k, :], k == 0, k == KD - 1)
            cp("vector", fst[i2], pf)
            dma("sync", f_d[:, gi * 256:(gi + 1) * 256], fst[i2])
        if opts.get("n", 0) & 1:
            if debug:
                dma("sync", dbg_out("KT", [128, 2 * S], BF16), KT.rearrange("p a b -> p (a b)"))
                dma("sync", dbg_out("V", [128, NKB * 260], BF16), V.rearrange("p a b c -> p (a b c)"))
            P.emit()
            return nc, dbg
        A.reset(m_alias)
        fT = A.alloc([8, S], F32)
        dma("sync", fT, f_d)
        tsc("vector", bfc, bfc, -1.0, None, ALU.mult)
        act(fT, fT, AF.Exp, scale=-1.0, bias=bfc[:, 0:1])
        act(fT, fT, AF.Ln, bias=1.0)
        P.op("vector", lambda e: e.tensor_tensor_scan(out=fT, data0=fT, data1=fT, initial=0.0, op0=ALU.add, op1=ALU.max), [fT], [fT])
        pF = bank(7)
        for kb in range(NKB):
            tr(pF[:, kb * 8:(kb + 1) * 8], fT[0:8, kb * 128:(kb + 1) * 128], ident_f[0:8, 0:8], signal=(kb == NKB - 1))
        cp("vector", Fneg.rearrange("p a b -> p (a b)"), pF)
        pG = bank(6)[0:8, 0:NQB]
        for kb in range(NKB):
            mm(pG, Fneg[:, kb, :], refs[:, kb, :], kb == 0, kb == NKB - 1)
        cp("vector", GrefT, pG)
        tt("vector", Eexp, GrefT.unsqueeze(2).to_broadcast([8, NQB, 8]), ident_f[0:8, 0:8].unsqueeze(1).to_broadcast([8, NQB, 8]), ALU.mult)
        pB = bank(5)[:, 0:128]
        mm(pB, ones_f[0:8, :], Eexp.rearrange("p a b -> p (a b)"), True, True)
        cp("vector", FrefB.rearrange("p a b -> p (a b)"), pB)
        if debug:
            dma("sync", dbg_out("KT", [128, 2 * S], BF16), KT.rearrange("p a b -> p (a b)"))
            dma("sync", dbg_out("V", [128, NKB * 260], BF16), V.rearrange("p a b c -> p (a b c)"))
            dma("sync", dbg_out("G", [128, NKB * 8]), Fneg.rearrange("p a b -> p (a b)"))
            dma("sync", dbg_out("Gref", [128, NQB * 8]), FrefB.rearrange("p a b -> p (a b)"))
        A.reset(m2)
        if stop_after <= 2:
            P.emit()
            return nc, dbg

        qpos = A.alloc([128, OWN], F32)
        kpos = A.alloc([128, NKB], F32)
        biasq = [A.alloc([128, NKB, 4], F32) for _ in range(2)]
        masks = [A.alloc([128, 8, 128], BF16) for _ in range(2)]
        Pt = A.alloc([128, 8, 128], BF16)
        rden = A.alloc([128, 4], F32)
        dma("sync", qpos, qposB)
        dma("sync", kpos, kposc)
        ztile = A.alloc([128, 2, D], BF16)
        ztile_f = A.alloc([128, 2, D], F32)
        mset("gpsimd", ztile, 0.0)
        mset("gpsimd", ztile_f, 0.0)
        nrows = NE * cap + 128
        for r0 in range(0, nrows, 256):
            nr = min(256, nrows - r0)
            dma("sync", xs_d[r0:r0 + nr, :].rearrange("(r p) d -> p r d", p=128), ztile[:, 0:nr // 128, :])
        Sps = ps_all[:, 0:2048].rearrange("p (s t) -> p s t", s=4)[:, :, 0:128]
        NQR = opts.get("q", NQB)
        for half in range(2):
            if half == 1:
                dma("sync", KT, kvB_K)
                dma("scalar", V.rearrange("p a b c -> p a (b c)"), kvB_V)
                for r0 in range(0, nrows, 256):
                    nr = min(256, nrows - r0)
                    dma("sync", ys_d[r0:r0 + nr, :].rearrange("(r p) d -> p r d", p=128), ztile_f[:, 0:nr // 128, :])
            tile_no = 0
            for qb in range(NQR):
                lc = qb // 2
                nkb, ws = kb_end(lc), win_start(lc)
                bq, mk = biasq[qb % 2], masks[qb % 2]
                tt("vector", bq, Fneg[:, :, half * 4:(half + 1) * 4], FrefB[:, qb, half * 4:(half + 1) * 4].unsqueeze(1).to_broadcast([128, NKB, 4]), ALU.subtract)
                tsc("vector", bq, bq, 0.0, None, ALU.min)
                for w in range(8):
                    tsc("gpsimd", mk[:, w, :], qpos[:, qb * 128:(qb + 1) * 128], kpos[:, ws + w:ws + w + 1], None, ALU.is_ge)
                tiles = [(hl, kb) for hl in range(4) for kb in range(nkb)]
                LA = 3

                def emit_S(i, tiles=tiles, qb=qb, half=half, tile_no=tile_no):
                    hl, kb = tiles[i]
                    n = tile_no + i
                    prt = (hl % 2) * 64
                    hpl = hl // 2
                    mm(Sps[:, n % 4, :], KT[prt:prt + 64, hpl, kb * 128:(kb + 1) * 128],
                       qT[prt:prt + 64, half * 2 + hpl, qb * 128:(qb + 1) * 128], True, True)

                for i in range(min(LA, len(tiles))):
                    emit_S(i)
                for i, (hl, kb) in enumerate(tiles):
                    n = tile_no + i
                    if i + LA < len(tiles):
                        emit_S(i + LA)
                    Ob = bank(4 + (hl % 2) + 2 * (qb % 2))
                    act(Pt[:, n % 8, :], Sps[:, n % 4, :], AF.Exp, scale=0.125, bias=bq[:, kb, hl:hl + 1])
                    if kb >= ws:
                        tt("vector", Pt[:, n % 8, :], Pt[:, n % 8, :], mk[:, kb - ws, :], ALU.mult)
                    mm(Ob[:, 0:65], Pt[:, n % 8, :], V[:, kb, hl, :], kb == 0, kb == nkb - 1, signal=True)
                    if kb == nkb - 1:
                        h = half * 4 + hl
                        recip(rden[:, hl:hl + 1], Ob[:, 64:65])
                        tsc("vector", attn[:, qb, h * 64:(h + 1) * 64], Ob[:, 0:64], rden[:, hl:hl + 1], None, ALU.mult)
                tile_no += len(tiles)
        if debug:
            dma("sync", dbg_out("attn", [128, NQB * 512], BF16), attn.rearrange("p a b -> p (a b)"))
        A.reset(m2)
        if stop_after <= 3:
            P.emit()
            return nc, dbg

        wao = A.alloc([128, 4, D], BF16)
        wout = A.alloc([128, KD, D], BF16)
        wr = A.alloc([128, KD, NE], F32)
        brp = A.alloc([128, NE], F32)
        ecp = A.alloc([128, NE], F32)
        maskb = A.alloc([128, NQB, NE], BF16)
        sga_b = [A.alloc([128, KD, 128], BF16) for _ in range(2)]
        cg_b = [A.alloc([128, KD, 128], BF16) for _ in range(2)]
        xblk = [A.alloc([128, D], F32) for _ in range(2)]
        attnT = A.alloc([128, 4, 128], BF16)
        tmpf = A.alloc([128, D], F32)
        mixT = A.alloc([128, KD, 128], BF16)
        x1 = [A.alloc([128, D], F32) for _ in range(2)]
        h2f = A.alloc([128, D], F32)
        h2b = [A.alloc([128, D], BF16) for _ in range(2)]
        h2T = A.alloc([128, KD, 128], F32)
        junk3 = A.alloc([128, D], BF16)
        ssq3 = A.alloc([128, 1], F32)
        rstd3 = A.alloc([128, 1], F32)
        lg = A.alloc([128, NE], F32)
        top8 = A.alloc([128, 8], F32)
        msk = A.alloc([128, NE], F32)
        negm = A.alloc([128, 1], F32)
        ex = A.alloc([128, NE], F32)
        ssum = A.alloc([128, 1], F32)
        g32 = A.alloc([128, NE], F32)
        destf = A.alloc([128, NE], F32)
        oh = A.alloc([128, NE], F32)
        jk32 = A.alloc([128, NE], F32)
        dkf = A.alloc([128, 4], F32)
        dma("gpsimd", wao, w_ao.rearrange("(k p) n -> p k n", p=128))
        dma("gpsimd", wout, w_out.rearrange("(k p) n -> p k n", p=128))
        dma("sync", wr, w_router.rearrange("(k p) n -> p k n", p=128))
        dma("sync", brp, br_rep)
        dma("sync", ecp, ecap)
        for qb in range(NQR):
            lc, off = qb // 2, (qb % 2) * 128
            i2 = qb % 2
            dma("sync", sga_b[i2], sga_d[lc].rearrange("p (a b) -> p a b", a=KD)[:, :, off:off + 128])
            dma("sync", cg_b[i2], cg_d[lc].rearrange("p (a b) -> p a b", a=KD)[:, :, off:off + 128])
            dma("scalar", xblk[i2], xown[lc, HALO + off:HALO + off + 128, :])
            pAT = bank(0).bitcast(BF16)[:, 0:512].rearrange("p (c t) -> p c t", c=4)
            for c4 in range(4):
                tr(pAT[:, c4, :], attn[:, qb, c4 * 128:(c4 + 1) * 128], ident_b, signal=(c4 == 3))
            cp("vector", attnT, pAT)
            pAO = ps_all[:, 512:1536]
            for dmc in range(KD):
                for c4 in range(4):
                    mm(pAO[:, dmc * 128:(dmc + 1) * 128], wao[:, c4, dmc * 128:(dmc + 1) * 128], attnT[:, c4, :], c4 == 0, c4 == 3)
            tt("vector", tmpf, pAO, sga_b[i2].rearrange("p a b -> p (a b)"), ALU.mult)
            tt("vector", mixT.rearrange("p a b -> p (a b)"), tmpf, cg_b[i2].rearrange("p a b -> p (a b)"), ALU.add)
            pD = ps_all[:, 1536:2560]
            for nh in range(2):
                for dk in range(KD):
                    mm(pD[:, nh * 512:(nh + 1) * 512], mixT[:, dk, :], wout[:, dk, nh * 512:(nh + 1) * 512], dk == 0, dk == KD - 1)
            x1b = x1[i2]
            tt("vector", tmpf, pD, modrow[:, 0, :], ALU.mult)
            tt("vector", x1b, tmpf, xblk[i2], ALU.add)
            dma("scalar", x1_d[qb], x1b)
            act(junk3, x1b, AF.Square, accum=ssq3)
            rms_rstd(ssq3, rstd3)
            stt(h2f, x1b, rstd3[:, 0:1], a2row, ALU.mult, ALU.mult)
            tt("vector", h2f, h2f, modrow[:, 1, :], ALU.add)
            cp("scalar", h2b[i2], h2f)
            pHT = ps_all[:, 2560:3584].rearrange("p (k t) -> p k t", k=KD)
            for k in range(KD):
                tr(pHT[:, k, :], h2f[:, k * 128:(k + 1) * 128], ident_f, signal=(k == KD - 1))
            cp("vector", h2T.rearrange("p a b -> p (a b)"), ps_all[:, 2560:3584])
            plog = bank(7)[:, 0:NE]
            for k in range(KD):
                mm(plog, h2T[:, k, :], wr[:, k, :], k == 0, k == KD - 1)
            tt("vector", lg, plog, brp, ALU.add)
            P.op("vector", lambda e: e.max(out=top8, in_=lg), [lg], [top8])
            tsc("vector", msk, lg, top8[:, 3:4], None, ALU.is_ge)
            cp("vector", maskb[:, qb, :], msk)
            tsc("vector", negm, top8[:, 0:1], -1.0, None, ALU.mult)
            act(ex, lg, AF.Exp, bias=negm[:, 0:1])
            ttr(ex, ex, msk, ssum)
            recip(ssum, ssum)
            tsc("vector", g32, ex, ssum[:, 0:1], None, ALU.mult)
            ppos = bank(7)[:, 64:64 + NE]
            for b2 in range(qb + 1):
                mm(ppos, ones_b if b2 < qb else ustrict, maskb[:, b2, :], b2 == 0, b2 == qb)
            tt("vector", destf, ppos, ecp, ALU.add)
            for k in range(4):
                tsc("vector", oh, lg, top8[:, k:k + 1], None, ALU.is_equal)
                ttr(jk32, oh, g32, gates[:, qb, k:k + 1])
                ttr(jk32, oh, destf, dkf[:, k:k + 1])
            cp("vector", dests[:, qb, :], dkf)
            for k in range(4):
                P.dma("gpsimd", lambda e, qb=qb, k=k, i2=i2: e.indirect_dma_start(
                    out=xs_d[:, :], out_offset=bass.IndirectOffsetOnAxis(ap=dests[:, qb, k:k + 1], axis=0),
                    in_=h2b[i2], in_offset=None), [h2b[i2], dests[:, qb, k:k + 1]], [xs_d[:, :]])
        pcnt = bank(7)[:, 128:128 + NE]
        for b2 in range(NQR):
            mm(pcnt, ones_b, maskb[:, b2, :], b2 == 0, b2 == NQR - 1)
        t_cnt = cp("vector", cnt_i, pcnt)
        if debug:
            dma("sync", dbg_out("cnt", [128, NE], I32), cnt_i)
            dma("sync", dbg_out("lg", [128, NE]), lg)
            dma("sync", dbg_out("top8", [128, 8]), top8)
            dma("sync", dbg_out("h2f", [128, D]), h2f)
            dma("sync", dbg_out("gates", [128, NQB * 4]), gates.rearrange("p a b -> p (a b)"))
            dma("sync", dbg_out("dests", [128, NQB * 4], I32), dests.rearrange("p a b -> p (a b)"))
            d_x1 = dbg_out("x1", [NQB, 128, D])
            for qb in range(NQR):
                dma("sync", xblk[0], x1_d[qb])
                dma("sync", d_x1[qb], xblk[0])
        A.reset(A_BASE)
        if stop_after <= 4:
            P.emit()
            return nc, dbg

        W1 = [A.alloc([128, KD, 2 * D], BF16) for _ in range(2)]
        W2 = [A.alloc([128, KD, D], BF16) for _ in range(2)]
        b1s = A.alloc([128, NE * 16], F32)
        b2r = [A.alloc([128, D], F32) for _ in range(2)]
        xbk = [A.alloc([128, D], BF16) for _ in range(2)]
        xT = [A.alloc([128, KD, 128], BF16) for _ in range(2)]
        gm = [A.alloc([128, 512], F32) for _ in range(2)]
        sgm = [A.alloc([128, 512], F32) for _ in range(2)]
        uc = [A.alloc([128, 512], F32) for _ in range(2)]
        hmT = [A.alloc([128, 8, 128], BF16) for _ in range(2)]
        yb = [A.alloc([128, D], F32) for _ in range(2)]
        dma("sync", b1s, b1c)
        NEr = opts.get("e", NE)
        stg = [A.alloc([128, KD, 256], F32) for _ in range(4)]
        stg_i = [0]

        def expert_chunks(ei):
            wb1_, wb2_ = W1[ei % 2], W2[ei % 2]
            w1v = w_e1[ei].rearrange("(k p) n -> p k n", p=128)
            w2v = w_e2[ei].rearrange("(k p) n -> p k n", p=128)
            ch = []
            for c in range(12):
                if c < 8:
                    ch.append((w1v[:, :, c * 256:(c + 1) * 256], wb1_[:, :, c * 256:(c + 1) * 256]))
                else:
                    ch.append((w2v[:, :, (c - 8) * 256:(c - 7) * 256], wb2_[:, :, (c - 8) * 256:(c - 7) * 256]))
            return ch

        def w_dma(ei, c):
            src, dst = expert_chunks(ei)[c]
            dma("sync", stg[(ei * 8 + c) % 4], src)

        def w_cast(ei, c):
            src, dst = expert_chunks(ei)[c]
            cp("scalar", dst, stg[(ei * 8 + c) % 4])

        NCHK = 8

        def w2_dma(ei):
            dma("gpsimd", W2[ei % 2], w_e2[ei].rearrange("(k p) n -> p k n", p=128))

        dma("sync", b2r[0], b_e2[0:1, :].to_broadcast([128, D]))
        w2_dma(0)
        for c in range(4):
            w_dma(0, c)
        for c in range(NCHK):
            w_cast(0, c)
            if c + 4 < NCHK:
                w_dma(0, c + 4)
        for e_ in range(NEr):
            wb1, wb2 = W1[e_ % 2], W2[e_ % 2]
            nxt = e_ + 1 if e_ + 1 < NEr else None
            if nxt is not None:
                dma("sync", b2r[nxt % 2], b_e2[nxt:nxt + 1, :].to_broadcast([128, D]))
                w2_dma(nxt)
                for c in range(4):
                    w_dma(nxt, c)
            P.regload(e_, [t_cnt])
            dma("scalar", xbk[0], xs_d[e_ * cap:e_ * cap + 128, :])

            def st_T(kb_):
                P.guard = (e_, kb_ * 128)
                xk = xbk[kb_ % 2]
                xTk = xT[kb_ % 2]
                pXT = bank(0).bitcast(BF16).rearrange("p (k t) -> p k t", k=KD)
                for k in range(KD):
                    tr(pXT[:, k, :], xk[:, k * 128:(k + 1) * 128], ident_b, signal=(k == KD - 1))
                cp("vector", xTk.rearrange("p a b -> p (a b)"), bank(0).bitcast(BF16))
                P.guard = None

            def st_H(kb_, half2):
                P.guard = (e_, kb_ * 128)
                xTk = xT[kb_ % 2]
                gm_, uc_, sgm_ = gm[half2], uc[half2], sgm[half2]
                pg, pu = bank(1 + half2), bank(3 + half2)
                for j4 in range(4):
                    fc = half2 * 4 + j4
                    for k in range(KD):
                        mm(pg[:, j4 * 128:(j4 + 1) * 128], wb1[:, k, fc * 128:(fc + 1) * 128], xTk[:, k, :], k == 0, k == KD - 1)
                for j4 in range(4):
                    fc = 8 + half2 * 4 + j4
                    for k in range(KD):
                        mm(pu[:, j4 * 128:(j4 + 1) * 128], wb1[:, k, fc * 128:(fc + 1) * 128], xTk[:, k, :], k == 0, k == KD - 1)
                for j4 in range(4):
                    fcg = half2 * 4 + j4
                    tsc("vector", gm_[:, j4 * 128:(j4 + 1) * 128], pg[:, j4 * 128:(j4 + 1) * 128], b1s[:, e_ * 16 + fcg:e_ * 16 + fcg + 1], 7.0, ALU.add, ALU.min)
                    tsc("vector", uc_[:, j4 * 128:(j4 + 1) * 128], pu[:, j4 * 128:(j4 + 1) * 128], b1s[:, e_ * 16 + 8 + fcg:e_ * 16 + 8 + fcg + 1], 7.0, ALU.add, ALU.min)
                act(sgm_, gm_, AF.Silu, scale=1.702)
                tsc("vector", uc_, uc_, -7.0, 1.0, ALU.max, ALU.add)
                stt(hmT[kb_ % 2][:, half2 * 4:(half2 + 1) * 4, :].rearrange("p a b -> p (a b)"), uc_, 1.0 / 1.702, sgm_, ALU.mult, ALU.mult)
                P.guard = None

            def st_Y(kb_):
                P.guard = (e_, kb_ * 128)
                row0 = e_ * cap + kb_ * 128
                pY = ps_all[:, 2560:3584]
                for nh in range(2):
                    for fc in range(8):
                        mm(pY[:, nh * 512:(nh + 1) * 512], hmT[kb_ % 2][:, fc, :], wb2[:, fc, nh * 512:(nh + 1) * 512], fc == 0, fc == 7)
                ybk = yb[kb_ % 2]
                tt("vector", ybk, pY, b2r[e_ % 2], ALU.add)
                dma("scalar", ys_d[row0:row0 + 128, :], ybk)
                P.guard = None

            def st_prefetch(kb_):
                if kb_ < kmax:
                    P.guard = (e_, kb_ * 128)
                    dma("scalar", xbk[kb_ % 2], xs_d[e_ * cap + kb_ * 128:e_ * cap + kb_ * 128 + 128, :])
                    P.guard = None

            st_prefetch(1)
            st_T(0)
            st_H(0, 0)
            for kblk in range(kmax):
                st_H(kblk, 1)
                if kblk + 1 < kmax:
                    st_prefetch(kblk + 2)
                    st_T(kblk + 1)
                    st_H(kblk + 1, 0)
                st_Y(kblk)
                if nxt is not None:
                    for c in (3 * kblk, 3 * kblk + 1, 3 * kblk + 2):
                        if c < NCHK:
                            w_cast(nxt, c)
                            if c + 4 < NCHK:
                                w_dma(nxt, c + 4)
        A.reset(A_BASE)
        if stop_after <= 5:
            P.emit()
            return nc, dbg

        yk = [A.alloc([128, D], F32) for _ in range(4)]
        acc = A.alloc([128, D], F32)
        x1r = [A.alloc([128, D], F32) for _ in range(2)]
        finr = A.alloc([128, D], F32)
        junk5 = A.alloc([128, D], BF16)
        ssq5 = A.alloc([128, 1], F32)
        rstd5 = A.alloc([128, 1], F32)
        ob = [A.alloc([128, D], F32) for _ in range(2)]
        dma("sync", finr, fin_rep)
        for qb in range(NQR):
            i2 = qb % 2
            dma("sync", x1r[i2], x1_d[qb])
            for k in range(4):
                P.dma("gpsimd", lambda e, qb=qb, k=k: e.indirect_dma_start(
                    out=yk[k], out_offset=None, in_=ys_d[:, :],
                    in_offset=bass.IndirectOffsetOnAxis(ap=dests[:, qb, k:k + 1], axis=0)), [ys_d[:, :], dests[:, qb, k:k + 1]], [yk[k]])
            tsc("vector", acc, yk[0], gates[:, qb, 0:1], None, ALU.mult)
            for k in range(1, 4):
                stt(acc, yk[k], gates[:, qb, k:k + 1], acc, ALU.mult, ALU.add)
            tt("vector", acc, acc, modrow[:, 3, :], ALU.mult)
            tt("vector", acc, acc, x1r[i2], ALU.add)
            act(junk5, acc, AF.Square, accum=ssq5)
            rms_rstd(ssq5, rstd5)
            stt(ob[i2], acc, rstd5[:, 0:1], finr, ALU.mult, ALU.mult)
            dma("sync", out[qb * 128:(qb + 1) * 128, :], ob[i2])
        P.emit()
    return nc, dbg


def host_inputs(inp, kmax=KMAX):
    cap = kmax * 128
    f = lambda a: np.ascontiguousarray(a, dtype=np.float32)
    x = inp["x"]
    col = lambda v: f(v.reshape(-1, 128).T)
    rep = lambda v: f(np.broadcast_to(v.reshape(1, -1), (128, v.size)))
    shared = {
        "w_ada": f(inp["w_ada"][0]),
        "b_ada_col": col(inp["b_ada"][0]),
        "n1g_col": col(inp["norm1_g"][0]),
        "n2g_rep": rep(inp["norm2_g"][0]),
        "fin_rep": rep(inp["final_g"]),
        "w_in": f(inp["w_in"][0]),
        "bf_col": f(inp["b_forget"][0].reshape(8, 1)),
        "cwT": f(inp["conv_w"][0].T.reshape(4, 128, 31).transpose(1, 0, 2).reshape(128, 124)),
        "cvecs": f(np.concatenate([col(inp["conv_b"][0]), col(inp["conv_ln_g"][0]), col(inp["conv_ln_b"][0])], axis=1)),
        "w_co": f(inp["w_conv_out"][0]),
        "w_ao": f(inp["w_attn_out"][0]),
        "w_out": f(inp["w_out"][0]),
        "w_router": f(inp["w_router"][0]),
        "br_rep": rep(inp["b_router"][0]),
        "w_e1": f(inp["w_exp_in"][0]),
        "b1c": f(inp["b_exp_in"][0].reshape(NE, 16, 128).transpose(2, 0, 1).reshape(128, NE * 16)),
        "w_e2": f(inp["w_exp_out"][0]),
        "b_e2": f(inp["b_exp_out"][0]),
        "kposc": f((np.arange(NKB)[None, :] * 128 + np.arange(128)[:, None])),
        "ecap": rep(np.arange(NE, dtype=np.float32) * cap),
    }
    maps = []
    poss = []
    for c in range(NCORES):
        b, j = c // 4, c % 4
        ch = chunks_of(j)
        xo = np.zeros((NCH, CW, D), np.float32)
        hv = np.zeros((128, NCH * HALO), np.float32)
        pos = np.zeros(OWN, np.int64)
        for lc, m in enumerate(ch):
            s0 = m * 256
            xo[lc, HALO:] = x[b, s0:s0 + 256]
            if m > 0:
                xo[lc, :HALO] = x[b, s0 - HALO:s0]
                hv[:, lc * HALO:(lc + 1) * HALO] = 1.0
            pos[lc * 256:(lc + 1) * 256] = np.arange(s0, s0 + 256)
        refs = np.zeros((128, NKB, NQB), np.float32)
        for qb in range(NQB):
            pl = pos[qb * 128 + 127]
            refs[pl % 128, pl // 128, qb] = 1.0
        d = dict(shared)
        d.update({
            "xfull": f(x[b]), "xown": xo, "halov": hv, "cvec": col(inp["c"][b]),
            "qposB": rep(pos.astype(np.float32)), "refsel": refs.reshape(128, NKB * NQB),
        })
        maps.append(d)
        poss.append((b, pos))
    return maps, poss


def kernel(**inp):
    inp = {k: np.asarray(v) for k, v in inp.items()}
    nc, _ = build()
    maps, poss = host_inputs(inp)
    res = run_bass_kernel_spmd(nc, maps, core_ids=list(range(NCORES)))
    out = np.zeros((2, S, D), np.float32)
    for c in range(NCORES):
        b, pos = poss[c]
        out[b, pos] = res.results[c]["out"]
    return out
```
